# Optimizing a Trainium2 kernel written in Bass

```python
import math
import jax, jax.numpy as jnp
from jax import lax
import numpy as np

D_MODEL = 4096
BATCH = 8
SEQ = 2048
DEPTH = 4

HEAD_DIM = 128
MIX_WIDTH = D_MODEL // 2
MLSTM_HEADS = MIX_WIDTH // (4 * HEAD_DIM)
NSA_HEADS = MIX_WIDTH // (2 * HEAD_DIM)
RET_HEADS = MIX_WIDTH // (4 * HEAD_DIM)
NSA_KV_GROUPS = 2
NSA_GROUP_SIZE = NSA_HEADS // NSA_KV_GROUPS
MLSTM_WIDTH = MLSTM_HEADS * HEAD_DIM
NSA_WIDTH = NSA_HEADS * HEAD_DIM
RET_WIDTH = RET_HEADS * HEAD_DIM
NSA_KV_WIDTH = NSA_KV_GROUPS * HEAD_DIM
IN_COLS = 4 * MLSTM_WIDTH + 2 * MLSTM_HEADS + NSA_WIDTH + 6 * NSA_KV_WIDTH + 3 * NSA_HEADS + 4 * RET_WIDTH

MLSTM_CHUNK = 64
CONV_WIDTH = 4
RET_CHUNK = 128
CMP_BLOCK = 32
CMP_STRIDE = 16
SEL_BLOCK = 64
SEL_TOPK = 8
SEL_Q_BLOCK = 64
WINDOW = 512
WIN_Q_BLOCK = 128
REL_BUCKETS = 32
REL_MAX_DIST = 128
D_FF = 4 * D_MODEL
GATE_RANK = D_MODEL // 4
N_BRANCH = 3
EPS = 1e-6
NEG_INF = -1e30

kernel_name = "hybrid_mlstm_nsa_retention_block"


def rms_norm(x, gain):
    x32 = x.astype(jnp.float32)
    y = x32 * lax.rsqrt(jnp.mean(jnp.square(x32), axis=-1, keepdims=True) + EPS)
    return (y * gain).astype(x.dtype)


def head_norm(h, gain):
    h = h.astype(jnp.float32)
    mu = jnp.mean(h, axis=-1, keepdims=True)
    var = jnp.mean(jnp.square(h - mu), axis=-1, keepdims=True)
    y = (h - mu) * lax.rsqrt(var + EPS)
    return y.reshape(h.shape[0], h.shape[1], -1) * gain


def causal_depthwise_conv(x, w):
    k_width, c = w.shape
    return lax.conv_general_dilated(x, w[:, None, :].astype(x.dtype), window_strides=(1,),
                                    padding=[(k_width - 1, 0)], dimension_numbers=("NWC", "WIO", "NWC"),
                                    feature_group_count=c)


def rope(x, pos):
    half = x.shape[-1] // 2
    inv_freq = 1.0 / (10000.0 ** jnp.linspace(0.0, 1.0, half))
    ang = pos.astype(jnp.float32)[:, None] * inv_freq[None, :]
    cos, sin = jnp.cos(ang), jnp.sin(ang)
    x1, x2 = x[..., :half], x[..., half:]
    return jnp.concatenate([x1 * cos - x2 * sin, x1 * sin + x2 * cos], axis=-1)


def rel_bucket(dist):
    n = jnp.maximum(dist, 0)
    exact = REL_BUCKETS // 2
    log_ratio = jnp.log(jnp.maximum(n, 1).astype(jnp.float32) / exact) / math.log(REL_MAX_DIST / exact)
    large = jnp.minimum(exact + (log_ratio * (REL_BUCKETS - exact)).astype(jnp.int32), REL_BUCKETS - 1)
    return jnp.where(n < exact, n, large)


def split_in_proj(z):
    sizes = ([MLSTM_WIDTH] * 4 + [MLSTM_HEADS] * 2 + [NSA_WIDTH] + [NSA_KV_WIDTH] * 6
             + [3 * NSA_HEADS] + [RET_WIDTH] * 4)
    offsets = [int(o) for o in np.cumsum(sizes)[:-1]]
    return jnp.split(z, offsets, axis=-1)


def mlstm_chunkwise(q, k, v, i_pre, f_pre):
    b, h, s, dh = q.shape
    l = MLSTM_CHUNK
    n = s // l
    q = q.reshape(b, h, n, l, dh)
    k = k.reshape(b, h, n, l, dh) * (dh ** -0.5)
    v = v.reshape(b, h, n, l, dh)
    i_pre = i_pre.reshape(b, h, n, l)
    a = jnp.cumsum(jax.nn.log_sigmoid(f_pre).reshape(b, h, n, l), axis=-1)
    g = a[..., -1]
    w = g[..., None] - a + i_pre
    m_loc = jnp.max(w, axis=-1)
    e = jnp.exp(w - m_loc[..., None])
    c_chunk = jnp.einsum("bhnl,bhnlv,bhnlk->nbhvk", e, v, k)
    n_chunk = jnp.einsum("bhnl,bhnlk->nbhk", e, k)

    def step(carry, inp):
        c, nv, m = carry
        c_k, n_k, m_k, g_k = inp
        m_new = jnp.maximum(g_k + m, m_k)
        a_old = jnp.exp(g_k + m - m_new)
        a_new = jnp.exp(m_k - m_new)
        c_new = a_old[..., None, None] * c + a_new[..., None, None] * c_k
        n_new = a_old[..., None] * nv + a_new[..., None] * n_k
        return (c_new, n_new, m_new), (c, nv, m)

    init = (jnp.zeros((b, h, dh, dh), jnp.float32), jnp.zeros((b, h, dh), jnp.float32),
            jnp.zeros((b, h), jnp.float32))
    xs = (c_chunk, n_chunk, m_loc.transpose(2, 0, 1), g.transpose(2, 0, 1))
    _, (c_prev, n_prev, m_prev) = lax.scan(step, init, xs)
    c_prev = c_prev.transpose(1, 2, 0, 3, 4)
    n_prev = n_prev.transpose(1, 2, 0, 3)
    m_prev = m_prev.transpose(1, 2, 0)

    causal = jnp.tril(jnp.ones((l, l), dtype=bool))
    log_d = jnp.where(causal, a[..., :, None] - a[..., None, :] + i_pre[..., None, :], -jnp.inf)
    log_inter = a + m_prev[..., None]
    m_row = jnp.maximum(log_inter, jnp.max(log_d, axis=-1))
    inter = jnp.exp(log_inter - m_row)
    sc = jnp.einsum("bhnik,bhnjk->bhnij", q, k) * jnp.exp(log_d - m_row[..., None])
    num = inter[..., None] * jnp.einsum("bhnik,bhnvk->bhniv", q, c_prev) + jnp.einsum("bhnij,bhnjv->bhniv", sc, v)
    den = inter * jnp.einsum("bhnik,bhnk->bhni", q, n_prev) + jnp.sum(sc, axis=-1)
    out = num / jnp.maximum(jnp.abs(den), jnp.exp(-m_row))[..., None]
    return out.reshape(b, h, s, dh)


def retention_chunkwise(q, k, v):
    b, h, s, dh = q.shape
    l = RET_CHUNK
    n = s // l
    log_gamma = jnp.log1p(-jnp.exp2(-5.0 - jnp.arange(h, dtype=jnp.float32)))
    idx = jnp.arange(l, dtype=jnp.float32)
    rel = idx[:, None] - idx[None, :]
    decay = jnp.where(rel >= 0, jnp.exp(log_gamma[:, None, None] * jnp.maximum(rel, 0.0)), 0.0)
    xi = jnp.exp(log_gamma[:, None] * (idx + 1.0))
    zeta = jnp.exp(log_gamma[:, None] * (l - 1.0 - idx))
    chunk_decay = jnp.exp(log_gamma * l)
    q = q.reshape(b, h, n, l, dh)
    k = k.reshape(b, h, n, l, dh) * (dh ** -0.5)
    v = v.reshape(b, h, n, l, dh)
    kv_chunk = jnp.einsum("hl,bhnlk,bhnlv->nbhkv", zeta, k, v)

    def step(r, kv_n):
        return chunk_decay[:, None, None] * r + kv_n, r

    _, r_prev = lax.scan(step, jnp.zeros((b, h, dh, dh), jnp.float32), kv_chunk)
    r_prev = r_prev.transpose(1, 2, 0, 3, 4)
    inner = jnp.einsum("bhnik,bhnjk->bhnij", q, k) * decay[:, None]
    out = (jnp.einsum("bhnij,bhnjv->bhniv", inner, v)
           + xi[:, None, :, None] * jnp.einsum("bhnik,bhnkv->bhniv", q, r_prev))
    return out.reshape(b, h, s, dh)


def nsa_attention(q, k_cmp_raw, v_cmp_raw, k_slc, v_slc, k_win, v_win, gate_pre, rel_bias,
                  cmp_pos_k, cmp_w1_k, cmp_w2_k, cmp_pos_v, cmp_w1_v, cmp_w2_v):
    b, s, _ = q.shape
    g_n, r_n, dh = NSA_KV_GROUPS, NSA_GROUP_SIZE, HEAD_DIM
    f32 = jnp.float32
    q = q.astype(f32).reshape(b, s, g_n, r_n, dh).transpose(0, 2, 3, 1, 4) * (dh ** -0.5)

    def kv(t):
        return t.astype(f32).reshape(b, s, g_n, dh).transpose(0, 2, 1, 3)

    tbl = rel_bias.astype(f32).T.reshape(g_n, r_n, REL_BUCKETS)
    t_pos = jnp.arange(s)

    n_cmp = (s - CMP_BLOCK) // CMP_STRIDE + 1
    starts = jnp.arange(n_cmp) * CMP_STRIDE
    win_idx = starts[:, None] + jnp.arange(CMP_BLOCK)[None, :]

    def compress(t, pos_emb, w1, w2):
        blocks = kv(t)[:, :, win_idx] + pos_emb
        return jax.nn.gelu(blocks.reshape(b, g_n, n_cmp, CMP_BLOCK * dh) @ w1) @ w2

    k_c = compress(k_cmp_raw, cmp_pos_k, cmp_w1_k, cmp_w2_k)
    v_c = compress(v_cmp_raw, cmp_pos_v, cmp_w1_v, cmp_w2_v)
    dist_c = t_pos[:, None] - (starts + CMP_BLOCK - 1)[None, :]
    valid_c = dist_c >= 0
    s_c = jnp.einsum("bgrtd,bgjd->bgrtj", q, k_c) + tbl[:, :, rel_bucket(dist_c)]
    p_c = jax.nn.softmax(jnp.where(valid_c, s_c, NEG_INF), axis=-1) * valid_c
    o_c = jnp.einsum("bgrtj,bgjd->bgrtd", p_c, v_c)

    n_sel = s // SEL_BLOCK
    top = min(SEL_TOPK, n_sel)
    sel_start = jnp.arange(n_sel) * SEL_BLOCK
    overlap = ((starts[:, None] < sel_start[None, :] + SEL_BLOCK)
               & (starts[:, None] + CMP_BLOCK > sel_start[None, :])).astype(f32)
    imp = jnp.einsum("bgrtj,js->bgts", p_c, overlap)
    blk = jnp.arange(n_sel)[None, :]
    cur = (t_pos // SEL_BLOCK)[:, None]
    forced = (blk == 0) | (blk == cur) | (blk == cur - 1)
    imp = jnp.where(forced, jnp.inf, jnp.where(blk > cur, -jnp.inf, imp))
    _, sel_idx = lax.top_k(imp, top)

    k_sb = kv(k_slc).reshape(b, g_n, n_sel, SEL_BLOCK, dh)
    v_sb = kv(v_slc).reshape(b, g_n, n_sel, SEL_BLOCK, dh)
    n_qb = s // SEL_Q_BLOCK
    q_ch = q.reshape(b, g_n, r_n, n_qb, SEL_Q_BLOCK, dh).transpose(3, 0, 1, 2, 4, 5)
    idx_ch = sel_idx.reshape(b, g_n, n_qb, SEL_Q_BLOCK, top).transpose(2, 0, 1, 3, 4)
    t_ch = t_pos.reshape(n_qb, SEL_Q_BLOCK)
    b_ix = jnp.arange(b)[:, None, None, None]
    g_ix = jnp.arange(g_n)[:, None, None]
    g_ix5 = jnp.arange(g_n)[None, :, None, None, None]
    r_ix5 = jnp.arange(r_n)[None, None, :, None, None]
    n_keys = top * SEL_BLOCK

    def sel_block(args):
        qb, ib, tb = args
        kb = k_sb[b_ix, g_ix, ib].reshape(b, g_n, SEL_Q_BLOCK, n_keys, dh)
        vb = v_sb[b_ix, g_ix, ib].reshape(b, g_n, SEL_Q_BLOCK, n_keys, dh)
        key_pos = (ib[..., None] * SEL_BLOCK + jnp.arange(SEL_BLOCK)).reshape(b, g_n, SEL_Q_BLOCK, n_keys)
        dist = tb[:, None] - key_pos
        bias = tbl[g_ix5, r_ix5, rel_bucket(dist)[:, :, None]]
        sc = jnp.einsum("bgrqd,bgqkd->bgrqk", qb, kb) + bias
        p = jax.nn.softmax(jnp.where(dist[:, :, None] >= 0, sc, NEG_INF), axis=-1)
        return jnp.einsum("bgrqk,bgqkd->bgrqd", p, vb)

    o_s = lax.map(sel_block, (q_ch, idx_ch, t_ch))
    o_s = o_s.transpose(1, 2, 3, 0, 4, 5).reshape(b, g_n, r_n, s, dh)

    nb = s // WIN_Q_BLOCK
    n_back = WINDOW // WIN_Q_BLOCK

    def band(t):
        tp = jnp.pad(kv(t), ((0, 0), (0, 0), (WINDOW, 0), (0, 0))).reshape(b, g_n, nb + n_back, WIN_Q_BLOCK, dh)
        return jnp.concatenate([tp[:, :, i:i + nb] for i in range(n_back + 1)], axis=3)

    k_wb, v_wb = band(k_win), band(v_win)
    qi = jnp.arange(WIN_Q_BLOCK)
    kj = jnp.arange(WINDOW + WIN_Q_BLOCK)
    dist_w = WINDOW + qi[:, None] - kj[None, :]
    key_pos_w = (jnp.arange(nb)[:, None] - n_back) * WIN_Q_BLOCK + kj[None, :]
    valid_w = ((dist_w >= 0) & (dist_w < WINDOW))[None] & (key_pos_w >= 0)[:, None, :]
    q_wb = q.reshape(b, g_n, r_n, nb, WIN_Q_BLOCK, dh)
    s_w = jnp.einsum("bgrnqd,bgnkd->bgrnqk", q_wb, k_wb) + tbl[:, :, rel_bucket(dist_w)][:, :, None]
    p_w = jax.nn.softmax(jnp.where(valid_w, s_w, NEG_INF), axis=-1)
    o_w = jnp.einsum("bgrnqk,bgnkd->bgrnqd", p_w, v_wb).reshape(b, g_n, r_n, s, dh)

    gt = jax.nn.sigmoid(gate_pre.astype(f32)).reshape(b, s, 3, g_n, r_n).transpose(2, 0, 3, 4, 1)[..., None]
    o = gt[0] * o_c + gt[1] * o_s + gt[2] * o_w
    return o.transpose(0, 3, 1, 2, 4).reshape(b, s, NSA_WIDTH)


def token_mixer(h, rel_bias, w_in, b_in, conv_qk, mlstm_norm_g, cmp_pos_k, cmp_w1_k, cmp_w2_k,
                cmp_pos_v, cmp_w1_v, cmp_w2_v, ret_norm_g, w_br_mlstm, w_br_nsa, w_br_ret,
                w_gate_down, w_gate_up, b_gate, w_out):
    b, s, _ = h.shape
    f32 = jnp.float32
    (m_q, m_k, m_v, m_o, m_i, m_f, n_q, n_kc, n_vc, n_ks, n_vs, n_kw, n_vw, n_gate,
     r_q, r_k, r_v, r_g) = split_in_proj(h @ w_in + b_in)

    def to_heads(t, n_heads):
        return t.astype(f32).reshape(b, s, n_heads, HEAD_DIM).transpose(0, 2, 1, 3)

    qk = jax.nn.silu(causal_depthwise_conv(jnp.concatenate([m_q, m_k], axis=-1), conv_qk))
    m_q, m_k = jnp.split(qk, 2, axis=-1)
    h_a = mlstm_chunkwise(to_heads(m_q, MLSTM_HEADS), to_heads(m_k, MLSTM_HEADS), to_heads(m_v, MLSTM_HEADS),
                          m_i.astype(f32).transpose(0, 2, 1), m_f.astype(f32).transpose(0, 2, 1))
    y_a = jax.nn.sigmoid(m_o.astype(f32)) * head_norm(h_a.transpose(0, 2, 1, 3), mlstm_norm_g)

    y_b = nsa_attention(n_q, n_kc, n_vc, n_ks, n_vs, n_kw, n_vw, n_gate, rel_bias,
                        cmp_pos_k, cmp_w1_k, cmp_w2_k, cmp_pos_v, cmp_w1_v, cmp_w2_v)

    pos = jnp.arange(s)
    h_c = retention_chunkwise(rope(to_heads(r_q, RET_HEADS), pos), rope(to_heads(r_k, RET_HEADS), pos),
                              to_heads(r_v, RET_HEADS))
    y_c = jax.nn.silu(r_g.astype(f32)) * head_norm(h_c.transpose(0, 2, 1, 3), ret_norm_g)

    g_low = h @ w_gate_down

    def gate(i):
        lo, hi = i * D_MODEL, (i + 1) * D_MODEL
        return jax.nn.sigmoid((g_low @ w_gate_up[:, lo:hi] + b_gate[lo:hi]).astype(f32))

    merged = gate(0) * (y_a @ w_br_mlstm) + gate(1) * (y_b @ w_br_nsa) + gate(2) * (y_c @ w_br_ret)
    return (merged @ w_out).astype(h.dtype)


def squared_relu_mlp(h, w_up, w_down):
    return (jnp.square(jax.nn.relu(h @ w_up)) @ w_down).astype(h.dtype)


def setup_inputs(seed: int = 0) -> dict:
    key = jax.random.key(seed)
    ks = jax.random.split(key, 32)

    def nrm(k, shape, scale):
        return jax.random.normal(k, shape, jnp.float32) * scale

    f_off = 4 * MLSTM_WIDTH + MLSTM_HEADS
    b_in = nrm(ks[4], (DEPTH, IN_COLS), 0.02)
    b_in = b_in.at[:, f_off:f_off + MLSTM_HEADS].add(jnp.linspace(3.0, 6.0, MLSTM_HEADS))
    cmp_in = CMP_BLOCK * HEAD_DIM
    return {
        "x": nrm(ks[0], (BATCH, SEQ, D_MODEL), 1.0),
        "rel_bias": nrm(ks[1], (REL_BUCKETS, NSA_HEADS), 0.5),
        "norm_mix_g": 1.0 + nrm(ks[2], (DEPTH, D_MODEL), 0.02),
        "w_in": nrm(ks[3], (DEPTH, D_MODEL, IN_COLS), D_MODEL ** -0.5),
        "b_in": b_in,
        "conv_qk": nrm(ks[5], (DEPTH, CONV_WIDTH, 2 * MLSTM_WIDTH), CONV_WIDTH ** -0.5),
        "mlstm_norm_g": 1.0 + nrm(ks[6], (DEPTH, MLSTM_WIDTH), 0.02),
        "cmp_pos_k": nrm(ks[7], (DEPTH, CMP_BLOCK, HEAD_DIM), 0.02),
        "cmp_w1_k": nrm(ks[8], (DEPTH, cmp_in, HEAD_DIM), cmp_in ** -0.5),
        "cmp_w2_k": nrm(ks[9], (DEPTH, HEAD_DIM, HEAD_DIM), HEAD_DIM ** -0.5),
        "cmp_pos_v": nrm(ks[10], (DEPTH, CMP_BLOCK, HEAD_DIM), 0.02),
        "cmp_w1_v": nrm(ks[11], (DEPTH, cmp_in, HEAD_DIM), cmp_in ** -0.5),
        "cmp_w2_v": nrm(ks[12], (DEPTH, HEAD_DIM, HEAD_DIM), HEAD_DIM ** -0.5),
        "ret_norm_g": 1.0 + nrm(ks[13], (DEPTH, RET_WIDTH), 0.02),
        "w_br_mlstm": nrm(ks[14], (DEPTH, MLSTM_WIDTH, D_MODEL), MLSTM_WIDTH ** -0.5),
        "w_br_nsa": nrm(ks[15], (DEPTH, NSA_WIDTH, D_MODEL), NSA_WIDTH ** -0.5),
        "w_br_ret": nrm(ks[16], (DEPTH, RET_WIDTH, D_MODEL), RET_WIDTH ** -0.5),
        "w_gate_down": nrm(ks[17], (DEPTH, D_MODEL, GATE_RANK), D_MODEL ** -0.5),
        "w_gate_up": nrm(ks[18], (DEPTH, GATE_RANK, N_BRANCH * D_MODEL), GATE_RANK ** -0.5),
        "b_gate": nrm(ks[19], (DEPTH, N_BRANCH * D_MODEL), 0.02),
        "w_out": nrm(ks[20], (DEPTH, D_MODEL, D_MODEL), D_MODEL ** -0.5),
        "norm_mlp_g": 1.0 + nrm(ks[21], (DEPTH, D_MODEL), 0.02),
        "w_up": nrm(ks[22], (DEPTH, D_MODEL, D_FF), D_MODEL ** -0.5),
        "w_down": nrm(ks[23], (DEPTH, D_FF, D_MODEL), D_FF ** -0.5),
        "final_norm_g": 1.0 + nrm(ks[24], (D_MODEL,), 0.02),
    }


def reference(x, rel_bias, norm_mix_g, w_in, b_in, conv_qk, mlstm_norm_g, cmp_pos_k, cmp_w1_k, cmp_w2_k,
              cmp_pos_v, cmp_w1_v, cmp_w2_v, ret_norm_g, w_br_mlstm, w_br_nsa, w_br_ret,
              w_gate_down, w_gate_up, b_gate, w_out, norm_mlp_g, w_up, w_down, final_norm_g):
    for l in range(DEPTH):
        h = rms_norm(x, norm_mix_g[l])
        x = x + token_mixer(h, rel_bias, w_in[l], b_in[l], conv_qk[l], mlstm_norm_g[l],
                            cmp_pos_k[l], cmp_w1_k[l], cmp_w2_k[l], cmp_pos_v[l], cmp_w1_v[l], cmp_w2_v[l],
                            ret_norm_g[l], w_br_mlstm[l], w_br_nsa[l], w_br_ret[l],
                            w_gate_down[l], w_gate_up[l], b_gate[l], w_out[l])
        h = rms_norm(x, norm_mlp_g[l])
        x = x + squared_relu_mlp(h, w_up[l], w_down[l])
    return rms_norm(x, final_norm_g)
```

```python
import numpy as np
import os
from contextlib import ExitStack
import concourse.bass as bass
import concourse.mybir as mybir
from concourse.bass_utils import run_bass_kernel_spmd

F32 = mybir.dt.float32
BF16 = mybir.dt.bfloat16
AF = mybir.ActivationFunctionType
ALU = mybir.AluOpType
AX = mybir.AxisListType

S = 2048
D = 4096
KC = D // 128
TB = 512
NTB = S // TB
EPS = 1e-6
ZROWS = 53 * 128


ENGS = ["pe", "act", "dve", "pool", "sp"]
_UID = [0]


def _sbt(nc, name, shape, dt):
    _UID[0] += 1
    return nc.sbuf_tensor(f"{name}_{_UID[0]}", shape, dt)


class Ctx:
    def __init__(self, nc, n_hw=52, n_sw=40):
        self.nc = nc
        self.esem = {e: nc.alloc_semaphore(name=f"eng_{e}") for e in ENGS[:4]}
        self.hw = [nc.alloc_semaphore(name=f"hw_{i}") for i in range(n_hw)]
        self.sw = [nc.alloc_semaphore(name=f"sw_{i}") for i in range(n_sw)]
        self.base = {}
        self.psum = None


class Rec:
    def __init__(self, ctx):
        self.ctx = ctx
        self.q = {e: [] for e in ENGS}
        self.cnt = {e: 0 for e in ENGS[:4]}
        self.dcnt = {}
        self.nsem = {"hw": 0, "sw": 0}
        self.pending = None

    def dsem(self, sw=False):
        kind = "sw" if sw else "hw"
        idx = self.nsem[kind]
        self.nsem[kind] += 1
        pool = self.ctx.sw if sw else self.ctx.hw
        assert idx < len(pool), f"out of {kind} dma sems"
        k = (kind, idx)
        self.dcnt[k] = self.ctx.base.get(k, 0)
        return k

    def op(self, eng, fn, deps=(), inc=True):
        deps = tuple(d for d in deps if d is not None)
        self.q[eng].append(("op", fn, deps, inc))
        if inc:
            self.cnt[eng] += 1
            return (eng, self.cnt[eng])
        return None

    def dma(self, eng, fn, deps, semkey):
        deps = tuple(d for d in deps if d is not None)
        assert (semkey[0] == "sw") == (eng == "pool"), (eng, semkey)
        self.q[eng].append(("dma", fn, deps, semkey))
        self.dcnt[semkey] += 16
        return (semkey, self.dcnt[semkey])

    def wait(self, eng, deps):
        deps = tuple(d for d in deps if d is not None)
        self.q[eng].append(("wait", None, deps, None))

    def _sem(self, k):
        if isinstance(k, tuple):
            return (self.ctx.sw if k[0] == "sw" else self.ctx.hw)[k[1]]
        return self.ctx.esem[k]

    def check(self):
        pos = {e: 0 for e in ENGS}
        val = {k: self.ctx.base.get(k, 0) for k in self.dcnt}
        progress = True
        while progress:
            progress = False
            for e in ENGS:
                q = self.q[e]
                while pos[e] < len(q):
                    kind, fn, deps, x = q[pos[e]]
                    if any(val.get(k, 0) < v for (k, v) in deps):
                        break
                    if kind == "op" and x:
                        val[e] = val.get(e, 0) + 1
                    elif kind == "dma":
                        val[x] = val.get(x, 0) + 16
                    pos[e] += 1
                    progress = True
        stuck = {e: (pos[e], len(self.q[e])) for e in ENGS if pos[e] < len(self.q[e])}
        if stuck:
            msg = []
            for e, (p, n) in stuck.items():
                kind, fn, deps, x = self.q[e][p]
                msg.append(f"{e}@{p}/{n} waits {[(k, v, val.get(k, 0)) for k, v in deps if val.get(k, 0) < v]}")
            raise RuntimeError("DEADLOCK in recorded schedule: " + "; ".join(msg))

    def emit(self):
        nc = self.ctx.nc
        self.check()
        self.wait("sp", [(k, v) for k, v in self.dcnt.items() if v > self.ctx.base.get(k, 0)])
        with nc.Block() as block:
            for name in ENGS:
                items = self.q[name]
                if not items:
                    continue

                def run(e, items=items, name=name):
                    known = {}
                    for kind, fn, deps, x in items:
                        for (k, v) in deps:
                            if known.get(k, 0) < v:
                                e.wait_ge(self._sem(k), v)
                                known[k] = v
                        if kind == "wait":
                            continue
                        ins = fn(e)
                        if kind == "op":
                            if x:
                                ins.then_inc(self._sem(name), 1)
                        else:
                            ins.then_inc(self._sem(x), 16)

                {"pe": block.tensor, "act": block.scalar, "dve": block.vector,
                 "pool": block.gpsimd, "sp": block.sync}[name](run)
        used = [self.ctx.esem[e] for e in ENGS[:4] if self.cnt[e] > 0]
        for k, v in self.dcnt.items():
            self.ctx.base[k] = v
        if used:
            nc.all_engine_barrier()
            with nc.Block() as block:
                def clr(e):
                    for s in used:
                        e.sem_clear(s)
                block.gpsimd(clr)
            nc.all_engine_barrier()


class Ring:
    def __init__(self, rec, n, with_sems=True, sw=False):
        self.n = n
        self.sems = [rec.dsem(sw) for _ in range(n)] if with_sems else None
        self.readers = [[] for _ in range(n)]
        self.i = 0

    def next(self):
        s = self.i % self.n
        self.i += 1
        deps = self.readers[s]
        self.readers[s] = []
        return s, deps

    def read(self, s, ticket):
        if ticket is not None:
            self.readers[s].append(ticket)


def phase_norm(ctx, xT, hT, g_cols, out_f32=False):
    nc = ctx.nc
    CG = 8
    NG = KC // CG
    odt = F32 if out_f32 else BF16
    with (_sbt(nc, "n_x", [128, 3, CG, TB], F32) as xb,
          _sbt(nc, "n_sq", [128, 2, CG, TB], BF16) as sq,
          _sbt(nc, "n_o", [128, 2, CG, TB], odt) as ob,
          _sbt(nc, "n_rstd", [128, S], F32) as rstd,
          _sbt(nc, "n_g", [128, KC], F32) as gc,
          _sbt(nc, "n_ones", [128, 128], BF16) as ones):
        r = Rec(ctx)
        ps = ctx.psum
        csem = r.dsem()
        tg = r.dma("sp", lambda e: e.dma_start(out=gc[:], in_=g_cols), [], csem)
        tones = r.op("pool", lambda e: e.memset(ones[:], 1.0))
        xring = Ring(r, 3)
        sqring = Ring(r, 2, with_sems=False)
        xv = xT.rearrange("(c p) t -> p c t", p=128)
        hv = hT.rearrange("(c p) t -> p c t", p=128)
        mm_last = [None] * NTB
        for tb in range(NTB):
            for g in range(NG):
                s, deps = xring.next()
                tl = r.dma("sp", lambda e, s=s, g=g, tb=tb: e.dma_start(
                    out=xb[:, s], in_=xv[:, g * CG:(g + 1) * CG, tb * TB:(tb + 1) * TB]), deps, xring.sems[s])
                q, qdeps = sqring.next()
                ta = r.op("act", lambda e, s=s, q=q: e.activation(out=sq[:, q], in_=xb[:, s], func=AF.Square),
                          [tl] + qdeps)
                xring.read(s, ta)
                for c in range(CG):
                    last = (c == CG - 1)
                    tm = r.op("pe", lambda e, q=q, c=c, tb=tb, g=g: e.matmul(
                        ps[:, tb, :], ones[:], sq[:, q, c, :], start=(g == 0 and c == 0),
                        stop=(g == NG - 1 and c == CG - 1)), [ta, tones] if c == 0 else [], inc=last)
                sqring.read(q, tm)
                mm_last[tb] = tm
        trs = []
        for tb in range(NTB):
            t1 = r.op("act", lambda e, tb=tb: e.activation(out=rstd[:, tb * TB:(tb + 1) * TB], in_=ps[:, tb, :],
                                                          func=AF.Sqrt, bias=EPS, scale=1.0 / D), [mm_last[tb]])
            t2 = r.op("dve", lambda e, tb=tb: e.reciprocal(out=rstd[:, tb * TB:(tb + 1) * TB],
                                                          in_=rstd[:, tb * TB:(tb + 1) * TB]), [t1])
            trs.append(t2)
        oring = Ring(r, 2)
        for tb in range(NTB):
            for g in range(NG):
                s, deps = xring.next()
                tl = r.dma("sp", lambda e, s=s, g=g, tb=tb: e.dma_start(
                    out=xb[:, s], in_=xv[:, g * CG:(g + 1) * CG, tb * TB:(tb + 1) * TB]), deps, xring.sems[s])
                o, odeps = oring.next()
                tv = None
                for c in range(CG):
                    cc = g * CG + c
                    tv = r.op("dve", lambda e, s=s, o=o, c=c, cc=cc, tb=tb: e.scalar_tensor_tensor(
                        out=ob[:, o, c, :], in0=xb[:, s, c, :], scalar=gc[:, cc:cc + 1],
                        in1=rstd[:, tb * TB:(tb + 1) * TB], op0=ALU.mult, op1=ALU.mult),
                        [tl, tg, trs[tb]] + odeps)
                xring.read(s, tv)
                ts = r.dma("sp", lambda e, o=o, g=g, tb=tb: e.dma_start(
                    out=hv[:, g * CG:(g + 1) * CG, tb * TB:(tb + 1) * TB], in_=ob[:, o]), [tv], oring.sems[o])
                oring.read(o, ts)
        r.emit()


_EPS = {}


def EPS_AP(ctx):
    return _EPS["ap"]


def phase_linear(ctx, inT, K, jobs, bias_cols=None, nbias=0):
    nc = ctx.nc
    KCk = K // 128
    with (_sbt(nc, "l_in", [128, KCk, S], BF16) as xin,
          _sbt(nc, "l_w", [128, 3, KCk * 128], BF16) as wb,
          _sbt(nc, "l_sf", [128, 2, S], F32) as sf,
          _sbt(nc, "l_sb", [128, 2, S], BF16) as sb,
          _sbt(nc, "l_bias", [128, max(nbias, 1)], F32) as bc):
        r = Rec(ctx)
        ps = ctx.psum
        csem = r.dsem()
        tb_ = None
        if nbias:
            tb_ = r.dma("sp", lambda e: e.dma_start(out=bc[:], in_=bias_cols), [], csem)
        iv = inT.rearrange("(c p) t -> p c t", p=128)
        tin = []
        CG = 8 if KCk >= 8 else KCk
        for g in range(KCk // CG):
            k = r.dsem()
            tin.append(r.dma("sp", lambda e, g=g: e.dma_start(out=xin[:, g * CG:(g + 1) * CG, :],
                                                             in_=iv[:, g * CG:(g + 1) * CG, :]), [], k))
        wring = Ring(r, 3, sw=True)
        pring = Ring(r, 2, with_sems=False)
        fring = Ring(r, 2)
        bring = Ring(r, 2)
        rsem = [r.dsem(), r.dsem()]
        for m, job in enumerate(jobs):
            ws, wdeps = wring.next()
            tw = r.dma("pool", lambda e, ws=ws, job=job: e.dma_start(out=wb[:, ws, :], in_=job["w"]), wdeps,
                       wring.sems[ws])
            pset, pdeps = pring.next()
            tm = None
            for c in range(KCk):
                for tb in range(NTB):
                    first = (c == 0 and tb == 0)
                    last = (c == KCk - 1 and tb == NTB - 1)
                    deps = []
                    if first:
                        deps = [tw] + pdeps + (tin if m == 0 else [])
                    tm = r.op("pe", lambda e, ws=ws, c=c, tb=tb, pset=pset: e.matmul(
                        ps[:, pset * 4 + tb, :], wb[:, ws, c * 128:(c + 1) * 128], xin[:, c, tb * TB:(tb + 1) * TB],
                        start=(c == 0), stop=(c == KCk - 1)), deps, inc=last)
            wring.read(ws, tm)
            psv = ps[:, pset * 4:(pset + 1) * 4, :]
            kind = job["kind"]
            if kind == "z":
                f, fdeps = fring.next()
                b = job["bias"]
                te = r.op("act", lambda e, f=f, b=b, psv=psv: e.activation(
                    out=sf[:, f, :].rearrange("p (a t) -> p a t", a=4), in_=psv, func=AF.Identity,
                    bias=bc[:, b:b + 1], scale=1.0), [tm, tb_] + fdeps)
                pring.read(pset, te)
                ts = r.dma("sp", lambda e, f=f, job=job: e.dma_start(out=job["out"], in_=sf[:, f, :]), [te],
                           fring.sems[f])
                fring.read(f, ts)
            elif kind == "bf":
                f, fdeps = bring.next()
                te = r.op("act", lambda e, f=f, psv=psv: e.activation(
                    out=sb[:, f, :].rearrange("p (a t) -> p a t", a=4), in_=psv, func=AF.Copy), [tm] + fdeps)
                pring.read(pset, te)
                ts = r.dma("sp", lambda e, f=f, job=job: e.dma_start(out=job["out"], in_=sb[:, f, :]), [te],
                           bring.sems[f])
                bring.read(f, ts)
            elif kind == "res":
                f, fdeps = fring.next()
                tr = r.dma("sp", lambda e, f=f, job=job: e.dma_start(out=sf[:, f, :], in_=job["resid"]), fdeps,
                           rsem[f])
                te = r.op("dve", lambda e, f=f, psv=psv: e.tensor_tensor(
                    out=sf[:, f, :].rearrange("p (a t) -> p a t", a=4),
                    in0=sf[:, f, :].rearrange("p (a t) -> p a t", a=4), in1=psv, op=ALU.add), [tm, tr])
                pring.read(pset, te)
                ts = r.dma("sp", lambda e, f=f, job=job: e.dma_start(out=job["out"], in_=sf[:, f, :]), [te],
                           fring.sems[f])
                fring.read(f, ts)
        r.emit()


def phase_mlp(ctx, hT, xin, xout, wup, wdn, n_ft=128):
    nc = ctx.nc
    G = 2
    NGR = n_ft // G
    with (_sbt(nc, "f_h", [128, KC, TB], BF16) as hb,
          _sbt(nc, "f_x", [128, KC, TB], F32) as xb,
          _sbt(nc, "f_wu", [128, 2, G, KC * 128], BF16) as wu,
          _sbt(nc, "f_wd", [128, 2, G, D], BF16) as wd,
          _sbt(nc, "f_a", [128, 2, G, TB], BF16) as ab):
        r = Rec(ctx)
        ps = ctx.psum
        hv = hT.rearrange("(c p) t -> p c t", p=128)
        xiv = xin.rearrange("(c p) t -> p c t", p=128)
        xov = xout.rearrange("(c p) t -> p c t", p=128)
        wdv = wdn.rearrange("(f p) m -> p f m", p=128)
        wring = Ring(r, 2, sw=True)
        dsems = [r.dsem(True), r.dsem(True)]
        hsem = r.dsem()
        xsem = r.dsem()
        osem = r.dsem()
        aring = Ring(r, 2, with_sems=False)
        upring = Ring(r, 2, with_sems=False)
        dnring = Ring(r, 4, with_sems=False)
        last_store = None
        last_x_readers = []
        last_h_readers = []
        for tb in range(NTB):
            th = r.dma("sp", lambda e, tb=tb: e.dma_start(out=hb[:], in_=hv[:, :, tb * TB:(tb + 1) * TB]),
                       last_h_readers, hsem)
            tx = r.dma("sp", lambda e, tb=tb: e.dma_start(out=xb[:], in_=xiv[:, :, tb * TB:(tb + 1) * TB]),
                       [last_store], xsem)
            tacc = None
            for gr in range(NGR):
                ws, wdeps = wring.next()
                tw = r.dma("pool", lambda e, ws=ws, gr=gr: e.dma_start(
                    out=wu[:, ws], in_=wup[gr * G:(gr + 1) * G].rearrange("g p k -> p g k")), wdeps, wring.sems[ws])
                tw2 = r.dma("pool", lambda e, ws=ws, gr=gr: e.dma_start(
                    out=wd[:, ws], in_=wdv[:, gr * G:(gr + 1) * G, :]), wdeps, dsems[ws])
                a, adeps = aring.next()
                tas = []
                for g in range(G):
                    pb, pdeps = upring.next()
                    tm = None
                    for c in range(KC):
                        deps = ([tw, th] + pdeps) if c == 0 else []
                        tm = r.op("pe", lambda e, ws=ws, g=g, c=c, pb=pb: e.matmul(
                            ps[:, pb, :], wu[:, ws, g, c * 128:(c + 1) * 128], hb[:, c, :],
                            start=(c == 0), stop=(c == KC - 1)), deps, inc=(c == KC - 1))
                    t1 = r.op("act", lambda e, a=a, g=g, pb=pb: e.activation(
                        out=ab[:, a, g, :], in_=ps[:, pb, :], func=AF.Relu), [tm] + adeps)
                    upring.read(pb, t1)
                    t2 = r.op("act", lambda e, a=a, g=g: e.activation(
                        out=ab[:, a, g, :], in_=ab[:, a, g, :], func=AF.Square), [t1])
                    tas.append(t2)
                tmd = None
                for mt in range(KC):
                    db, ddeps = dnring.next()
                    for g in range(G):
                        deps = (tas + [tw2] + ddeps) if g == 0 else []
                        tmd = r.op("pe", lambda e, ws=ws, g=g, mt=mt, a=a, db=db: e.matmul(
                            ps[:, 4 + db, :], wd[:, ws, g, mt * 128:(mt + 1) * 128], ab[:, a, g, :],
                            start=(g == 0), stop=(g == G - 1)), deps, inc=(g == G - 1))
                    tacc = r.op("dve", lambda e, mt=mt, db=db: e.tensor_tensor(
                        out=xb[:, mt, :], in0=xb[:, mt, :], in1=ps[:, 4 + db, :], op=ALU.add), [tmd, tx])
                    dnring.read(db, tacc)
                wring.read(ws, tmd)
                aring.read(a, tmd)
            last_h_readers = [tmd]
            last_store = r.dma("sp", lambda e, tb=tb: e.dma_start(out=xov[:, :, tb * TB:(tb + 1) * TB], in_=xb[:]),
                               [tacc], osem)
        r.emit()


def tile_w(w):
    K, M = w.shape
    return np.ascontiguousarray(w.reshape(K // 128, 128, M // 128, 128).transpose(2, 1, 0, 3)).reshape(
        M // 128, 128, (K // 128) * 128)


def cols_layout(v):
    return np.ascontiguousarray(v.reshape(-1, 128).T)


def in_colmap():
    off = {}
    o = 0
    for name, sz in [("m_q", 512), ("m_k", 512), ("m_v", 512), ("m_o", 512), ("m_i", 4), ("m_f", 4),
                     ("n_q", 1024), ("n_kc", 256), ("n_vc", 256), ("n_ks", 256), ("n_vs", 256),
                     ("n_kw", 256), ("n_vw", 256), ("n_gate", 24), ("r_q", 512), ("r_k", 512),
                     ("r_v", 512), ("r_g", 512)]:
        off[name] = (o, sz)
        o += sz
    order = ["m_q", "m_k", "m_v", "m_o", "n_q", "n_kc", "n_vc", "n_ks", "n_vs", "n_kw", "n_vw",
             "r_q", "r_k", "r_v", "r_g"]
    cm = []
    zoff = {}
    for n in order:
        zoff[n] = len(cm)
        cm += list(range(off[n][0], off[n][0] + off[n][1]))
    small = [-1] * 128
    for i in range(4):
        small[i] = off["m_i"][0] + i
        small[32 + i] = off["m_f"][0] + i
    for i in range(24):
        small[64 + i] = off["n_gate"][0] + i
    zoff["small"] = len(cm)
    cm += small
    return np.array(cm), zoff


def permute_cols(w, cm):
    out = np.zeros(w.shape[:-1] + (len(cm),), w.dtype)
    valid = cm >= 0
    out[..., valid] = w[..., cm[valid]]
    return out


LN_SCALE = -0.5 * float(np.log(128.0))


def phase_decay(ctx, mode, ZT, YT, zq, zk, zv, zg, yrow, cst, par):
    nc = ctx.nc
    ml = (mode == "mlstm")
    NH = 4
    with ExitStack() as _st:
        G = _st.enter_context(_sbt(nc, "d_G", [64, S], F32))
        Gt = _st.enter_context(_sbt(nc, "d_tmp", [64, S], F32))
        G1 = _st.enter_context(_sbt(nc, "d_one64", [64, S], F32))
        Ab = _st.enter_context(_sbt(nc, "d_Ab", [128, NH, S], F32))
        bcol = _st.enter_context(_sbt(nc, "d_bcol", [128, 16, NH], F32))
        sel = _st.enter_context(_sbt(nc, "d_sel", [64, NH * 128], F32))
        i64 = _st.enter_context(_sbt(nc, "d_i64", [64, 64], F32))
        ident = _st.enter_context(_sbt(nc, "d_ident", [128, 128], BF16))
        onesb = _st.enter_context(_sbt(nc, "d_onesb", [128, 128], BF16))
        avg = _st.enter_context(_sbt(nc, "d_avg", [128, 128], BF16))
        U = _st.enter_context(_sbt(nc, "d_U", [128, 896], BF16))
        xf = _st.enter_context(_sbt(nc, "d_x", [128, 2, S], F32))
        acc = _st.enter_context(_sbt(nc, "d_acc", [128, S], F32))
        qT = _st.enter_context(_sbt(nc, "d_qT", [128, S], BF16))
        kT = _st.enter_context(_sbt(nc, "d_kT", [128, S], BF16))
        vT = _st.enter_context(_sbt(nc, "d_vT", [128, S], BF16))
        V = _st.enter_context(_sbt(nc, "d_V", [128, 16, 128], BF16))
        go = _st.enter_context(_sbt(nc, "d_go", [128, S], F32))
        cw = _st.enter_context(_sbt(nc, "d_cw", [128, 32], F32))
        cs = _st.enter_context(_sbt(nc, "d_cs", [128, 2, S], F32))
        rt = _st.enter_context(_sbt(nc, "d_rt", [128, 128], F32))
        gain = _st.enter_context(_sbt(nc, "d_gain", [128, NH], F32))
        Dt = _st.enter_context(_sbt(nc, "d_Dt", [128, 2, TB], F32))
        P = _st.enter_context(_sbt(nc, "d_P", [128, 2, TB], BF16))
        e1 = _st.enter_context(_sbt(nc, "d_e1", [128, TB], F32))
        e2 = _st.enter_context(_sbt(nc, "d_e2", [128, TB], F32))
        e3 = _st.enter_context(_sbt(nc, "d_e3", [128, TB], BF16))
        yb = _st.enter_context(_sbt(nc, "d_y", [128, S], BF16))
        r = Rec(ctx)
        ps = ctx.psum
        ld = lambda dst, src, deps=(): r.dma("sp", lambda e: e.dma_start(out=dst, in_=src), list(deps), r.dsem())
        t_sel = ld(sel[:], cst["sel64"])
        t_i64 = ld(i64[:], cst["i64"])
        t_id = ld(ident[:], cst["ident_bf"])
        t_U = ld(U[:], cst["U_bf"])
        t_on = r.op("pool", lambda e: e.memset(onesb[:], 1.0))
        t_av = r.op("pool", lambda e: e.memset(avg[:], 1.0 / 128.0))
        t_gain = ld(gain[:], par["gain"])
        if ml:
            t_g = ld(G[:], ZT[52 * 128:52 * 128 + 64, :])
            a1 = r.op("act", lambda e: e.activation(out=Gt[32:36, :], in_=G[32:36, :], func=AF.Exp, scale=-1.0), [t_g])
            a2 = r.op("act", lambda e: e.activation(out=Gt[32:36, :], in_=Gt[32:36, :], func=AF.Ln, bias=1.0,
                                                   scale=1.0), [a1])
            a3 = r.op("pool", lambda e: e.memset(G1[32:36, :], 1.0), [])
            t_G = r.op("dve", lambda e: e.tensor_tensor_scan(out=G[32:36, :], data0=G1[32:36, :], data1=Gt[32:36, :],
                                                            initial=0.0, op0=ALU.mult, op1=ALU.subtract), [a2, a3])
        else:
            t_G = ld(G[:], cst["ret_G"])
        tcol = None
        for tt in range(16):
            tcol = r.op("pe", lambda e, tt=tt: e.matmul(
                ps[:, 0:2, :].rearrange("p a (b c) -> p (a b) c", c=64)[:, tt, :], G[:, tt * 128:(tt + 1) * 128],
                i64[:], start=True, stop=True), [t_G, t_i64] if tt == 0 else [], inc=(tt == 15))
        colv = ps[:, 0:2, :].rearrange("p a (b c) -> p (a b) c", c=64)
        t_b0 = r.op("act", lambda e: e.activation(out=bcol[:], in_=colv[:, :, 0:4], func=AF.Copy), [tcol])
        t_b1 = r.op("dve", lambda e: e.tensor_tensor(out=bcol[:], in0=bcol[:], in1=colv[:, :, 32:36],
                                                    op=ALU.subtract), [t_b0])
        t_bc = r.op("dve", lambda e: e.tensor_scalar_add(out=bcol[:], in0=bcol[:], scalar1=LN_SCALE), [t_b1])
        t_ab = []
        bank = 2
        for h in range(NH):
            for tb in range(NTB):
                b = 2 + ((h * NTB + tb) % 4)
                tm = r.op("pe", lambda e, h=h, tb=tb, b=b: e.matmul(
                    ps[:, b, :], sel[:, h * 128:(h + 1) * 128], G[:, tb * TB:(tb + 1) * TB], start=True, stop=True),
                    [t_G, t_sel] + ([t_ab[-4]] if len(t_ab) >= 4 else []))
                tc = r.op("act", lambda e, h=h, tb=tb, b=b: e.activation(
                    out=Ab[:, h, tb * TB:(tb + 1) * TB], in_=ps[:, b, :], func=AF.Copy), [tm])
                t_ab.append(tc)
        t_ab_all = t_ab[-1]
        if ml:
            t_cw = ld(cw[:], par["conv_cols"])
        else:
            t_cos = ld(cs[:, 0, :], cst["cosT"])
            t_sin = ld(cs[:, 1, :], cst["sinT"])
            t_rt = ld(rt[:], cst["rotT"])
        prev_head_done = []
        xr_readers = [[], []]
        ty = None
        ep_prev = None
        for h in range(NH):
            tq = []
            for wi, (zt, dst) in enumerate([(zq + h, qT), (zk + h, kT)]):
                tl = ld(xf[:, wi, :], ZT[zt * 128:(zt + 1) * 128, :], xr_readers[wi] + prev_head_done)
                if ml:
                    j = wi * 4 + h
                    t0 = r.op("dve", lambda e, wi=wi, j=j: e.tensor_scalar(
                        out=acc[:], in0=xf[:, wi, :], scalar1=cw[:, 3 * 8 + j:3 * 8 + j + 1], scalar2=None,
                        op0=ALU.mult), [tl, t_cw] + tq + prev_head_done)
                    for sft in (1, 2, 3):
                        t0 = r.op("dve", lambda e, wi=wi, j=j, sft=sft: e.scalar_tensor_tensor(
                            out=acc[:, sft:], in0=xf[:, wi, 0:S - sft],
                            scalar=cw[:, (3 - sft) * 8 + j:(3 - sft) * 8 + j + 1], in1=acc[:, sft:],
                            op0=ALU.mult, op1=ALU.add), [t0])
                    t1 = r.op("act", lambda e, dst=dst: e.activation(out=dst[:], in_=acc[:], func=AF.Silu), [t0])
                    xr_readers[wi] = [t0]
                    tq.append(t1)
                else:
                    tlast = None
                    for tb in range(NTB):
                        sl = slice(tb * TB, (tb + 1) * TB)
                        b = 2 + (tb % 2)
                        tm = r.op("pe", lambda e, wi=wi, sl=sl, b=b: e.matmul(
                            ps[:, b, :], rt[:], xf[:, wi, sl], start=True, stop=True),
                            [tl, t_rt, t_ab_all] + ([tlast] if tlast else []) + prev_head_done)
                        ta = r.op("pool", lambda e, wi=wi, sl=sl: e.tensor_tensor(
                            out=acc[:, sl], in0=xf[:, wi, sl], in1=cs[:, 0, sl], op=ALU.mult), [tl, t_cos] + tq)
                        tb2 = r.op("dve", lambda e, sl=sl, b=b: e.tensor_tensor(
                            out=e1[:], in0=ps[:, b, :], in1=cs[:, 1, sl], op=ALU.mult), [tm, t_sin] + ([tlast] if tlast else []))
                        tlast = r.op("dve", lambda e, sl=sl, dst=dst: e.tensor_tensor(
                            out=dst[:, sl], in0=acc[:, sl], in1=e1[:], op=ALU.add), [ta, tb2])
                    xr_readers[wi] = [tlast]
                    tq.append(tlast)
            tv = r.dma("pool", lambda e, h=h: e.dma_start(out=vT[:], in_=ZT[(zv + h) * 128:(zv + h + 1) * 128, :]),
                       prev_head_done, r.dsem(True))
            tg_ = ld(go[:], ZT[(zg + h) * 128:(zg + h + 1) * 128, :], prev_head_done)
            tV = None
            for g4 in range(4):
                b = 2 + (g4 % 2)
                for i in range(4):
                    tt = g4 * 4 + i
                    tm = r.op("pe", lambda e, tt=tt, i=i, b=b: e.matmul(
                        ps[:, b, i * 128:(i + 1) * 128], vT[:, tt * 128:(tt + 1) * 128], ident[:],
                        start=True, stop=True), [tv, t_id, t_ab_all] + tq + ([tV] if tV else []), inc=(i == 3))
                tV = r.op("act", lambda e, g4=g4, b=b: e.activation(
                    out=V[:, g4 * 4:(g4 + 1) * 4, :], in_=ps[:, b, :].rearrange("p (a c) -> p a c", c=128),
                    func=AF.Copy), [tm])
            tgo = r.op("act", lambda e: e.activation(out=go[:], in_=go[:], func=AF.Sigmoid if ml else AF.Silu), [tg_])
            dring = Ring(r, 2, with_sems=False)
            pring = Ring(r, 2, with_sems=False)
            sring = Ring(r, 2, with_sems=False)
            for qb in range(NTB):
                qsl = slice(qb * TB, (qb + 1) * TB)
                nk = 4 * qb + 4
                tpv = None
                for kt in range(nk):
                    c = 512 * qb - 128 * kt
                    sb, sdeps = sring.next()
                    tS = r.op("pe", lambda e, kt=kt, qsl=qsl, sb=sb: e.matmul(
                        ps[:, 2 + sb, :], kT[:, kt * 128:(kt + 1) * 128], qT[:, qsl], start=True, stop=True),
                        tq + [tV] + sdeps)
                    d, ddeps = dring.next()
                    tD = r.op("act", lambda e, d=d, h=h, kt=kt, qsl=qsl: e.activation(
                        out=Dt[:, d, :], in_=Ab[:, h, qsl], func=AF.Exp, bias=bcol[:, kt, h:h + 1], scale=1.0),
                        [t_ab_all, t_bc] + ddeps)
                    if c <= 0:
                        tD = r.op("pool", lambda e, d=d, c=c: e.tensor_tensor(
                            out=Dt[:, d, :], in0=Dt[:, d, :], in1=U[:, c + 384:c + 384 + TB], op=ALU.mult),
                            [tD, t_U])
                    p, pdeps = pring.next()
                    tP = r.op("dve", lambda e, p=p, d=d, sb=sb: e.tensor_tensor(
                        out=P[:, p, :], in0=ps[:, 2 + sb, :], in1=Dt[:, d, :], op=ALU.mult), [tS, tD] + pdeps)
                    sring.read(sb, tP)
                    dring.read(d, tP)
                    first = (kt == 0)
                    lastk = (kt == nk - 1)
                    r.op("pe", lambda e, p=p, kt=kt, first=first, lastk=lastk: e.matmul(
                        ps[:, 4, :], V[:, kt, :], P[:, p, :], start=first, stop=lastk),
                        [tP] + ([ep_prev] if first and ep_prev else []), inc=False)
                    tpv = r.op("pe", lambda e, p=p, first=first, lastk=lastk: e.matmul(
                        ps[:, 5, :], onesb[:], P[:, p, :], start=first, stop=lastk), [t_on])
                    pring.read(p, tpv)
                if ml:
                    t1 = r.op("act", lambda e: e.activation(out=e1[:], in_=ps[:, 5, :], func=AF.Abs), [tpv, ty])
                    t1 = r.op("dve", lambda e: e.tensor_scalar_max(out=e1[:], in0=e1[:], scalar1=1.0), [t1])
                    t1 = r.op("dve", lambda e: e.reciprocal(out=e1[:], in_=e1[:]), [t1])
                    t2 = r.op("dve", lambda e: e.tensor_tensor(out=e2[:], in0=ps[:, 4, :], in1=e1[:], op=ALU.mult),
                              [t1])
                else:
                    t2 = r.op("act", lambda e: e.activation(out=e2[:], in_=ps[:, 4, :], func=AF.Copy), [tpv, ty])
                t3 = r.op("act", lambda e: e.activation(out=e3[:], in_=e2[:], func=AF.Copy), [t2])
                tm1 = r.op("pe", lambda e: e.matmul(ps[:, 6, :], avg[:], e3[:], start=True, stop=True), [t3, t_av])
                t4 = r.op("dve", lambda e: e.tensor_tensor(out=e2[:], in0=e2[:], in1=ps[:, 6, :], op=ALU.subtract),
                          [tm1])
                t5 = r.op("act", lambda e: e.activation(out=e3[:], in_=e2[:], func=AF.Square), [t4])
                tm2 = r.op("pe", lambda e: e.matmul(ps[:, 7, :], avg[:], e3[:], start=True, stop=True), [t5])
                t6 = r.op("act", lambda e: e.activation(out=e1[:], in_=ps[:, 7, :], func=AF.Sqrt, bias=EPS,
                                                       scale=1.0), [tm2])
                t7 = r.op("dve", lambda e: e.reciprocal(out=e1[:], in_=e1[:]), [t6])
                t8 = r.op("dve", lambda e: e.tensor_tensor(out=e2[:], in0=e2[:], in1=e1[:], op=ALU.mult), [t7])
                ty = r.op("dve", lambda e, h=h, qsl=qsl: e.scalar_tensor_tensor(
                    out=yb[:, qsl], in0=e2[:], scalar=gain[:, h:h + 1], in1=go[:, qsl], op0=ALU.mult, op1=ALU.mult),
                    [t8, tgo, t_gain])
                ep_prev = t2
            tst = r.dma("sp", lambda e, h=h: e.dma_start(out=YT[yrow + h * 128:yrow + (h + 1) * 128, :], in_=yb[:]),
                        [ty], r.dsem())
            prev_head_done = [tst, ty]
        r.emit()


def bucket_starts():
    n = np.arange(0, 4096)
    exact = 16
    lr = np.log(np.maximum(n, 1).astype(np.float32) / np.float32(exact)) / np.float32(np.log(128 / exact))
    large = np.minimum(exact + (lr.astype(np.float32) * np.float32(32 - exact)).astype(np.int32), 31)
    bk = np.where(n < exact, n, large)
    return [int(np.argmax(bk == b)) for b in range(32)], bk


def phase_setup(ctx, rb_row, TT, TTW, BC):
    nc = ctx.nc
    starts, _ = bucket_starts()
    W = 1408
    with ExitStack() as st:
        sb = lambda n, s, d: st.enter_context(_sbt(nc, n, s, d))
        rbr = sb("s_rbr", [1, 256], F32)
        one1 = sb("s_one1", [1, 128], F32)
        rbB = sb("s_rbB", [128, 32, 8], F32)
        eB = sb("s_eB", [128, 32, 8], F32)
        CB = sb("s_CB", [128, 32, 8], F32)
        dT = sb("s_dT", [128, W], F32)
        dC = sb("s_dC", [128, S], F32)
        mT = sb("s_mT", [128, W], F32)
        mC = sb("s_mC", [128, S], F32)
        aT = sb("s_aT", [128, 8, W], F32)
        aC = sb("s_aC", [128, 8, S], F32)
        aW = sb("s_aW", [128, 8, W], F32)
        r = Rec(ctx)
        ps = ctx.psum
        t0 = r.dma("sp", lambda e: e.dma_start(out=rbr[:], in_=rb_row), [], r.dsem())
        t1 = r.op("pool", lambda e: e.memset(one1[:], 1.0))
        tm = r.op("pe", lambda e: e.matmul(ps[:, 0, 0:256], one1[:], rbr[:], start=True, stop=True), [t0, t1])
        tc = r.op("act", lambda e: e.activation(out=rbB[:].rearrange("p b h -> p (b h)"), in_=ps[:, 0, 0:256],
                                               func=AF.Copy), [tm])
        td = tc
        for b in range(32):
            td = r.op("dve", lambda e, b=b: e.tensor_tensor(out=eB[:, b, :], in0=rbB[:, b, :], in1=rbB[:, 31, :],
                                                           op=ALU.subtract), [tc])
        te = r.op("act", lambda e: e.activation(out=eB[:].rearrange("p b h -> p (b h)"),
                                               in_=eB[:].rearrange("p b h -> p (b h)"), func=AF.Exp), [td])
        tcb = r.op("dve", lambda e: e.tensor_tensor(out=CB[:, 1:32, :], in0=eB[:, 1:32, :], in1=eB[:, 0:31, :],
                                                   op=ALU.subtract), [te])
        tcb = r.op("dve", lambda e: e.tensor_copy(out=CB[:, 0, :], in_=eB[:, 0, :]), [tcb])
        ti1 = r.op("pool", lambda e: e.iota(dT[:], [[1, W]], base=-384, channel_multiplier=-1,
                                           allow_small_or_imprecise_dtypes=True))
        ti2 = r.op("pool", lambda e: e.iota(dC[:], [[1, S]], base=-31, channel_multiplier=-16,
                                           allow_small_or_imprecise_dtypes=True))
        ta = None
        for b in range(32):
            sv = float(starts[b])
            tmk = r.op("dve", lambda e, sv=sv: e.tensor_single_scalar(out=mT[:], in_=dT[:], scalar=sv, op=ALU.is_ge),
                       [ti1, tcb] + ([ta] if ta else []))
            tmk2 = r.op("dve", lambda e, sv=sv: e.tensor_single_scalar(out=mC[:], in_=dC[:], scalar=sv, op=ALU.is_ge),
                        [ti2])
            for h in range(8):
                if b == 0:
                    r.op("dve", lambda e, h=h, b=b: e.tensor_scalar(out=aT[:, h, :], in0=mT[:], scalar1=CB[:, b, h:h + 1],
                                                                   scalar2=None, op0=ALU.mult), [tmk])
                    ta = r.op("dve", lambda e, h=h, b=b: e.tensor_scalar(out=aC[:, h, :], in0=mC[:],
                                                                        scalar1=CB[:, b, h:h + 1], scalar2=None,
                                                                        op0=ALU.mult), [tmk2])
                else:
                    r.op("dve", lambda e, h=h, b=b: e.scalar_tensor_tensor(
                        out=aT[:, h, :], in0=mT[:], scalar=CB[:, b, h:h + 1], in1=aT[:, h, :], op0=ALU.mult,
                        op1=ALU.add), [tmk])
                    ta = r.op("dve", lambda e, h=h, b=b: e.scalar_tensor_tensor(
                        out=aC[:, h, :], in0=mC[:], scalar=CB[:, b, h:h + 1], in1=aC[:, h, :], op0=ALU.mult,
                        op1=ALU.add), [tmk2])
        tw = r.op("dve", lambda e: e.tensor_single_scalar(out=mT[:], in_=dT[:], scalar=512.0, op=ALU.is_lt), [ta])
        for h in range(8):
            tw2 = r.op("dve", lambda e, h=h: e.tensor_tensor(out=aW[:, h, :], in0=aT[:, h, :], in1=mT[:], op=ALU.mult),
                       [tw])
        r.dma("pool", lambda e: e.dma_start(out=TT.rearrange("h p x -> p h x"), in_=aT[:]), [tw2], r.dsem(True))
        r.dma("pool", lambda e: e.dma_start(out=TTW.rearrange("h p x -> p h x"), in_=aW[:]), [tw2], r.dsem(True))
        r.dma("pool", lambda e: e.dma_start(out=BC.rearrange("h p x -> p h x"), in_=aC[:]), [tw2], r.dsem(True))
        r.emit()


QSCALE = float(128.0 ** -0.5)
GC1 = 0.044715
GC2 = 2.0 * float(np.sqrt(2.0 / np.pi))


def phase_nsa(ctx, ZT, YT, cst, par, TT, TTW, BC):
    nc = ctx.nc
    ZQ, ZKC, ZVC, ZKS, ZVS, ZKW, ZVW = 16, 24, 26, 28, 30, 32, 34
    with ExitStack() as st:
        sb = lambda n, s, d: st.enter_context(_sbt(nc, n, s, d))
        ident = sb("a_ident", [128, 128], BF16)
        onesb = sb("a_onesb", [128, 128], BF16)
        expand = sb("a_expand", [32, S], BF16)
        selg = sb("a_selg", [128, 24 * 128], F32)
        force = sb("a_force", [128, 16, 32], F32)
        SG = sb("a_SG", [128, S], F32)
        w1 = sb("a_w1", [128, 2, 32, 128], BF16)
        w2 = sb("a_w2", [128, 2, 128], BF16)
        posT = sb("a_pos", [128, 2, 32], F32)
        xf = sb("a_xf", [128, S], F32)
        xl = sb("a_xl", [128, 32, 127], BF16)
        g1 = sb("a_g1", [128, 128], F32)
        g2 = sb("a_g2", [128, 128], F32)
        gT = sb("a_gT", [128, 128], BF16)
        kcT = sb("a_kcT", [128, 128], BF16)
        VC = sb("a_VC", [128, 161], BF16)
        qT = sb("a_qT", [128, 4, S], BF16)
        ksT = sb("a_ksT", [128, S], BF16)
        kwT = sb("a_kwT", [128, S], BF16)
        vT = sb("a_vT", [128, S], BF16)
        VS = sb("a_VS", [128, 16, 128], BF16)
        VW = sb("a_VW", [128, 16, 128], BF16)
        imp = sb("a_imp", [128, 16, 32], F32)
        m8 = sb("a_m8", [128, 16, 8], F32)
        Mm = sb("a_M", [128, 16, 32], BF16)
        MT = sb("a_MT", [32, S], BF16)
        bc = sb("a_bc", [128, S], BF16)
        tt = sb("a_tt", [128, 1408], BF16)
        ttw = sb("a_ttw", [128, 1408], BF16)
        yacc = sb("a_yacc", [128, 4, S], F32)
        E = sb("a_E", [128, 2, TB], F32)
        P = sb("a_P", [128, 2, TB], BF16)
        e1 = sb("a_e1", [128, TB], F32)
        e2 = sb("a_e2", [128, TB], F32)
        rd = sb("a_rd", [128, 4], F32)
        ys = sb("a_ys", [128, S], BF16)
        r = Rec(ctx)
        ps = ctx.psum
        roles = {}

        def rsem(role, sw=False):
            if role is None:
                return r.dsem(sw)
            if role not in roles:
                roles[role] = r.dsem(sw)
            return roles[role]
        ld = lambda dst, src, deps=(), role=None: r.dma("sp", lambda e: e.dma_start(out=dst, in_=src), list(deps),
                                                       rsem(role))
        ldc = lambda dst, src, deps=(), role=None: r.dma("pool", lambda e: e.dma_start(out=dst, in_=src), list(deps),
                                                        rsem(role, True))
        t_id = ld(ident[:], cst["ident_bf"])
        t_ex = ld(expand[:], cst["expand_bf"])
        t_sg = ld(selg[64:96, :], cst["selg"])
        t_fo = ld(force[:], cst["force"])
        t_on = r.op("pool", lambda e: e.memset(onesb[:], 1.0))
        t_SG = ld(SG[64:96, :], ZT[52 * 128 + 64:52 * 128 + 96, :])
        t_SG = r.op("act", lambda e: e.activation(out=SG[64:96, :], in_=SG[64:96, :], func=AF.Sigmoid), [t_SG])
        t_w1 = [ldc(w1[:, i].rearrange("p l o -> p (l o)"), par["w1"][i]) for i in range(2)]
        t_w2 = [ldc(w2[:, i], par["w2"][i]) for i in range(2)]
        t_pos = ld(posT[:], par["posT"])
        gdone = []
        sring = Ring(r, 2, with_sems=False)
        mring = Ring(r, 2, with_sems=False)
        ering = Ring(r, 2, with_sems=False)
        pring = Ring(r, 2, with_sems=False)
        ep_prev = None
        ep2_prev = None
        tlast_any = None
        for g in range(2):
            t_init = r.op("pool", lambda e: e.memset(kcT[:], 0.0), gdone)
            t_init2 = r.op("pool", lambda e: e.memset(VC[:], 0.0), gdone)
            t_ov = ld(VC[:, 129:161], cst["ov_bf"], [t_init2], "ov")
            t_one = r.op("pool", lambda e: e.memset(VC[0:127, 128:129], 1.0), [t_init2])
            tcmp = []
            for i, zt in enumerate((ZKC + g, ZVC + g)):
                tl = ld(xf[:], ZT[zt * 128:(zt + 1) * 128, :], gdone + tcmp, "xf")
                xv = xf[:].rearrange("p (j i) -> p j i", i=16)
                tx = None
                for l in range(32):
                    j0 = 0 if l < 16 else 1
                    tx = r.op("dve", lambda e, l=l, j0=j0, i=i, xv=xv: e.tensor_scalar(
                        out=xl[:, l, :], in0=xv[:, j0:j0 + 127, l % 16], scalar1=posT[:, i, l:l + 1], scalar2=None,
                        op0=ALU.add), [tl, t_pos] + tcmp)
                b, bdeps = mring.next()
                tm = None
                for l in range(32):
                    tm = r.op("pe", lambda e, l=l, i=i, b=b: e.matmul(
                        ps[:, 2 + b, 0:127], w1[:, i, l, :], xl[:, l, :], start=(l == 0), stop=(l == 31)),
                        [tx, t_w1[i]] + bdeps, inc=(l == 31))
                pre = ps[:, 2 + b, 0:127]
                ta = r.op("act", lambda e, pre=pre: e.activation(out=g1[:, 0:127], in_=pre, func=AF.Square), [tm])
                ta = r.op("dve", lambda e: e.tensor_scalar(out=g1[:, 0:127], in0=g1[:, 0:127], scalar1=GC1,
                                                          scalar2=1.0, op0=ALU.mult, op1=ALU.add), [ta])
                ta = r.op("dve", lambda e, pre=pre: e.tensor_tensor(out=g1[:, 0:127], in0=g1[:, 0:127], in1=pre,
                                                                  op=ALU.mult), [ta])
                ta = r.op("act", lambda e: e.activation(out=g2[:, 0:127], in_=g1[:, 0:127], func=AF.Sigmoid,
                                                       scale=GC2), [ta])
                tg = r.op("dve", lambda e, pre=pre: e.tensor_tensor(out=gT[:, 0:127], in0=g2[:, 0:127], in1=pre,
                                                                  op=ALU.mult), [ta])
                mring.read(b, tg)
                b2, b2deps = mring.next()
                if i == 0:
                    tm2 = r.op("pe", lambda e, b2=b2: e.matmul(ps[:, 2 + b2, 0:127], w2[:, 0, :], gT[:, 0:127],
                                                              start=True, stop=True), [tg, t_w2[0]] + b2deps)
                    tk = r.op("act", lambda e, b2=b2: e.activation(out=kcT[:, 0:127], in_=ps[:, 2 + b2, 0:127],
                                                                  func=AF.Copy), [tm2, t_init])
                else:
                    tm2 = r.op("pe", lambda e, b2=b2: e.matmul(ps[0:127, 2 + b2, 0:128], gT[:, 0:127], w2[:, 1, :],
                                                              start=True, stop=True), [tg, t_w2[1]] + b2deps)
                    tk = r.op("act", lambda e, b2=b2: e.activation(out=VC[0:127, 0:128], in_=ps[0:127, 2 + b2, 0:128],
                                                                  func=AF.Copy), [tm2, t_init2])
                mring.read(b2, tk)
                tcmp = [tk]
            t_cmp = [tk, t_ov, t_one]
            if os.environ.get("NSA_STOP") == "1":
                r.emit()
                return
            t_q = [ldc(qT[:, rr, :], ZT[(ZQ + g * 4 + rr) * 128:(ZQ + g * 4 + rr + 1) * 128, :], gdone, "q%d" % rr)
                   for rr in range(4)]
            t_ks = ldc(ksT[:], ZT[(ZKS + g) * 128:(ZKS + g + 1) * 128, :], gdone, "ks")
            t_kw = ldc(kwT[:], ZT[(ZKW + g) * 128:(ZKW + g + 1) * 128, :], gdone, "kw")
            tV = {}
            tprev = t_cmp
            for nm, zt, dstV in (("s", ZVS + g, VS), ("w", ZVW + g, VW)):
                tv = ldc(vT[:], ZT[zt * 128:(zt + 1) * 128, :], gdone + ([tV["s"]] if nm == "w" else []), "vT")
                tvv = None
                for g4 in range(4):
                    b, bdeps = mring.next()
                    for i in range(4):
                        ttt = g4 * 4 + i
                        tm = r.op("pe", lambda e, ttt=ttt, i=i, b=b: e.matmul(
                            ps[:, 2 + b, i * 128:(i + 1) * 128], vT[:, ttt * 128:(ttt + 1) * 128], ident[:],
                            start=True, stop=True), [tv, t_id] + bdeps, inc=(i == 3))
                    tvv = r.op("act", lambda e, g4=g4, b=b, dstV=dstV: e.activation(
                        out=dstV[:, g4 * 4:(g4 + 1) * 4, :], in_=ps[:, 2 + b, :].rearrange("p (a c) -> p a c", c=128),
                        func=AF.Copy), [tm] + gdone)
                    mring.read(b, tvv)
                tV[nm] = tvv
            if os.environ.get("NSA_STOP") == "2":
                r.emit()
                return
            timp = None
            for rr in range(4):
                hq = g * 4 + rr
                t_bc = ldc(bc[:], BC[hq], [tlast_any] if tlast_any else [], "bc")
                for qb in range(NTB):
                    qsl = slice(qb * TB, (qb + 1) * TB)
                    s_, sdeps = sring.next()
                    tS = r.op("pe", lambda e, rr=rr, qsl=qsl, s_=s_: e.matmul(
                        ps[:, s_, :], kcT[:], qT[:, rr, qsl], start=True, stop=True), t_cmp + [t_q[rr]] + sdeps)
                    ee, edeps = ering.next()
                    tE = r.op("act", lambda e, ee=ee, s_=s_: e.activation(out=E[:, ee, :], in_=ps[:, s_, :],
                                                                        func=AF.Exp, scale=QSCALE), [tS] + edeps)
                    sring.read(s_, tE)
                    pp, pdeps = pring.next()
                    tP = r.op("dve", lambda e, pp=pp, ee=ee, qsl=qsl: e.tensor_tensor(
                        out=P[:, pp, :], in0=E[:, ee, :], in1=bc[:, qsl], op=ALU.mult), [tE, t_bc] + pdeps)
                    ering.read(ee, tP)
                    r.op("pe", lambda e, pp=pp: e.matmul(ps[:, 4, :], VC[:, 0:128], P[:, pp, :], start=True,
                                                        stop=True), [tP] + ([ep_prev] if ep_prev else []), inc=False)
                    r.op("pe", lambda e, pp=pp: e.matmul(ps[:, 5, :], onesb[:], P[:, pp, :], start=True, stop=True),
                         [t_on], inc=False)
                    tI = None
                    for i in range(4):
                        tI = r.op("pe", lambda e, pp=pp, i=i: e.matmul(
                            ps[:, 6, i * 64:i * 64 + 33], P[:, pp, i * 128:(i + 1) * 128], VC[:, 128:161],
                            start=True, stop=True), [ep2_prev] if (i == 0 and ep2_prev) else [], inc=(i == 3))
                    pring.read(pp, tI)
                    mb, mdeps = mring.next()
                    tgm = r.op("pe", lambda e, mb=mb, hq=hq, qsl=qsl: e.matmul(
                        ps[:, 2 + mb, :], selg[64:96, (0 * 8 + hq) * 128:(0 * 8 + hq + 1) * 128], SG[64:96, qsl],
                        start=True, stop=True), [t_SG, t_sg] + mdeps)
                    t1 = r.op("dve", lambda e: e.tensor_scalar_max(out=e1[:], in0=ps[:, 5, :], scalar1=1e-30),
                              [tI, tlast_any])
                    t1 = r.op("dve", lambda e: e.reciprocal(out=e1[:], in_=e1[:]), [t1])
                    t2 = r.op("dve", lambda e: e.tensor_tensor(out=e2[:], in0=ps[:, 4, :], in1=e1[:], op=ALU.mult),
                              [t1])
                    ep_prev = t2
                    t3 = r.op("dve", lambda e, rr=rr, qsl=qsl, mb=mb: e.tensor_tensor(
                        out=yacc[:, rr, qsl], in0=e2[:], in1=ps[:, 2 + mb, :], op=ALU.mult), [t2, tgm] + gdone)
                    mring.read(mb, t3)
                    t4 = r.op("dve", lambda e: e.tensor_scalar_max(
                        out=rd[:], in0=ps[:, 6, 0:256].rearrange("p (i c) -> p i c", c=64)[:, :, 0], scalar1=1e-30),
                        [tI, t3])
                    t4 = r.op("dve", lambda e: e.reciprocal(out=rd[:], in_=rd[:]), [t4])
                    for i in range(4):
                        qt = qb * 4 + i
                        if rr == 0:
                            timp = r.op("dve", lambda e, i=i, qt=qt: e.tensor_scalar(
                                out=imp[:, qt, :], in0=ps[:, 6, i * 64 + 1:i * 64 + 33], scalar1=rd[:, i:i + 1],
                                scalar2=None, op0=ALU.mult), [t4] + gdone)
                        else:
                            timp = r.op("dve", lambda e, i=i, qt=qt: e.scalar_tensor_tensor(
                                out=imp[:, qt, :], in0=ps[:, 6, i * 64 + 1:i * 64 + 33], scalar=rd[:, i:i + 1],
                                in1=imp[:, qt, :], op0=ALU.mult, op1=ALU.add), [t4])
                    ep2_prev = timp
                    tlast_any = timp
            if os.environ.get("NSA_STOP") == "3":
                r.emit()
                return
            tk_ = r.op("dve", lambda e: e.tensor_tensor(out=imp[:], in0=imp[:], in1=force[:], op=ALU.add),
                       [timp, t_fo])
            for qt in range(16):
                tk1 = r.op("dve", lambda e, qt=qt: e.max(out=m8[:, qt, :], in_=imp[:, qt, :]), [tk_])
                tk2 = r.op("dve", lambda e, qt=qt: e.tensor_scalar(
                    out=Mm[:, qt, :], in0=imp[:, qt, :], scalar1=m8[:, qt, 7:8], scalar2=None, op0=ALU.is_ge),
                    [tk1] + gdone)
            tMT = None
            for g4 in range(4):
                b, bdeps = mring.next()
                for i in range(4):
                    qt = g4 * 4 + i
                    tm = r.op("pe", lambda e, qt=qt, i=i, b=b: e.matmul(
                        ps[0:32, 2 + b, i * 128:(i + 1) * 128], Mm[:, qt, :], ident[:], start=True, stop=True),
                        [tk2, t_id] + bdeps, inc=(i == 3))
                tMT = r.op("act", lambda e, g4=g4, b=b: e.activation(
                    out=MT[:, g4 * TB:(g4 + 1) * TB], in_=ps[0:32, 2 + b, :], func=AF.Copy), [tm] + gdone)
                mring.read(b, tMT)
            if os.environ.get("NSA_STOP") == "4":
                r.emit()
                return
            for rr in range(4):
                hq = g * 4 + rr
                t_tt = ldc(tt[:], TT[hq], [tlast_any], "tt")
                t_tw = ldc(ttw[:], TTW[hq], [tlast_any], "ttw")
                for qb in range(NTB):
                    qsl = slice(qb * TB, (qb + 1) * TB)
                    for br in (2, 1):
                        win = (br == 2)
                        kts = list(range(max(0, 4 * qb - 4), 4 * qb + 4)) if win else list(range(0, 4 * qb + 4))
                        Kt = kwT if win else ksT
                        Vt = VW if win else VS
                        tkk = t_kw if win else t_ks
                        ob = 4 if win else 6
                        tpv = None
                        for n_, kt in enumerate(kts):
                            c = 512 * qb - 128 * kt
                            s_, sdeps = sring.next()
                            tS = r.op("pe", lambda e, kt=kt, rr=rr, qsl=qsl, s_=s_, Kt=Kt: e.matmul(
                                ps[:, s_, :], Kt[:, kt * 128:(kt + 1) * 128], qT[:, rr, qsl], start=True, stop=True),
                                [tkk, t_q[rr], tV["w"]] + sdeps)
                            if not win:
                                mb, mdeps = mring.next()
                                tM = r.op("pe", lambda e, kt=kt, qsl=qsl, mb=mb: e.matmul(
                                    ps[:, 2 + mb, :], expand[:, kt * 128:(kt + 1) * 128], MT[:, qsl], start=True,
                                    stop=True), [tMT, t_ex] + mdeps)
                            ee, edeps = ering.next()
                            tE = r.op("act", lambda e, ee=ee, s_=s_: e.activation(
                                out=E[:, ee, :], in_=ps[:, s_, :], func=AF.Exp, scale=QSCALE), [tS] + edeps)
                            sring.read(s_, tE)
                            pp, pdeps = pring.next()
                            if win:
                                tP = r.op("dve", lambda e, pp=pp, ee=ee, c=c: e.tensor_tensor(
                                    out=P[:, pp, :], in0=E[:, ee, :], in1=ttw[:, c + 384:c + 384 + TB], op=ALU.mult),
                                    [tE, t_tw] + pdeps)
                            else:
                                if c < 256:
                                    tE = r.op("pool", lambda e, ee=ee, c=c: e.tensor_tensor(
                                        out=E[:, ee, :], in0=E[:, ee, :], in1=tt[:, c + 384:c + 384 + TB],
                                        op=ALU.mult), [tE, t_tt])
                                tP = r.op("dve", lambda e, pp=pp, ee=ee, mb=mb: e.tensor_tensor(
                                    out=P[:, pp, :], in0=E[:, ee, :], in1=ps[:, 2 + mb, :], op=ALU.mult),
                                    [tE, tM] + pdeps)
                                mring.read(mb, tP)
                            ering.read(ee, tP)
                            first = (n_ == 0)
                            lastk = (n_ == len(kts) - 1)
                            prevdep = (ep_prev if win else ep2_prev)
                            r.op("pe", lambda e, pp=pp, kt=kt, first=first, lastk=lastk, Vt=Vt, ob=ob: e.matmul(
                                ps[:, ob, :], Vt[:, kt, :], P[:, pp, :], start=first, stop=lastk),
                                [tP] + ([prevdep] if first and prevdep else []), inc=False)
                            tpv = r.op("pe", lambda e, pp=pp, first=first, lastk=lastk, ob=ob: e.matmul(
                                ps[:, ob + 1, :], onesb[:], P[:, pp, :], start=first, stop=lastk), [t_on])
                            pring.read(pp, tpv)
                        mb, mdeps = mring.next()
                        tgm = r.op("pe", lambda e, mb=mb, hq=hq, qsl=qsl, br=br: e.matmul(
                            ps[:, 2 + mb, :], selg[64:96, (br * 8 + hq) * 128:(br * 8 + hq + 1) * 128],
                            SG[64:96, qsl], start=True, stop=True), [t_SG, t_sg] + mdeps)
                        t1 = r.op("dve", lambda e, ob=ob: e.reciprocal(out=e1[:], in_=ps[:, ob + 1, :]),
                                  [tpv, tlast_any])
                        t2 = r.op("dve", lambda e, ob=ob: e.tensor_tensor(out=e2[:], in0=ps[:, ob, :], in1=e1[:],
                                                                        op=ALU.mult), [t1])
                        if win:
                            ep_prev = t2
                        else:
                            ep2_prev = t2
                        t3 = r.op("dve", lambda e, mb=mb: e.tensor_tensor(out=e2[:], in0=e2[:], in1=ps[:, 2 + mb, :],
                                                                        op=ALU.mult), [t2, tgm])
                        mring.read(mb, t3)
                        tlast_any = r.op("dve", lambda e, rr=rr, qsl=qsl: e.tensor_tensor(
                            out=yacc[:, rr, qsl], in0=yacc[:, rr, qsl], in1=e2[:], op=ALU.add), [t3])
                tys = r.op("act", lambda e, rr=rr: e.activation(out=ys[:], in_=yacc[:, rr, :], func=AF.Copy),
                           [tlast_any] + gdone)
                tst = r.dma("sp", lambda e, hq=hq: e.dma_start(out=YT[512 + hq * 128:512 + (hq + 1) * 128, :],
                                                              in_=ys[:]), [tys], rsem("st"))
                gdone = [tst, tys]
            gdone = gdone + [tlast_any]
        r.emit()


def phase_merge(ctx, GL, YT, MTo, wgu, wbr, bg_cols):
    nc = ctx.nc
    ybase = [0, 4, 12]
    ykc = [4, 8, 4]
    with ExitStack() as st:
        sb = lambda n, s, d: st.enter_context(_sbt(nc, n, s, d))
        gl = sb("m_gl", [128, 8, S], BF16)
        yt = sb("m_yt", [128, 16, S], BF16)
        wg = sb("m_wg", [128, 2, 3, 8 * 128], BF16)
        wb = sb("m_wb", [128, 2, 16 * 128], BF16)
        bg = sb("m_bg", [128, 96], F32)
        sg = sb("m_sg", [128, 2, TB], F32)
        acc = sb("m_acc", [128, TB], F32)
        tmp = sb("m_tmp", [128, TB], F32)
        ob = sb("m_ob", [128, 2, S], BF16)
        r = Rec(ctx)
        ps = ctx.psum
        t_bg = r.dma("sp", lambda e: e.dma_start(out=bg[:], in_=bg_cols), [], r.dsem())
        t_gl = r.dma("sp", lambda e: e.dma_start(out=gl[:], in_=GL.rearrange("(c p) t -> p c t", p=128)), [], r.dsem())
        t_yt = [r.dma("sp", lambda e, i=i: e.dma_start(
            out=yt[:, i * 8:(i + 1) * 8, :], in_=YT.rearrange("(c p) t -> p c t", p=128)[:, i * 8:(i + 1) * 8, :]), [],
            r.dsem()) for i in range(2)]
        wring = Ring(r, 2, sw=True)
        wsem2 = [r.dsem(True), r.dsem(True)]
        gring = Ring(r, 2, with_sems=False)
        bring = Ring(r, 2, with_sems=False)
        sring = Ring(r, 2, with_sems=False)
        oring = Ring(r, 2)
        tacc = None
        for mt in range(KC):
            ws, wdeps = wring.next()
            tw1 = r.dma("pool", lambda e, ws=ws, mt=mt: e.dma_start(
                out=wg[:, ws], in_=wgu.rearrange("(b m) p k -> m p b k", b=3)[mt]), wdeps, wring.sems[ws])
            tw2s = []
            off = 0
            for b in range(3):
                n = ykc[b] * 128
                tw2s.append(r.dma("pool", lambda e, ws=ws, mt=mt, b=b, off=off, n=n: e.dma_start(
                    out=wb[:, ws, off:off + n], in_=wbr[b][mt]), wdeps, wsem2[ws]))
                off += n
            o, odeps = oring.next()
            tlastmm = None
            for tb in range(NTB):
                tsl = slice(tb * TB, (tb + 1) * TB)
                for b in range(3):
                    gb, gdeps = gring.next()
                    tmg = None
                    for c in range(8):
                        tmg = r.op("pe", lambda e, ws=ws, b=b, c=c, gb=gb, tsl=tsl: e.matmul(
                            ps[:, gb, :], wg[:, ws, b, c * 128:(c + 1) * 128], gl[:, c, tsl], start=(c == 0),
                            stop=(c == 7)), [tw1, t_gl] + gdeps, inc=(c == 7))
                    bb, bdeps = bring.next()
                    boff = sum(ykc[:b]) * 128
                    tmb = None
                    for c in range(ykc[b]):
                        tmb = r.op("pe", lambda e, ws=ws, b=b, c=c, bb=bb, tsl=tsl, boff=boff: e.matmul(
                            ps[:, 2 + bb, :], wb[:, ws, boff + c * 128:boff + (c + 1) * 128],
                            yt[:, ybase[b] + c, tsl], start=(c == 0), stop=(c == ykc[b] - 1)),
                            tw2s + t_yt + bdeps, inc=(c == ykc[b] - 1))
                    tlastmm = tmb
                    s_, sdeps = sring.next()
                    tsg = r.op("act", lambda e, s_=s_, gb=gb, b=b, mt=mt: e.activation(
                        out=sg[:, s_, :], in_=ps[:, gb, :], func=AF.Sigmoid,
                        bias=bg[:, b * 32 + mt:b * 32 + mt + 1], scale=1.0), [tmg, t_bg] + sdeps)
                    gring.read(gb, tsg)
                    if b == 0:
                        tacc = r.op("dve", lambda e, s_=s_, bb=bb: e.tensor_tensor(
                            out=acc[:], in0=sg[:, s_, :], in1=ps[:, 2 + bb, :], op=ALU.mult), [tsg, tmb, tacc])
                    elif b == 1:
                        t_ = r.op("dve", lambda e, s_=s_, bb=bb: e.tensor_tensor(
                            out=tmp[:], in0=sg[:, s_, :], in1=ps[:, 2 + bb, :], op=ALU.mult), [tsg, tmb, tacc])
                        tacc = r.op("dve", lambda e: e.tensor_tensor(out=acc[:], in0=acc[:], in1=tmp[:], op=ALU.add),
                                    [t_])
                    else:
                        t_ = r.op("dve", lambda e, s_=s_, bb=bb: e.tensor_tensor(
                            out=tmp[:], in0=sg[:, s_, :], in1=ps[:, 2 + bb, :], op=ALU.mult), [tsg, tmb, tacc])
                        tacc = r.op("dve", lambda e, o=o, tsl=tsl: e.tensor_tensor(
                            out=ob[:, o, tsl], in0=acc[:], in1=tmp[:], op=ALU.add), [t_] + odeps)
                    sring.read(s_, tacc if b == 0 else t_)
                    bring.read(bb, tacc if b == 0 else t_)
            wring.read(ws, tlastmm)
            ts = r.dma("sp", lambda e, o=o, mt=mt: e.dma_start(out=MTo[mt * 128:(mt + 1) * 128, :], in_=ob[:, o, :]),
                       [tacc], oring.sems[o])
            oring.read(o, ts)
        r.emit()


CONST_SPECS = None


class LazyInputs:
    def __init__(self, nc, L, consts):
        import ml_dtypes
        self.nc = nc
        self.decl = {}
        sp = {}
        sp["xT"] = ([D, S], F32)
        sp["rb"] = ([1, 256], F32)
        for k, v in consts.items():
            sp["c_" + k] = (list(v.shape), BF16 if v.dtype == ml_dtypes.bfloat16 else F32)
        for n, shp in [("g1c", [L, 128, KC]), ("g2c", [L, 128, KC]), ("gfc", [128, KC]),
                       ("win_t", [L, 53, 128, D]), ("bin_c", [L, 128, 53]), ("wgd_t", [L, 8, 128, D]),
                       ("conv_c", [L, 128, 32]), ("mgain", [L, 128, 4]), ("rgain", [L, 128, 4]),
                       ("w1", [L, 2, 128, 4096]), ("w2", [L, 2, 128, 128]), ("posT", [L, 128, 2, 32]),
                       ("wgu_t", [L, 96, 128, 1024]), ("bg_c", [L, 128, 96]),
                       ("wbrm_t", [L, 32, 128, 512]), ("wbrn_t", [L, 32, 128, 1024]), ("wbrr_t", [L, 32, 128, 512]),
                       ("wout_t", [L, 32, 128, D]), ("wup_t", [L, 128, 128, D]), ("wdn", [L, 4 * D, D])]:
            sp[n] = (shp, F32)
        self.sp = sp

    def __getitem__(self, name):
        if name not in self.decl:
            shp, dt = self.sp[name]
            self.decl[name] = self.nc.dram_tensor(name, list(shp), dt, kind="ExternalInput").ap()
        return self.decl[name]


def declare_inputs(nc, L, consts):
    return LazyInputs(nc, L, consts)


def build_program(L, consts, debug=False, upto=99):
    nc = bass.Bass("TRN2", target_bir_lowering=False)
    I = declare_inputs(nc, L, consts)
    yT = nc.dram_tensor("yT", [D, S], F32, kind="ExternalOutput").ap()
    kind = "ExternalOutput" if debug else "Internal"
    XA = nc.dram_tensor("XA", [D, S], F32, kind=kind).ap()
    XB = nc.dram_tensor("XB", [D, S], F32, kind=kind).ap()
    HT = nc.dram_tensor("HT", [D, S], BF16).ap()
    ZT = nc.dram_tensor("ZT", [ZROWS, S], F32).ap()
    GL = nc.dram_tensor("GL", [1024, S], BF16).ap()
    YT = nc.dram_tensor("YT", [2048, S], BF16, kind=kind).ap()
    MT = nc.dram_tensor("MT", [D, S], BF16).ap()
    TT = nc.dram_tensor("TT", [8, 128, 1408], BF16).ap()
    TTW = nc.dram_tensor("TTW", [8, 128, 1408], BF16).ap()
    BC = nc.dram_tensor("BC", [8, 128, S], BF16).ap()
    class _C(dict):
        def __missing__(self, k):
            return I["c_" + k]
    cst = _C()
    ctx = Ctx(nc)
    with nc.psum_tensor("ps", [128, 8, 512], F32) as ps:
        ctx.psum = ps
        step = [0]

        def go():
            step[0] += 1
            return step[0] <= upto
        if go():
            phase_setup(ctx, I["rb"], TT, TTW, BC)
        xcur = I["xT"]
        for l in range(L):
            if go() and not os.environ.get("SKIP2"):
                phase_norm(ctx, xcur, HT, I["g1c"][l])
            if go() and not os.environ.get("SKIP3"):
                jobs = [dict(w=I["win_t"][l, m], kind="z", bias=m, out=ZT[m * 128:(m + 1) * 128, :]) for m in range(53)]
                jobs += [dict(w=I["wgd_t"][l, m], kind="bf", out=GL[m * 128:(m + 1) * 128, :]) for m in range(8)]
                phase_linear(ctx, HT, D, jobs, bias_cols=I["bin_c"][l], nbias=53)
            if go() and not os.environ.get("SKIP4"):
                phase_decay(ctx, "mlstm", ZT, YT, 0, 4, 8, 12, 0, cst, dict(gain=I["mgain"][l], conv_cols=I["conv_c"][l]))
            if go() and not os.environ.get("SKIP5"):
                phase_decay(ctx, "ret", ZT, YT, 36, 40, 44, 48, 1536, cst, dict(gain=I["rgain"][l]))
            if go():
                phase_nsa(ctx, ZT, YT, cst, dict(w1=I["w1"][l], w2=I["w2"][l], posT=I["posT"][l]), TT, TTW, BC)
            if go():
                phase_merge(ctx, GL, YT, MT, I["wgu_t"][l], [I["wbrm_t"][l], I["wbrn_t"][l], I["wbrr_t"][l]], I["bg_c"][l])
            if go():
                jobs = [dict(w=I["wout_t"][l, m], kind="res", resid=xcur[m * 128:(m + 1) * 128, :],
                             out=XA[m * 128:(m + 1) * 128, :]) for m in range(KC)]
                phase_linear(ctx, MT, D, jobs)
            if go():
                phase_norm(ctx, XA, HT, I["g2c"][l])
            if go():
                phase_mlp(ctx, HT, XA, XB, I["wup_t"][l], I["wdn"][l])
            xcur = XB
        if go():
            phase_norm(ctx, xcur, yT, I["gfc"], out_f32=True)
    nc._lazy_inputs = I
    return nc


def prep_weights(inp, L, layers=None):
    layers = list(range(L)) if layers is None else layers
    cm, _ = in_colmap()
    W = {}
    W["rb"] = np.ascontiguousarray(inp["rel_bias"].reshape(1, 256))
    W["g1c"] = np.stack([cols_layout(inp["norm_mix_g"][l]) for l in layers])
    W["g2c"] = np.stack([cols_layout(inp["norm_mlp_g"][l]) for l in layers])
    W["gfc"] = cols_layout(inp["final_norm_g"])
    W["win_t"] = np.stack([tile_w(permute_cols(inp["w_in"][l], cm)) for l in layers])
    W["bin_c"] = np.stack([cols_layout(permute_cols(inp["b_in"][l], cm)) for l in layers])
    W["wgd_t"] = np.stack([tile_w(inp["w_gate_down"][l]) for l in layers])
    W["conv_c"] = np.stack([np.ascontiguousarray(inp["conv_qk"][l].reshape(4, 8, 128).transpose(2, 0, 1).reshape(128, 32))
                            for l in layers])
    W["mgain"] = np.stack([cols_layout(inp["mlstm_norm_g"][l]) for l in layers])
    W["rgain"] = np.stack([cols_layout(inp["ret_norm_g"][l]) for l in layers])
    W["w1"] = np.stack([np.stack([np.ascontiguousarray(inp[k][l].reshape(32, 128, 128).transpose(1, 0, 2)).reshape(128, 4096)
                                  for k in ("cmp_w1_k", "cmp_w1_v")]) for l in layers])
    W["w2"] = np.stack([np.stack([inp["cmp_w2_k"][l], inp["cmp_w2_v"][l]]) for l in layers])
    W["posT"] = np.stack([np.ascontiguousarray(np.stack([inp["cmp_pos_k"][l].T, inp["cmp_pos_v"][l].T], 1))
                          for l in layers])
    W["wgu_t"] = np.stack([tile_w(inp["w_gate_up"][l]) for l in layers])
    W["bg_c"] = np.stack([cols_layout(inp["b_gate"][l]) for l in layers])
    W["wbrm_t"] = np.stack([tile_w(inp["w_br_mlstm"][l]) for l in layers])
    W["wbrn_t"] = np.stack([tile_w(inp["w_br_nsa"][l]) for l in layers])
    W["wbrr_t"] = np.stack([tile_w(inp["w_br_ret"][l]) for l in layers])
    W["wout_t"] = np.stack([tile_w(inp["w_out"][l]) for l in layers])
    W["wup_t"] = np.stack([tile_w(inp["w_up"][l]) for l in layers])
    W["wdn"] = np.stack([np.ascontiguousarray(inp["w_down"][l]) for l in layers])
    return W


import ml_dtypes
def make_consts():
    c={}
    sel=np.zeros((64,4*128),np.float32)
    for h in range(4): sel[32+h,h*128:(h+1)*128]=1.0
    c["sel64"]=sel
    c["i64"]=np.eye(64,dtype=np.float32)
    c["ident_bf"]=np.eye(128,dtype=np.float32).astype(ml_dtypes.bfloat16)
    ik=np.arange(128)[:,None]; x=np.arange(896)[None,:]
    c["U_bf"]=((x-384-ik)>=0).astype(np.float32).astype(ml_dtypes.bfloat16)
    G=np.zeros((64,S),np.float32)
    lg=np.log1p(-np.exp2(-5.0-np.arange(4,dtype=np.float32))).astype(np.float32)
    G[32:36,:]=lg[:,None]*np.arange(S,dtype=np.float32)[None,:]
    c["ret_G"]=G
    half=64
    inv_freq=(1.0/(10000.0**np.linspace(0.0,1.0,half,dtype=np.float32))).astype(np.float32)
    ang=np.arange(S,dtype=np.float32)[:,None]*inv_freq[None,:]
    cos=np.cos(ang).astype(np.float32).T; sin=np.sin(ang).astype(np.float32).T
    c["cosT"]=np.ascontiguousarray(np.concatenate([cos,cos],0)); c["sinT"]=np.ascontiguousarray(np.concatenate([sin,sin],0))
    R=np.zeros((128,128),np.float32)
    for m in range(64): R[m+64,m]=-1.0
    for m in range(64,128): R[m-64,m]=1.0
    c["rotT"]=R
    return c
def make_consts_nsa(c):
    k=np.arange(S)[None,:]; s=np.arange(32)[:,None]
    c["expand_bf"]=((k//64)==s).astype(np.float32).astype(ml_dtypes.bfloat16)
    t=(np.arange(16)[None,:,None]*128+np.arange(128)[:,None,None]); cur=t//64; sb=np.arange(32)[None,None,:]
    F=np.zeros((128,16,32),np.float32)
    F[sb>cur]=-1e4
    F[(sb==0)|(sb==cur)|(sb==cur-1)]=1e4
    c["force"]=F
    j=np.arange(128)[:,None]; ss=np.arange(32)[None,:]
    ov=((16*j<64*ss+64)&(16*j+32>64*ss)&(j<=126)).astype(np.float32)
    c["ov_bf"]=ov.astype(ml_dtypes.bfloat16)
    sg=np.zeros((32,24*128),np.float32)
    for i in range(24): sg[i,i*128:(i+1)*128]=1.0
    c["selg"]=sg
    return c


N_CORES = 8
DEPTH = 4


def kernel(**inputs):
    consts = make_consts_nsa(make_consts())
    nc = build_program(DEPTH, consts)
    W = prep_weights(inputs, DEPTH)
    x = np.asarray(inputs["x"], dtype=np.float32)
    decl = nc._lazy_inputs.decl
    base = dict(W)
    for k, v in consts.items():
        base["c_" + k] = v
    in_maps = []
    for b in range(N_CORES):
        m = {k: v for k, v in base.items() if k in decl}
        m["xT"] = np.ascontiguousarray(x[b].T)
        in_maps.append(m)
    res = run_bass_kernel_spmd(nc, in_maps, core_ids=list(range(N_CORES)))
    out = np.stack([np.ascontiguousarray(np.asarray(res.results[b]["yT"]).T) for b in range(N_CORES)])
    return out.astype(np.float32)
```

```python
import numpy as np
import os
from contextlib import ExitStack
import concourse.bass as bass
import concourse.mybir as mybir
from concourse.bass_utils import run_bass_kernel_spmd

F32 = mybir.dt.float32
BF16 = mybir.dt.bfloat16
AF = mybir.ActivationFunctionType
ALU = mybir.AluOpType
AX = mybir.AxisListType

S = 2048
D = 4096
KC = D // 128
TB = 512
NTB = S // TB
EPS = 1e-6
ZROWS = 53 * 128


ENGS = ["pe", "act", "dve", "pool", "sp"]
_UID = [0]


def _sbt(nc, name, shape, dt):
    _UID[0] += 1
    return nc.sbuf_tensor(f"{name}_{_UID[0]}", shape, dt)


class Ctx:
    def __init__(self, nc, n_hw=52, n_sw=40):
        self.nc = nc
        self.esem = {e: nc.alloc_semaphore(name=f"eng_{e}") for e in ENGS[:4]}
        self.hw = [nc.alloc_semaphore(name=f"hw_{i}") for i in range(n_hw)]
        self.sw = [nc.alloc_semaphore(name=f"sw_{i}") for i in range(n_sw)]
        self.base = {}
        self.psum = None


class Rec:
    def __init__(self, ctx):
        self.ctx = ctx
        self.q = {e: [] for e in ENGS}
        self.cnt = {e: 0 for e in ENGS[:4]}
        self.dcnt = {}
        self.nsem = {"hw": 0, "sw": 0}
        self.pending = None

    def dsem(self, sw=False):
        kind = "sw" if sw else "hw"
        idx = self.nsem[kind]
        self.nsem[kind] += 1
        pool = self.ctx.sw if sw else self.ctx.hw
        assert idx < len(pool), f"out of {kind} dma sems"
        k = (kind, idx)
        self.dcnt[k] = self.ctx.base.get(k, 0)
        return k

    def op(self, eng, fn, deps=(), inc=True):
        deps = tuple(d for d in deps if d is not None)
        self.q[eng].append(("op", fn, deps, inc))
        if inc:
            self.cnt[eng] += 1
            return (eng, self.cnt[eng])
        return None

    def dma(self, eng, fn, deps, semkey):
        deps = tuple(d for d in deps if d is not None)
        assert (semkey[0] == "sw") == (eng == "pool"), (eng, semkey)
        self.q[eng].append(("dma", fn, deps, semkey))
        self.dcnt[semkey] += 16
        return (semkey, self.dcnt[semkey])

    def wait(self, eng, deps):
        deps = tuple(d for d in deps if d is not None)
        self.q[eng].append(("wait", None, deps, None))

    def _sem(self, k):
        if isinstance(k, tuple):
            return (self.ctx.sw if k[0] == "sw" else self.ctx.hw)[k[1]]
        return self.ctx.esem[k]

    def check(self):
        pos = {e: 0 for e in ENGS}
        val = {k: self.ctx.base.get(k, 0) for k in self.dcnt}
        progress = True
        while progress:
            progress = False
            for e in ENGS:
                q = self.q[e]
                while pos[e] < len(q):
                    kind, fn, deps, x = q[pos[e]]
                    if any(val.get(k, 0) < v for (k, v) in deps):
                        break
                    if kind == "op" and x:
                        val[e] = val.get(e, 0) + 1
                    elif kind == "dma":
                        val[x] = val.get(x, 0) + 16
                    pos[e] += 1
                    progress = True
        stuck = {e: (pos[e], len(self.q[e])) for e in ENGS if pos[e] < len(self.q[e])}
        if stuck:
            msg = []
            for e, (p, n) in stuck.items():
                kind, fn, deps, x = self.q[e][p]
                msg.append(f"{e}@{p}/{n} waits {[(k, v, val.get(k, 0)) for k, v in deps if val.get(k, 0) < v]}")
            raise RuntimeError("DEADLOCK in recorded schedule: " + "; ".join(msg))

    def emit(self):
        nc = self.ctx.nc
        self.check()
        self.wait("sp", [(k, v) for k, v in self.dcnt.items() if v > self.ctx.base.get(k, 0)])
        with nc.Block() as block:
            for name in ENGS:
                items = self.q[name]
                if not items:
                    continue

                def run(e, items=items, name=name):
                    known = {}
                    for kind, fn, deps, x in items:
                        for (k, v) in deps:
                            if known.get(k, 0) < v:
                                e.wait_ge(self._sem(k), v)
                                known[k] = v
                        if kind == "wait":
                            continue
                        ins = fn(e)
                        if kind == "op":
                            if x:
                                ins.then_inc(self._sem(name), 1)
                        else:
                            ins.then_inc(self._sem(x), 16)

                {"pe": block.tensor, "act": block.scalar, "dve": block.vector,
                 "pool": block.gpsimd, "sp": block.sync}[name](run)
        used = [self.ctx.esem[e] for e in ENGS[:4] if self.cnt[e] > 0]
        for k, v in self.dcnt.items():
            self.ctx.base[k] = v
        if used:
            nc.all_engine_barrier()
            with nc.Block() as block:
                def clr(e):
                    for s in used:
                        e.sem_clear(s)
                block.gpsimd(clr)
            nc.all_engine_barrier()


class Ring:
    def __init__(self, rec, n, with_sems=True, sw=False):
        self.n = n
        self.sems = [rec.dsem(sw) for _ in range(n)] if with_sems else None
        self.readers = [[] for _ in range(n)]
        self.i = 0

    def next(self):
        s = self.i % self.n
        self.i += 1
        deps = self.readers[s]
        self.readers[s] = []
        return s, deps

    def read(self, s, ticket):
        if ticket is not None:
            self.readers[s].append(ticket)


def phase_norm(ctx, xT, hT, g_cols, out_f32=False):
    nc = ctx.nc
    CG = 8
    NG = KC // CG
    odt = F32 if out_f32 else BF16
    with (_sbt(nc, "n_x", [128, 3, CG, TB], F32) as xb,
          _sbt(nc, "n_sq", [128, 2, CG, TB], BF16) as sq,
          _sbt(nc, "n_o", [128, 2, CG, TB], odt) as ob,
          _sbt(nc, "n_rstd", [128, S], F32) as rstd,
          _sbt(nc, "n_g", [128, KC], F32) as gc,
          _sbt(nc, "n_ones", [128, 128], BF16) as ones):
        r = Rec(ctx)
        ps = ctx.psum
        csem = r.dsem()
        tg = r.dma("sp", lambda e: e.dma_start(out=gc[:], in_=g_cols), [], csem)
        tones = r.op("pool", lambda e: e.memset(ones[:], 1.0))
        xring = Ring(r, 3)
        sqring = Ring(r, 2, with_sems=False)
        xv = xT.rearrange("(c p) t -> p c t", p=128)
        hv = hT.rearrange("(c p) t -> p c t", p=128)
        mm_last = [None] * NTB
        for tb in range(NTB):
            for g in range(NG):
                s, deps = xring.next()
                tl = r.dma("sp", lambda e, s=s, g=g, tb=tb: e.dma_start(
                    out=xb[:, s], in_=xv[:, g * CG:(g + 1) * CG, tb * TB:(tb + 1) * TB]), deps, xring.sems[s])
                q, qdeps = sqring.next()
                ta = r.op("act", lambda e, s=s, q=q: e.activation(out=sq[:, q], in_=xb[:, s], func=AF.Square),
                          [tl] + qdeps)
                xring.read(s, ta)
                for c in range(CG):
                    last = (c == CG - 1)
                    tm = r.op("pe", lambda e, q=q, c=c, tb=tb, g=g: e.matmul(
                        ps[:, tb, :], ones[:], sq[:, q, c, :], start=(g == 0 and c == 0),
                        stop=(g == NG - 1 and c == CG - 1)), [ta, tones] if c == 0 else [], inc=last)
                sqring.read(q, tm)
                mm_last[tb] = tm
        trs = []
        for tb in range(NTB):
            t1 = r.op("act", lambda e, tb=tb: e.activation(out=rstd[:, tb * TB:(tb + 1) * TB], in_=ps[:, tb, :],
                                                          func=AF.Sqrt, bias=EPS, scale=1.0 / D), [mm_last[tb]])
            t2 = r.op("dve", lambda e, tb=tb: e.reciprocal(out=rstd[:, tb * TB:(tb + 1) * TB],
                                                                      in_=rstd[:, tb * TB:(tb + 1) * TB]), [t1])
            trs.append(t2)
        oring = Ring(r, 2)
        for tb in range(NTB):
            for g in range(NG):
                s, deps = xring.next()
                tl = r.dma("sp", lambda e, s=s, g=g, tb=tb: e.dma_start(
                    out=xb[:, s], in_=xv[:, g * CG:(g + 1) * CG, tb * TB:(tb + 1) * TB]), deps, xring.sems[s])
                o, odeps = oring.next()
                tv = None
                for c in range(CG):
                    cc = g * CG + c
                    tv = r.op("dve", lambda e, s=s, o=o, c=c, cc=cc, tb=tb: e.scalar_tensor_tensor(
                        out=ob[:, o, c, :], in0=xb[:, s, c, :], scalar=gc[:, cc:cc + 1],
                        in1=rstd[:, tb * TB:(tb + 1) * TB], op0=ALU.mult, op1=ALU.mult),
                        [tl, tg, trs[tb]] + odeps)
                xring.read(s, tv)
                ts = r.dma("sp", lambda e, o=o, g=g, tb=tb: e.dma_start(
                    out=hv[:, g * CG:(g + 1) * CG, tb * TB:(tb + 1) * TB], in_=ob[:, o]), [tv], oring.sems[o])
                oring.read(o, ts)
        r.emit()


_EPS = {}


def EPS_AP(ctx):
    return _EPS["ap"]


def phase_linear(ctx, inT, K, jobs, bias_cols=None, nbias=0):
    nc = ctx.nc
    KCk = K // 128
    with (_sbt(nc, "l_in", [128, KCk, S], BF16) as xin,
          _sbt(nc, "l_w", [128, 3, KCk * 128], BF16) as wb,
          _sbt(nc, "l_sf", [128, 2, S], F32) as sf,
          _sbt(nc, "l_sb", [128, 2, S], BF16) as sb,
          _sbt(nc, "l_bias", [128, max(nbias, 1)], F32) as bc):
        r = Rec(ctx)
        ps = ctx.psum
        csem = r.dsem()
        tb_ = None
        if nbias:
            tb_ = r.dma("sp", lambda e: e.dma_start(out=bc[:], in_=bias_cols), [], csem)
        iv = inT.rearrange("(c p) t -> p c t", p=128)
        tin = []
        CG = 8 if KCk >= 8 else KCk
        for g in range(KCk // CG):
            k = r.dsem()
            tin.append(r.dma("sp", lambda e, g=g: e.dma_start(out=xin[:, g * CG:(g + 1) * CG, :],
                                                             in_=iv[:, g * CG:(g + 1) * CG, :]), [], k))
        wring = Ring(r, 3, sw=True)
        pring = Ring(r, 2, with_sems=False)
        fring = Ring(r, 2)
        bring = Ring(r, 2)
        rsem = [r.dsem(), r.dsem()]
        for m, job in enumerate(jobs):
            ws, wdeps = wring.next()
            tw = r.dma("pool", lambda e, ws=ws, job=job: e.dma_start(out=wb[:, ws, :], in_=job["w"]), wdeps,
                       wring.sems[ws])
            pset, pdeps = pring.next()
            tm = None
            for c in range(KCk):
                for tb in range(NTB):
                    first = (c == 0 and tb == 0)
                    last = (c == KCk - 1 and tb == NTB - 1)
                    deps = []
                    if first:
                        deps = [tw] + pdeps + (tin if m == 0 else [])
                    tm = r.op("pe", lambda e, ws=ws, c=c, tb=tb, pset=pset: e.matmul(
                        ps[:, pset * 4 + tb, :], wb[:, ws, c * 128:(c + 1) * 128], xin[:, c, tb * TB:(tb + 1) * TB],
                        start=(c == 0), stop=(c == KCk - 1)), deps, inc=last)
            wring.read(ws, tm)
            psv = ps[:, pset * 4:(pset + 1) * 4, :]
            kind = job["kind"]
            if kind == "z":
                f, fdeps = fring.next()
                b = job["bias"]
                te = r.op("act", lambda e, f=f, b=b, psv=psv: e.activation(
                    out=sf[:, f, :].rearrange("p (a t) -> p a t", a=4), in_=psv, func=AF.Identity,
                    bias=bc[:, b:b + 1], scale=1.0), [tm, tb_] + fdeps)
                pring.read(pset, te)
                ts = r.dma("sp", lambda e, f=f, job=job: e.dma_start(out=job["out"], in_=sf[:, f, :]), [te],
                           fring.sems[f])
                fring.read(f, ts)
            elif kind == "bf":
                f, fdeps = bring.next()
                te = r.op("act", lambda e, f=f, psv=psv: e.activation(
                    out=sb[:, f, :].rearrange("p (a t) -> p a t", a=4), in_=psv, func=AF.Copy), [tm] + fdeps)
                pring.read(pset, te)
                ts = r.dma("sp", lambda e, f=f, job=job: e.dma_start(out=job["out"], in_=sb[:, f, :]), [te],
                           bring.sems[f])
                bring.read(f, ts)
            elif kind == "res":
                f, fdeps = fring.next()
                tr = r.dma("sp", lambda e, f=f, job=job: e.dma_start(out=sf[:, f, :], in_=job["resid"]), fdeps,
                           rsem[f])
                te = r.op("dve", lambda e, f=f, psv=psv: e.tensor_tensor(
                    out=sf[:, f, :].rearrange("p (a t) -> p a t", a=4),
                    in0=sf[:, f, :].rearrange("p (a t) -> p a t", a=4), in1=psv, op=ALU.add), [tm, tr])
                pring.read(pset, te)
                ts = r.dma("sp", lambda e, f=f, job=job: e.dma_start(out=job["out"], in_=sf[:, f, :]), [te],
                           fring.sems[f])
                fring.read(f, ts)
        r.emit()


def phase_mlp(ctx, hT, xin, xout, wup, wdn, n_ft=128):
    nc = ctx.nc
    G = 2
    NGR = n_ft // G
    with (_sbt(nc, "f_h", [128, KC, TB], BF16) as hb,
          _sbt(nc, "f_x", [128, KC, TB], F32) as xb,
          _sbt(nc, "f_wu", [128, 2, G, KC * 128], BF16) as wu,
          _sbt(nc, "f_wd", [128, 2, G, D], BF16) as wd,
          _sbt(nc, "f_a", [128, 2, G, TB], BF16) as ab):
        r = Rec(ctx)
        ps = ctx.psum
        hv = hT.rearrange("(c p) t -> p c t", p=128)
        xiv = xin.rearrange("(c p) t -> p c t", p=128)
        xov = xout.rearrange("(c p) t -> p c t", p=128)
        wdv = wdn.rearrange("(f p) m -> p f m", p=128)
        wring = Ring(r, 2, sw=True)
        dsems = [r.dsem(True), r.dsem(True)]
        hsem = r.dsem()
        xsem = r.dsem()
        osem = r.dsem()
        aring = Ring(r, 2, with_sems=False)
        upring = Ring(r, 2, with_sems=False)
        dnring = Ring(r, 4, with_sems=False)
        last_store = None
        last_x_readers = []
        last_h_readers = []
        for tb in range(NTB):
            th = r.dma("sp", lambda e, tb=tb: e.dma_start(out=hb[:], in_=hv[:, :, tb * TB:(tb + 1) * TB]),
                       last_h_readers, hsem)
            tx = r.dma("sp", lambda e, tb=tb: e.dma_start(out=xb[:], in_=xiv[:, :, tb * TB:(tb + 1) * TB]),
                       [last_store], xsem)
            tacc = None
            for gr in range(NGR):
                ws, wdeps = wring.next()
                tw = r.dma("pool", lambda e, ws=ws, gr=gr: e.dma_start(
                    out=wu[:, ws], in_=wup[gr * G:(gr + 1) * G].rearrange("g p k -> p g k")), wdeps, wring.sems[ws])
                tw2 = r.dma("pool", lambda e, ws=ws, gr=gr: e.dma_start(
                    out=wd[:, ws], in_=wdv[:, gr * G:(gr + 1) * G, :]), wdeps, dsems[ws])
                a, adeps = aring.next()
                tas = []
                for g in range(G):
                    pb, pdeps = upring.next()
                    tm = None
                    for c in range(KC):
                        deps = ([tw, th] + pdeps) if c == 0 else []
                        tm = r.op("pe", lambda e, ws=ws, g=g, c=c, pb=pb: e.matmul(
                            ps[:, pb, :], wu[:, ws, g, c * 128:(c + 1) * 128], hb[:, c, :],
                            start=(c == 0), stop=(c == KC - 1)), deps, inc=(c == KC - 1))
                    t1 = r.op("act", lambda e, a=a, g=g, pb=pb: e.activation(
                        out=ab[:, a, g, :], in_=ps[:, pb, :], func=AF.Relu), [tm] + adeps)
                    upring.read(pb, t1)
                    t2 = r.op("act", lambda e, a=a, g=g: e.activation(
                        out=ab[:, a, g, :], in_=ab[:, a, g, :], func=AF.Square), [t1])
                    tas.append(t2)
                tmd = None
                for mt in range(KC):
                    db, ddeps = dnring.next()
                    for g in range(G):
                        deps = (tas + [tw2] + ddeps) if g == 0 else []
                        tmd = r.op("pe", lambda e, ws=ws, g=g, mt=mt, a=a, db=db: e.matmul(
                            ps[:, 4 + db, :], wd[:, ws, g, mt * 128:(mt + 1) * 128], ab[:, a, g, :],
                            start=(g == 0), stop=(g == G - 1)), deps, inc=(g == G - 1))
                    tacc = r.op("dve", lambda e, mt=mt, db=db: e.tensor_tensor(
                        out=xb[:, mt, :], in0=xb[:, mt, :], in1=ps[:, 4 + db, :], op=ALU.add), [tmd, tx])
                    dnring.read(db, tacc)
                wring.read(ws, tmd)
                aring.read(a, tmd)
            last_h_readers = [tmd]
            last_store = r.dma("sp", lambda e, tb=tb: e.dma_start(out=xov[:, :, tb * TB:(tb + 1) * TB], in_=xb[:]),
                               [tacc], osem)
        r.emit()


def tile_w(w):
    K, M = w.shape
    return np.ascontiguousarray(w.reshape(K // 128, 128, M // 128, 128).transpose(2, 1, 0, 3)).reshape(
        M // 128, 128, (K // 128) * 128)


def cols_layout(v):
    return np.ascontiguousarray(v.reshape(-1, 128).T)


def in_colmap():
    off = {}
    o = 0
    for name, sz in [("m_q", 512), ("m_k", 512), ("m_v", 512), ("m_o", 512), ("m_i", 4), ("m_f", 4),
                     ("n_q", 1024), ("n_kc", 256), ("n_vc", 256), ("n_ks", 256), ("n_vs", 256),
                     ("n_kw", 256), ("n_vw", 256), ("n_gate", 24), ("r_q", 512), ("r_k", 512),
                     ("r_v", 512), ("r_g", 512)]:
        off[name] = (o, sz)
        o += sz
    order = ["m_q", "m_k", "m_v", "m_o", "n_q", "n_kc", "n_vc", "n_ks", "n_vs", "n_kw", "n_vw",
             "r_q", "r_k", "r_v", "r_g"]
    cm = []
    zoff = {}
    for n in order:
        zoff[n] = len(cm)
        cm += list(range(off[n][0], off[n][0] + off[n][1]))
    small = [-1] * 128
    for i in range(4):
        small[i] = off["m_i"][0] + i
        small[32 + i] = off["m_f"][0] + i
    for i in range(24):
        small[64 + i] = off["n_gate"][0] + i
    zoff["small"] = len(cm)
    cm += small
    return np.array(cm), zoff


def permute_cols(w, cm):
    out = np.zeros(w.shape[:-1] + (len(cm),), w.dtype)
    valid = cm >= 0
    out[..., valid] = w[..., cm[valid]]
    return out


LN_SCALE = -0.5 * float(np.log(128.0))


def phase_decay(ctx, mode, ZT, YT, zq, zk, zv, zg, yrow, cst, par):
    nc = ctx.nc
    ml = (mode == "mlstm")
    NH = 4
    with ExitStack() as _st:
        G = _st.enter_context(_sbt(nc, "d_G", [64, S], F32))
        Gt = _st.enter_context(_sbt(nc, "d_tmp", [64, S], F32))
        G1 = _st.enter_context(_sbt(nc, "d_one64", [64, S], F32))
        Ab = _st.enter_context(_sbt(nc, "d_Ab", [128, NH, S], F32))
        bcol = _st.enter_context(_sbt(nc, "d_bcol", [128, 16, NH], F32))
        sel = _st.enter_context(_sbt(nc, "d_sel", [64, NH * 128], F32))
        i64 = _st.enter_context(_sbt(nc, "d_i64", [64, 64], F32))
        ident = _st.enter_context(_sbt(nc, "d_ident", [128, 128], BF16))
        onesb = _st.enter_context(_sbt(nc, "d_onesb", [128, 128], BF16))
        avg = _st.enter_context(_sbt(nc, "d_avg", [128, 128], BF16))
        U = _st.enter_context(_sbt(nc, "d_U", [128, 896], BF16))
        xf = _st.enter_context(_sbt(nc, "d_x", [128, 2, S], F32))
        acc = _st.enter_context(_sbt(nc, "d_acc", [128, S], F32))
        qT = _st.enter_context(_sbt(nc, "d_qT", [128, S], BF16))
        kT = _st.enter_context(_sbt(nc, "d_kT", [128, S], BF16))
        vT = _st.enter_context(_sbt(nc, "d_vT", [128, S], BF16))
        V = _st.enter_context(_sbt(nc, "d_V", [128, 16, 128], BF16))
        go = _st.enter_context(_sbt(nc, "d_go", [128, S], F32))
        cw = _st.enter_context(_sbt(nc, "d_cw", [128, 32], F32))
        cs = _st.enter_context(_sbt(nc, "d_cs", [128, 2, S], F32))
        rt = _st.enter_context(_sbt(nc, "d_rt", [128, 128], F32))
        gain = _st.enter_context(_sbt(nc, "d_gain", [128, NH], F32))
        Dt = _st.enter_context(_sbt(nc, "d_Dt", [128, 2, TB], F32))
        P = _st.enter_context(_sbt(nc, "d_P", [128, 2, TB], BF16))
        e1 = _st.enter_context(_sbt(nc, "d_e1", [128, TB], F32))
        e2 = _st.enter_context(_sbt(nc, "d_e2", [128, TB], F32))
        e3 = _st.enter_context(_sbt(nc, "d_e3", [128, TB], BF16))
        yb = _st.enter_context(_sbt(nc, "d_y", [128, S], BF16))
        r = Rec(ctx)
        ps = ctx.psum
        ld = lambda dst, src, deps=(): r.dma("sp", lambda e: e.dma_start(out=dst, in_=src), list(deps), r.dsem())
        t_sel = ld(sel[:], cst["sel64"])
        t_i64 = ld(i64[:], cst["i64"])
        t_id = ld(ident[:], cst["ident_bf"])
        t_U = ld(U[:], cst["U_bf"])
        t_on = r.op("pool", lambda e: e.memset(onesb[:], 1.0))
        t_av = r.op("pool", lambda e: e.memset(avg[:], 1.0 / 128.0))
        t_gain = ld(gain[:], par["gain"])
        if ml:
            t_g = ld(G[:], ZT[52 * 128:52 * 128 + 64, :])
            a1 = r.op("act", lambda e: e.activation(out=Gt[32:36, :], in_=G[32:36, :], func=AF.Exp, scale=-1.0), [t_g])
            a2 = r.op("act", lambda e: e.activation(out=Gt[32:36, :], in_=Gt[32:36, :], func=AF.Ln, bias=1.0,
                                                   scale=1.0), [a1])
            a3 = r.op("pool", lambda e: e.memset(G1[32:36, :], 1.0), [])
            t_G = r.op("dve", lambda e: e.tensor_tensor_scan(out=G[32:36, :], data0=G1[32:36, :], data1=Gt[32:36, :],
                                                            initial=0.0, op0=ALU.mult, op1=ALU.subtract), [a2, a3])
        else:
            t_G = ld(G[:], cst["ret_G"])
        tcol = None
        for tt in range(16):
            tcol = r.op("pe", lambda e, tt=tt: e.matmul(
                ps[:, 0:2, :].rearrange("p a (b c) -> p (a b) c", c=64)[:, tt, :], G[:, tt * 128:(tt + 1) * 128],
                i64[:], start=True, stop=True), [t_G, t_i64] if tt == 0 else [], inc=(tt == 15))
        colv = ps[:, 0:2, :].rearrange("p a (b c) -> p (a b) c", c=64)
        t_b0 = r.op("act", lambda e: e.activation(out=bcol[:], in_=colv[:, :, 0:4], func=AF.Copy), [tcol])
        t_b1 = r.op("dve", lambda e: e.tensor_tensor(out=bcol[:], in0=bcol[:], in1=colv[:, :, 32:36],
                                                    op=ALU.subtract), [t_b0])
        t_bc = r.op("dve", lambda e: e.tensor_scalar_add(out=bcol[:], in0=bcol[:], scalar1=LN_SCALE), [t_b1])
        t_ab = []
        bank = 2
        for h in range(NH):
            for tb in range(NTB):
                b = 2 + ((h * NTB + tb) % 4)
                tm = r.op("pe", lambda e, h=h, tb=tb, b=b: e.matmul(
                    ps[:, b, :], sel[:, h * 128:(h + 1) * 128], G[:, tb * TB:(tb + 1) * TB], start=True, stop=True),
                    [t_G, t_sel] + ([t_ab[-4]] if len(t_ab) >= 4 else []))
                tc = r.op("act", lambda e, h=h, tb=tb, b=b: e.activation(
                    out=Ab[:, h, tb * TB:(tb + 1) * TB], in_=ps[:, b, :], func=AF.Copy), [tm])
                t_ab.append(tc)
        t_ab_all = t_ab[-1]
        if ml:
            t_cw = ld(cw[:], par["conv_cols"])
        else:
            t_cos = ld(cs[:, 0, :], cst["cosT"])
            t_sin = ld(cs[:, 1, :], cst["sinT"])
            t_rt = ld(rt[:], cst["rotT"])
        prev_head_done = []
        xr_readers = [[], []]
        ty = None
        dst_ = {"blk": 0, "bank_free": [None, None], "ty": None}
        for h in range(NH):
            tq = []
            for wi, (zt, dst) in enumerate([(zq + h, qT), (zk + h, kT)]):
                tl = ld(xf[:, wi, :], ZT[zt * 128:(zt + 1) * 128, :], xr_readers[wi] + prev_head_done)
                if ml:
                    j = wi * 4 + h
                    t0 = r.op("dve", lambda e, wi=wi, j=j: e.tensor_scalar(
                        out=acc[:], in0=xf[:, wi, :], scalar1=cw[:, 3 * 8 + j:3 * 8 + j + 1], scalar2=None,
                        op0=ALU.mult), [tl, t_cw] + tq + prev_head_done)
                    for sft in (1, 2, 3):
                        t0 = r.op("dve", lambda e, wi=wi, j=j, sft=sft: e.scalar_tensor_tensor(
                            out=acc[:, sft:], in0=xf[:, wi, 0:S - sft],
                            scalar=cw[:, (3 - sft) * 8 + j:(3 - sft) * 8 + j + 1], in1=acc[:, sft:],
                            op0=ALU.mult, op1=ALU.add), [t0])
                    t1 = r.op("act", lambda e, dst=dst: e.activation(out=dst[:], in_=acc[:], func=AF.Silu), [t0])
                    xr_readers[wi] = [t0]
                    tq.append(t1)
                else:
                    tlast = None
                    for tb in range(NTB):
                        sl = slice(tb * TB, (tb + 1) * TB)
                        b = 2 + (tb % 2)
                        tm = r.op("pe", lambda e, wi=wi, sl=sl, b=b: e.matmul(
                            ps[:, b, :], rt[:], xf[:, wi, sl], start=True, stop=True),
                            [tl, t_rt, t_ab_all] + ([tlast] if tlast else []) + prev_head_done)
                        ta = r.op("pool", lambda e, wi=wi, sl=sl: e.tensor_tensor(
                            out=acc[:, sl], in0=xf[:, wi, sl], in1=cs[:, 0, sl], op=ALU.mult), [tl, t_cos] + tq)
                        tb2 = r.op("dve", lambda e, sl=sl, b=b: e.tensor_tensor(
                            out=e1[:], in0=ps[:, b, :], in1=cs[:, 1, sl], op=ALU.mult), [tm, t_sin] + ([tlast] if tlast else []))
                        tlast = r.op("dve", lambda e, sl=sl, dst=dst: e.tensor_tensor(
                            out=dst[:, sl], in0=acc[:, sl], in1=e1[:], op=ALU.add), [ta, tb2])
                    xr_readers[wi] = [tlast]
                    tq.append(tlast)
            tv = r.dma("pool", lambda e, h=h: e.dma_start(out=vT[:], in_=ZT[(zv + h) * 128:(zv + h + 1) * 128, :]),
                       prev_head_done, r.dsem(True))
            tg_ = ld(go[:], ZT[(zg + h) * 128:(zg + h + 1) * 128, :], prev_head_done)
            tV = None
            for g4 in range(4):
                b = 2 + (g4 % 2)
                for i in range(4):
                    tt = g4 * 4 + i
                    tm = r.op("pe", lambda e, tt=tt, i=i, b=b: e.matmul(
                        ps[:, b, i * 128:(i + 1) * 128], vT[:, tt * 128:(tt + 1) * 128], ident[:],
                        start=True, stop=True), [tv, t_id, t_ab_all] + tq + ([tV] if tV else []), inc=(i == 3))
                tV = r.op("act", lambda e, g4=g4, b=b: e.activation(
                    out=V[:, g4 * 4:(g4 + 1) * 4, :], in_=ps[:, b, :].rearrange("p (a c) -> p a c", c=128),
                    func=AF.Copy), [tm])
            tgo = r.op("act", lambda e: e.activation(out=go[:], in_=go[:], func=AF.Sigmoid if ml else AF.Silu), [tg_])
            dring = Ring(r, 2, with_sems=False)
            pring = Ring(r, 2, with_sems=False)
            sring = Ring(r, 2, with_sems=False)
            pending = None
            for qb in range(NTB):
                qsl = slice(qb * TB, (qb + 1) * TB)
                nk = 4 * qb + 4
                tpv = None
                pset = dst_["blk"] % 2
                dst_["blk"] += 1
                ob = 4 + 2 * pset
                def stS(kt, qsl=qsl):
                    sb, sdeps = sring.next()
                    tS = r.op("pe", lambda e: e.matmul(
                        ps[:, 2 + sb, :], kT[:, kt * 128:(kt + 1) * 128], qT[:, qsl], start=True, stop=True),
                        tq + [tV] + sdeps)
                    return sb, tS
                nxt = stS(0)
                for kt in range(nk):
                    c = 512 * qb - 128 * kt
                    sb, tS = nxt
                    if kt + 1 < nk:
                        nxt = stS(kt + 1)
                    d, ddeps = dring.next()
                    tD = r.op("act", lambda e, d=d, h=h, kt=kt, qsl=qsl: e.activation(
                        out=Dt[:, d, :], in_=Ab[:, h, qsl], func=AF.Exp, bias=bcol[:, kt, h:h + 1], scale=1.0),
                        [t_ab_all, t_bc] + ddeps)
                    if c <= 0:
                        tD = r.op("pool", lambda e, d=d, c=c: e.tensor_tensor(
                            out=Dt[:, d, :], in0=Dt[:, d, :], in1=U[:, c + 384:c + 384 + TB], op=ALU.mult),
                            [tD, t_U])
                    p, pdeps = pring.next()
                    tP = r.op("dve", lambda e, p=p, d=d, sb=sb: e.tensor_tensor(
                        out=P[:, p, :], in0=ps[:, 2 + sb, :], in1=Dt[:, d, :], op=ALU.mult), [tS, tD] + pdeps)
                    sring.read(sb, tP)
                    dring.read(d, tP)
                    first = (kt == 0)
                    lastk = (kt == nk - 1)
                    bfree = dst_["bank_free"][pset]
                    r.op("pe", lambda e, p=p, kt=kt, first=first, lastk=lastk, ob=ob: e.matmul(
                        ps[:, ob, :], V[:, kt, :], P[:, p, :], start=first, stop=lastk),
                        [tP] + ([bfree] if first and bfree else []), inc=False)
                    tpv = r.op("pe", lambda e, p=p, first=first, lastk=lastk, ob=ob: e.matmul(
                        ps[:, ob + 1, :], onesb[:], P[:, p, :], start=first, stop=lastk), [t_on])
                    pring.read(p, tpv)
                    if kt >= 1 and pending is not None:
                        if next(pending, "done") == "done":
                            pending = None

                def ep(h=h, qsl=qsl, ob=ob, pset=pset, tpv=tpv):
                    ty = dst_["ty"]
                    if ml:
                        t1 = r.op("act", lambda e: e.activation(out=e1[:], in_=ps[:, ob + 1, :], func=AF.Abs),
                                  [tpv, ty])
                        yield
                        t1 = r.op("dve", lambda e: e.tensor_scalar_max(out=e1[:], in0=e1[:], scalar1=1.0), [t1])
                        t1 = r.op("act", lambda e: e.activation(out=e1[:], in_=e1[:], func=AF.Ln), [t1])
                        t1 = r.op("act", lambda e: e.activation(out=e1[:], in_=e1[:], func=AF.Exp, scale=-1.0), [t1])
                        t2 = r.op("dve", lambda e: e.tensor_tensor(out=e2[:], in0=ps[:, ob, :], in1=e1[:],
                                                                  op=ALU.mult), [t1])
                    else:
                        t2 = r.op("act", lambda e: e.activation(out=e2[:], in_=ps[:, ob, :], func=AF.Copy),
                                  [tpv, ty])
                    dst_["bank_free"][pset] = t2
                    yield
                    t3 = r.op("act", lambda e: e.activation(out=e3[:], in_=e2[:], func=AF.Copy), [t2])
                    yield
                    tm1 = r.op("pe", lambda e: e.matmul(ps[:, 0, :], avg[:], e3[:], start=True, stop=True),
                               [t3, t_av, t_bc])
                    yield
                    t4 = r.op("dve", lambda e: e.tensor_tensor(out=e2[:], in0=e2[:], in1=ps[:, 0, :],
                                                              op=ALU.subtract), [tm1])
                    yield
                    t5 = r.op("act", lambda e: e.activation(out=e3[:], in_=e2[:], func=AF.Square), [t4])
                    yield
                    tm2 = r.op("pe", lambda e: e.matmul(ps[:, 1, :], avg[:], e3[:], start=True, stop=True), [t5])
                    yield
                    t6 = r.op("act", lambda e: e.activation(out=e1[:], in_=ps[:, 1, :], func=AF.Ln, bias=EPS,
                                                           scale=1.0), [tm2])
                    t7 = r.op("act", lambda e: e.activation(out=e1[:], in_=e1[:], func=AF.Exp, scale=-0.5), [t6])
                    yield
                    t8 = r.op("dve", lambda e: e.tensor_tensor(out=e2[:], in0=e2[:], in1=e1[:], op=ALU.mult), [t7])
                    dst_["ty"] = r.op("dve", lambda e: e.scalar_tensor_tensor(
                        out=yb[:, qsl], in0=e2[:], scalar=gain[:, h:h + 1], in1=go[:, qsl], op0=ALU.mult,
                        op1=ALU.mult), [t8, tgo, t_gain])
                if pending is not None:
                    for _ in pending:
                        pass
                pending = ep()
            for _ in pending:
                pass
            pending = None
            ty = dst_["ty"]
            tst = r.dma("sp", lambda e, h=h: e.dma_start(out=YT[yrow + h * 128:yrow + (h + 1) * 128, :], in_=yb[:]),
                        [ty], r.dsem())
            prev_head_done = [tst, ty]
        r.emit()


def bucket_starts():
    n = np.arange(0, 4096)
    exact = 16
    lr = np.log(np.maximum(n, 1).astype(np.float32) / np.float32(exact)) / np.float32(np.log(128 / exact))
    large = np.minimum(exact + (lr.astype(np.float32) * np.float32(32 - exact)).astype(np.int32), 31)
    bk = np.where(n < exact, n, large)
    return [int(np.argmax(bk == b)) for b in range(32)], bk


def phase_setup(ctx, rb_row, TT, TTW, BC):
    nc = ctx.nc
    starts, _ = bucket_starts()
    W = 1408
    with ExitStack() as st:
        sb = lambda n, s, d: st.enter_context(_sbt(nc, n, s, d))
        rbr = sb("s_rbr", [1, 256], F32)
        one1 = sb("s_one1", [1, 128], F32)
        rbB = sb("s_rbB", [128, 32, 8], F32)
        eB = sb("s_eB", [128, 32, 8], F32)
        CB = sb("s_CB", [128, 32, 8], F32)
        dT = sb("s_dT", [128, W], F32)
        dC = sb("s_dC", [128, S], F32)
        mT = sb("s_mT", [128, W], F32)
        mC = sb("s_mC", [128, S], F32)
        aT = sb("s_aT", [128, 8, W], F32)
        aC = sb("s_aC", [128, 8, S], F32)
        aW = sb("s_aW", [128, 8, W], F32)
        r = Rec(ctx)
        ps = ctx.psum
        t0 = r.dma("sp", lambda e: e.dma_start(out=rbr[:], in_=rb_row), [], r.dsem())
        t1 = r.op("pool", lambda e: e.memset(one1[:], 1.0))
        tm = r.op("pe", lambda e: e.matmul(ps[:, 0, 0:256], one1[:], rbr[:], start=True, stop=True), [t0, t1])
        tc = r.op("act", lambda e: e.activation(out=rbB[:].rearrange("p b h -> p (b h)"), in_=ps[:, 0, 0:256],
                                               func=AF.Copy), [tm])
        td = tc
        for b in range(32):
            td = r.op("dve", lambda e, b=b: e.tensor_tensor(out=eB[:, b, :], in0=rbB[:, b, :], in1=rbB[:, 31, :],
                                                           op=ALU.subtract), [tc])
        te = r.op("act", lambda e: e.activation(out=eB[:].rearrange("p b h -> p (b h)"),
                                               in_=eB[:].rearrange("p b h -> p (b h)"), func=AF.Exp), [td])
        tcb = r.op("dve", lambda e: e.tensor_tensor(out=CB[:, 1:32, :], in0=eB[:, 1:32, :], in1=eB[:, 0:31, :],
                                                   op=ALU.subtract), [te])
        tcb = r.op("dve", lambda e: e.tensor_copy(out=CB[:, 0, :], in_=eB[:, 0, :]), [tcb])
        ti1 = r.op("pool", lambda e: e.iota(dT[:], [[1, W]], base=-384, channel_multiplier=-1,
                                           allow_small_or_imprecise_dtypes=True))
        ti2 = r.op("pool", lambda e: e.iota(dC[:], [[1, S]], base=-31, channel_multiplier=-16,
                                           allow_small_or_imprecise_dtypes=True))
        ta = None
        for b in range(32):
            sv = float(starts[b])
            tmk = r.op("dve", lambda e, sv=sv: e.tensor_single_scalar(out=mT[:], in_=dT[:], scalar=sv, op=ALU.is_ge),
                       [ti1, tcb] + ([ta] if ta else []))
            tmk2 = r.op("dve", lambda e, sv=sv: e.tensor_single_scalar(out=mC[:], in_=dC[:], scalar=sv, op=ALU.is_ge),
                        [ti2])
            for h in range(8):
                if b == 0:
                    r.op("dve", lambda e, h=h, b=b: e.tensor_scalar(out=aT[:, h, :], in0=mT[:], scalar1=CB[:, b, h:h + 1],
                                                                   scalar2=None, op0=ALU.mult), [tmk])
                    ta = r.op("dve", lambda e, h=h, b=b: e.tensor_scalar(out=aC[:, h, :], in0=mC[:],
                                                                        scalar1=CB[:, b, h:h + 1], scalar2=None,
                                                                        op0=ALU.mult), [tmk2])
                else:
                    r.op("dve", lambda e, h=h, b=b: e.scalar_tensor_tensor(
                        out=aT[:, h, :], in0=mT[:], scalar=CB[:, b, h:h + 1], in1=aT[:, h, :], op0=ALU.mult,
                        op1=ALU.add), [tmk])
                    ta = r.op("dve", lambda e, h=h, b=b: e.scalar_tensor_tensor(
                        out=aC[:, h, :], in0=mC[:], scalar=CB[:, b, h:h + 1], in1=aC[:, h, :], op0=ALU.mult,
                        op1=ALU.add), [tmk2])
        tw = r.op("dve", lambda e: e.tensor_single_scalar(out=mT[:], in_=dT[:], scalar=512.0, op=ALU.is_lt), [ta])
        for h in range(8):
            tw2 = r.op("dve", lambda e, h=h: e.tensor_tensor(out=aW[:, h, :], in0=aT[:, h, :], in1=mT[:], op=ALU.mult),
                       [tw])
        r.dma("pool", lambda e: e.dma_start(out=TT.rearrange("h p x -> p h x"), in_=aT[:]), [tw2], r.dsem(True))
        r.dma("pool", lambda e: e.dma_start(out=TTW.rearrange("h p x -> p h x"), in_=aW[:]), [tw2], r.dsem(True))
        r.dma("pool", lambda e: e.dma_start(out=BC.rearrange("h p x -> p h x"), in_=aC[:]), [tw2], r.dsem(True))
        r.emit()


QSCALE = float(128.0 ** -0.5)
GC1 = 0.044715
GC2 = 2.0 * float(np.sqrt(2.0 / np.pi))


def phase_nsa(ctx, ZT, YT, cst, par, TT, TTW, BC):
    nc = ctx.nc
    ZQ, ZKC, ZVC, ZKS, ZVS, ZKW, ZVW = 16, 24, 26, 28, 30, 32, 34
    with ExitStack() as st:
        sb = lambda n, s, d: st.enter_context(_sbt(nc, n, s, d))
        ident = sb("a_ident", [128, 128], BF16)
        onesb = sb("a_onesb", [128, 128], BF16)
        expand = sb("a_expand", [32, S], BF16)
        selg = sb("a_selg", [128, 24 * 128], F32)
        force = sb("a_force", [128, 16, 32], F32)
        SG = sb("a_SG", [128, S], F32)
        w1 = sb("a_w1", [128, 2, 32, 128], BF16)
        w2 = sb("a_w2", [128, 2, 128], BF16)
        posT = sb("a_pos", [128, 2, 32], F32)
        xf = sb("a_xf", [128, S], F32)
        xl = sb("a_xl", [128, 32, 127], BF16)
        g1 = sb("a_g1", [128, 128], F32)
        g2 = sb("a_g2", [128, 128], F32)
        gT = sb("a_gT", [128, 128], BF16)
        kcT = sb("a_kcT", [128, 128], BF16)
        VC = sb("a_VC", [128, 161], BF16)
        qT = sb("a_qT", [128, 4, S], BF16)
        ksT = sb("a_ksT", [128, S], BF16)
        kwT = sb("a_kwT", [128, S], BF16)
        vT = sb("a_vT", [128, S], BF16)
        VS = sb("a_VS", [128, 16, 128], BF16)
        VW = sb("a_VW", [128, 16, 128], BF16)
        imp = sb("a_imp", [128, 16, 32], F32)
        m8 = sb("a_m8", [128, 16, 8], F32)
        Mm = sb("a_M", [128, 16, 32], BF16)
        MT = sb("a_MT", [32, S], BF16)
        bc = sb("a_bc", [128, S], BF16)
        tt = sb("a_tt", [128, 1408], BF16)
        ttw = sb("a_ttw", [128, 1408], BF16)
        yacc = sb("a_yacc", [128, 4, S], F32)
        E = sb("a_E", [128, 2, TB], F32)
        P = sb("a_P", [128, 2, TB], BF16)
        e1 = sb("a_e1", [128, TB], F32)
        e2 = sb("a_e2", [128, TB], F32)
        rd = sb("a_rd", [128, 4], F32)
        ys = sb("a_ys", [128, S], BF16)
        gbS = sb("a_gbS", [128, 2, S], F32)
        r = Rec(ctx)
        ps = ctx.psum
        roles = {}

        def rsem(role, sw=False):
            if role is None:
                return r.dsem(sw)
            if role not in roles:
                roles[role] = r.dsem(sw)
            return roles[role]
        ld = lambda dst, src, deps=(), role=None: r.dma("sp", lambda e: e.dma_start(out=dst, in_=src), list(deps),
                                                       rsem(role))
        ldc = lambda dst, src, deps=(), role=None: r.dma("pool", lambda e: e.dma_start(out=dst, in_=src), list(deps),
                                                        rsem(role, True))
        t_id = ld(ident[:], cst["ident_bf"])
        t_ex = ld(expand[:], cst["expand_bf"])
        t_sg = ld(selg[64:96, :], cst["selg"])
        t_fo = ld(force[:], cst["force"])
        t_on = r.op("pool", lambda e: e.memset(onesb[:], 1.0))
        t_SG = ld(SG[64:96, :], ZT[52 * 128 + 64:52 * 128 + 96, :])
        t_SG = r.op("act", lambda e: e.activation(out=SG[64:96, :], in_=SG[64:96, :], func=AF.Sigmoid), [t_SG])
        t_w1 = [ldc(w1[:, i].rearrange("p l o -> p (l o)"), par["w1"][i]) for i in range(2)]
        t_w2 = [ldc(w2[:, i], par["w2"][i]) for i in range(2)]
        t_pos = ld(posT[:], par["posT"])
        gdone = []
        sring = Ring(r, 2, with_sems=False)
        mring = Ring(r, 2, with_sems=False)
        ering = Ring(r, 2, with_sems=False)
        pring = Ring(r, 2, with_sems=False)
        ep_prev = None
        ep2_prev = None
        tlast_any = None
        for g in range(2):
            t_init = r.op("pool", lambda e: e.memset(kcT[:], 0.0), gdone)
            t_init2 = r.op("pool", lambda e: e.memset(VC[:], 0.0), gdone)
            t_ov = ld(VC[:, 129:161], cst["ov_bf"], [t_init2], "ov")
            t_one = r.op("pool", lambda e: e.memset(VC[0:127, 128:129], 1.0), [t_init2])
            tcmp = []
            for i, zt in enumerate((ZKC + g, ZVC + g)):
                tl = ld(xf[:], ZT[zt * 128:(zt + 1) * 128, :], gdone + tcmp, "xf")
                xv = xf[:].rearrange("p (j i) -> p j i", i=16)
                tx = None
                for l in range(32):
                    j0 = 0 if l < 16 else 1
                    tx = r.op("dve", lambda e, l=l, j0=j0, i=i, xv=xv: e.tensor_scalar(
                        out=xl[:, l, :], in0=xv[:, j0:j0 + 127, l % 16], scalar1=posT[:, i, l:l + 1], scalar2=None,
                        op0=ALU.add), [tl, t_pos] + tcmp)
                b, bdeps = mring.next()
                tm = None
                for l in range(32):
                    tm = r.op("pe", lambda e, l=l, i=i, b=b: e.matmul(
                        ps[:, 2 + b, 0:127], w1[:, i, l, :], xl[:, l, :], start=(l == 0), stop=(l == 31)),
                        [tx, t_w1[i]] + bdeps, inc=(l == 31))
                pre = ps[:, 2 + b, 0:127]
                ta = r.op("act", lambda e, pre=pre: e.activation(out=g1[:, 0:127], in_=pre, func=AF.Square), [tm])
                ta = r.op("dve", lambda e: e.tensor_scalar(out=g1[:, 0:127], in0=g1[:, 0:127], scalar1=GC1,
                                                          scalar2=1.0, op0=ALU.mult, op1=ALU.add), [ta])
                ta = r.op("dve", lambda e, pre=pre: e.tensor_tensor(out=g1[:, 0:127], in0=g1[:, 0:127], in1=pre,
                                                                  op=ALU.mult), [ta])
                ta = r.op("act", lambda e: e.activation(out=g2[:, 0:127], in_=g1[:, 0:127], func=AF.Sigmoid,
                                                       scale=GC2), [ta])
                tg = r.op("dve", lambda e, pre=pre: e.tensor_tensor(out=gT[:, 0:127], in0=g2[:, 0:127], in1=pre,
                                                                  op=ALU.mult), [ta])
                mring.read(b, tg)
                b2, b2deps = mring.next()
                if i == 0:
                    tm2 = r.op("pe", lambda e, b2=b2: e.matmul(ps[:, 2 + b2, 0:127], w2[:, 0, :], gT[:, 0:127],
                                                              start=True, stop=True), [tg, t_w2[0]] + b2deps)
                    tk = r.op("act", lambda e, b2=b2: e.activation(out=kcT[:, 0:127], in_=ps[:, 2 + b2, 0:127],
                                                                  func=AF.Copy), [tm2, t_init])
                else:
                    tm2 = r.op("pe", lambda e, b2=b2: e.matmul(ps[0:127, 2 + b2, 0:128], gT[:, 0:127], w2[:, 1, :],
                                                              start=True, stop=True), [tg, t_w2[1]] + b2deps)
                    tk = r.op("act", lambda e, b2=b2: e.activation(out=VC[0:127, 0:128], in_=ps[0:127, 2 + b2, 0:128],
                                                                  func=AF.Copy), [tm2, t_init2])
                mring.read(b2, tk)
                tcmp = [tk]
            t_cmp = [tk, t_ov, t_one]
            if os.environ.get("NSA_STOP") == "1":
                r.emit()
                return
            t_q = [ldc(qT[:, rr, :], ZT[(ZQ + g * 4 + rr) * 128:(ZQ + g * 4 + rr + 1) * 128, :], gdone, "q%d" % rr)
                   for rr in range(4)]
            t_ks = ldc(ksT[:], ZT[(ZKS + g) * 128:(ZKS + g + 1) * 128, :], gdone, "ks")
            t_kw = ldc(kwT[:], ZT[(ZKW + g) * 128:(ZKW + g + 1) * 128, :], gdone, "kw")
            tV = {}
            tprev = t_cmp
            for nm, zt, dstV in (("s", ZVS + g, VS), ("w", ZVW + g, VW)):
                tv = ldc(vT[:], ZT[zt * 128:(zt + 1) * 128, :], gdone + ([tV["s"]] if nm == "w" else []), "vT")
                tvv = None
                for g4 in range(4):
                    b, bdeps = mring.next()
                    for i in range(4):
                        ttt = g4 * 4 + i
                        tm = r.op("pe", lambda e, ttt=ttt, i=i, b=b: e.matmul(
                            ps[:, 2 + b, i * 128:(i + 1) * 128], vT[:, ttt * 128:(ttt + 1) * 128], ident[:],
                            start=True, stop=True), [tv, t_id] + bdeps, inc=(i == 3))
                    tvv = r.op("act", lambda e, g4=g4, b=b, dstV=dstV: e.activation(
                        out=dstV[:, g4 * 4:(g4 + 1) * 4, :], in_=ps[:, 2 + b, :].rearrange("p (a c) -> p a c", c=128),
                        func=AF.Copy), [tm] + gdone)
                    mring.read(b, tvv)
                tV[nm] = tvv
            if os.environ.get("NSA_STOP") == "2":
                r.emit()
                return
            timp = None
            for rr in range(4):
                hq = g * 4 + rr
                t_bc = ldc(bc[:], BC[hq], [tlast_any] if tlast_any else [], "bc")
                for qb in range(NTB):
                    qsl = slice(qb * TB, (qb + 1) * TB)
                    s_, sdeps = sring.next()
                    tS = r.op("pe", lambda e, rr=rr, qsl=qsl, s_=s_: e.matmul(
                        ps[:, s_, :], kcT[:], qT[:, rr, qsl], start=True, stop=True), t_cmp + [t_q[rr]] + sdeps)
                    ee, edeps = ering.next()
                    tE = r.op("act", lambda e, ee=ee, s_=s_: e.activation(out=E[:, ee, :], in_=ps[:, s_, :],
                                                                        func=AF.Exp, scale=QSCALE), [tS] + edeps)
                    sring.read(s_, tE)
                    pp, pdeps = pring.next()
                    tP = r.op("dve", lambda e, pp=pp, ee=ee, qsl=qsl: e.tensor_tensor(
                        out=P[:, pp, :], in0=E[:, ee, :], in1=bc[:, qsl], op=ALU.mult), [tE, t_bc] + pdeps)
                    ering.read(ee, tP)
                    r.op("pe", lambda e, pp=pp: e.matmul(ps[:, 4, :], VC[:, 0:128], P[:, pp, :], start=True,
                                                        stop=True), [tP] + ([ep_prev] if ep_prev else []), inc=False)
                    r.op("pe", lambda e, pp=pp: e.matmul(ps[:, 5, :], onesb[:], P[:, pp, :], start=True, stop=True),
                         [t_on], inc=False)
                    tI = None
                    for i in range(4):
                        tI = r.op("pe", lambda e, pp=pp, i=i: e.matmul(
                            ps[:, 6, i * 64:i * 64 + 33], P[:, pp, i * 128:(i + 1) * 128], VC[:, 128:161],
                            start=True, stop=True), [ep2_prev] if (i == 0 and ep2_prev) else [], inc=(i == 3))
                    pring.read(pp, tI)
                    mb, mdeps = mring.next()
                    tgm = r.op("pe", lambda e, mb=mb, hq=hq, qsl=qsl: e.matmul(
                        ps[:, 2 + mb, :], selg[64:96, (0 * 8 + hq) * 128:(0 * 8 + hq + 1) * 128], SG[64:96, qsl],
                        start=True, stop=True), [t_SG, t_sg] + mdeps)
                    t1 = r.op("dve", lambda e: e.tensor_scalar_max(out=e1[:], in0=ps[:, 5, :], scalar1=1e-18),
                              [tI, tlast_any])
                    t1 = r.op("act", lambda e: e.activation(out=e1[:], in_=e1[:], func=AF.Ln), [t1])
                    t1 = r.op("act", lambda e: e.activation(out=e1[:], in_=e1[:], func=AF.Exp, scale=-1.0), [t1])
                    t2 = r.op("dve", lambda e: e.tensor_tensor(out=e2[:], in0=ps[:, 4, :], in1=e1[:], op=ALU.mult),
                              [t1])
                    ep_prev = t2
                    t3 = r.op("dve", lambda e, rr=rr, qsl=qsl, mb=mb: e.tensor_tensor(
                        out=yacc[:, rr, qsl], in0=e2[:], in1=ps[:, 2 + mb, :], op=ALU.mult), [t2, tgm] + gdone)
                    mring.read(mb, t3)
                    t4 = r.op("dve", lambda e: e.tensor_scalar_max(
                        out=rd[:], in0=ps[:, 6, 0:256].rearrange("p (i c) -> p i c", c=64)[:, :, 0], scalar1=1e-30),
                        [tI, t3])
                    t4 = r.op("dve", lambda e: e.reciprocal(out=rd[:], in_=rd[:]), [t4])
                    for i in range(4):
                        qt = qb * 4 + i
                        if rr == 0:
                            timp = r.op("dve", lambda e, i=i, qt=qt: e.tensor_scalar(
                                out=imp[:, qt, :], in0=ps[:, 6, i * 64 + 1:i * 64 + 33], scalar1=rd[:, i:i + 1],
                                scalar2=None, op0=ALU.mult), [t4] + gdone)
                        else:
                            timp = r.op("dve", lambda e, i=i, qt=qt: e.scalar_tensor_tensor(
                                out=imp[:, qt, :], in0=ps[:, 6, i * 64 + 1:i * 64 + 33], scalar=rd[:, i:i + 1],
                                in1=imp[:, qt, :], op0=ALU.mult, op1=ALU.add), [t4])
                    ep2_prev = timp
                    tlast_any = timp
            if os.environ.get("NSA_STOP") == "3":
                r.emit()
                return
            tk_ = r.op("dve", lambda e: e.tensor_tensor(out=imp[:], in0=imp[:], in1=force[:], op=ALU.add),
                       [timp, t_fo])
            for qt in range(16):
                tk1 = r.op("dve", lambda e, qt=qt: e.max(out=m8[:, qt, :], in_=imp[:, qt, :]), [tk_])
                tk2 = r.op("dve", lambda e, qt=qt: e.tensor_scalar(
                    out=Mm[:, qt, :], in0=imp[:, qt, :], scalar1=m8[:, qt, 7:8], scalar2=None, op0=ALU.is_ge),
                    [tk1] + gdone)
            tMT = None
            for g4 in range(4):
                b, bdeps = mring.next()
                for i in range(4):
                    qt = g4 * 4 + i
                    tm = r.op("pe", lambda e, qt=qt, i=i, b=b: e.matmul(
                        ps[0:32, 2 + b, i * 128:(i + 1) * 128], Mm[:, qt, :], ident[:], start=True, stop=True),
                        [tk2, t_id] + bdeps, inc=(i == 3))
                tMT = r.op("act", lambda e, g4=g4, b=b: e.activation(
                    out=MT[:, g4 * TB:(g4 + 1) * TB], in_=ps[0:32, 2 + b, :], func=AF.Copy), [tm] + gdone)
                mring.read(b, tMT)
            if os.environ.get("NSA_STOP") == "4":
                r.emit()
                return
            pend = [None]
            for rr in range(4):
                hq = g * 4 + rr
                t_tt = ldc(tt[:], TT[hq], [tlast_any], "tt")
                t_tw = ldc(ttw[:], TTW[hq], [tlast_any], "ttw")
                t_gb = None
                for bi, br_ in enumerate((1, 2)):
                    for qb_ in range(NTB):
                        mb, mdeps = mring.next()
                        tgm = r.op("pe", lambda e, mb=mb, br_=br_, qb_=qb_, hq=hq: e.matmul(
                            ps[:, 2 + mb, :], selg[64:96, (br_ * 8 + hq) * 128:(br_ * 8 + hq + 1) * 128],
                            SG[64:96, qb_ * TB:(qb_ + 1) * TB], start=True, stop=True), [t_SG, t_sg] + mdeps)
                        t_gb = r.op("act", lambda e, mb=mb, bi=bi, qb_=qb_: e.activation(
                            out=gbS[:, bi, qb_ * TB:(qb_ + 1) * TB], in_=ps[:, 2 + mb, :], func=AF.Copy),
                            [tgm, tlast_any])
                        mring.read(mb, t_gb)
                for qb in range(NTB):
                    qsl = slice(qb * TB, (qb + 1) * TB)
                    for br in (2, 1):
                        win = (br == 2)
                        kts = list(range(max(0, 4 * qb - 4), 4 * qb + 4)) if win else list(range(0, 4 * qb + 4))
                        Kt = kwT if win else ksT
                        Vt = VW if win else VS
                        tkk = t_kw if win else t_ks
                        ob = 4 if win else 6
                        tpv = None
                        def stS(kt, rr=rr, qsl=qsl, Kt=Kt, win=win, tkk=tkk):
                            s_, sdeps = sring.next()
                            tS = r.op("pe", lambda e: e.matmul(
                                ps[:, s_, :], Kt[:, kt * 128:(kt + 1) * 128], qT[:, rr, qsl], start=True, stop=True),
                                [tkk, t_q[rr], tV["w"]] + sdeps)
                            mb = tM = None
                            if not win:
                                mb, mdeps = mring.next()
                                tM = r.op("pe", lambda e: e.matmul(
                                    ps[:, 2 + mb, :], expand[:, kt * 128:(kt + 1) * 128], MT[:, qsl], start=True,
                                    stop=True), [tMT, t_ex] + mdeps)
                            return s_, tS, mb, tM
                        NOLA = bool(os.environ.get("NSA_NOLA"))
                        nxt = None if NOLA else stS(kts[0])
                        for n_, kt in enumerate(kts):
                            c = 512 * qb - 128 * kt
                            if NOLA:
                                s_, tS, mb, tM = stS(kt)
                            else:
                                s_, tS, mb, tM = nxt
                                if n_ + 1 < len(kts):
                                    nxt = stS(kts[n_ + 1])
                            ee, edeps = ering.next()
                            tE = r.op("act", lambda e, ee=ee, s_=s_: e.activation(
                                out=E[:, ee, :], in_=ps[:, s_, :], func=AF.Exp, scale=QSCALE), [tS] + edeps)
                            sring.read(s_, tE)
                            pp, pdeps = pring.next()
                            if win:
                                tP = r.op("dve", lambda e, pp=pp, ee=ee, c=c: e.tensor_tensor(
                                    out=P[:, pp, :], in0=E[:, ee, :], in1=ttw[:, c + 384:c + 384 + TB], op=ALU.mult),
                                    [tE, t_tw] + pdeps)
                            else:
                                if c < 256:
                                    tE = r.op("pool", lambda e, ee=ee, c=c: e.tensor_tensor(
                                        out=E[:, ee, :], in0=E[:, ee, :], in1=tt[:, c + 384:c + 384 + TB],
                                        op=ALU.mult), [tE, t_tt])
                                tP = r.op("dve", lambda e, pp=pp, ee=ee, mb=mb: e.tensor_tensor(
                                    out=P[:, pp, :], in0=E[:, ee, :], in1=ps[:, 2 + mb, :], op=ALU.mult),
                                    [tE, tM] + pdeps)
                                mring.read(mb, tP)
                            ering.read(ee, tP)
                            first = (n_ == 0)
                            lastk = (n_ == len(kts) - 1)
                            prevdep = (ep_prev if win else ep2_prev)
                            r.op("pe", lambda e, pp=pp, kt=kt, first=first, lastk=lastk, Vt=Vt, ob=ob: e.matmul(
                                ps[:, ob, :], Vt[:, kt, :], P[:, pp, :], start=first, stop=lastk),
                                [tP] + ([prevdep] if first and prevdep else []), inc=False)
                            tpv = r.op("pe", lambda e, pp=pp, first=first, lastk=lastk, ob=ob: e.matmul(
                                ps[:, ob + 1, :], onesb[:], P[:, pp, :], start=first, stop=lastk), [t_on])
                            pring.read(pp, tpv)
                            if n_ >= 1 and pend[0] is not None:
                                if next(pend[0], "done") == "done":
                                    pend[0] = None

                        def ep(hq=hq, qsl=qsl, br=br, ob=ob, win=win, tpv=tpv, rr=rr, t_gb=t_gb):
                            nonlocal ep_prev, ep2_prev, tlast_any
                            if os.environ.get("NSA_RECIP"):
                                t1 = r.op("dve", lambda e: e.reciprocal(out=e1[:], in_=ps[:, ob + 1, :]),
                                          [tpv, tlast_any])
                            else:
                                t1 = r.op("act", lambda e: e.activation(out=e1[:], in_=ps[:, ob + 1, :], func=AF.Ln),
                                          [tpv, tlast_any])
                                t1 = r.op("act", lambda e: e.activation(out=e1[:], in_=e1[:], func=AF.Exp,
                                                                       scale=-1.0), [t1])
                            t2 = r.op("dve", lambda e: e.tensor_tensor(out=e2[:], in0=ps[:, ob, :], in1=e1[:],
                                                                      op=ALU.mult), [t1])
                            if win:
                                ep_prev = t2
                            else:
                                ep2_prev = t2
                            tlast_any = t2
                            yield
                            t3 = r.op("dve", lambda e: e.tensor_tensor(out=e2[:], in0=e2[:], in1=gbS[:, br - 1, qsl],
                                                                      op=ALU.mult), [t2, t_gb])
                            tlast_any = r.op("dve", lambda e: e.tensor_tensor(
                                out=yacc[:, rr, qsl], in0=yacc[:, rr, qsl], in1=e2[:], op=ALU.add), [t3])
                        if pend[0] is not None:
                            for _ in pend[0]:
                                pass
                        pend[0] = ep()
                if pend[0] is not None:
                    for _ in pend[0]:
                        pass
                    pend[0] = None
                tys = r.op("act", lambda e, rr=rr: e.activation(out=ys[:], in_=yacc[:, rr, :], func=AF.Copy),
                           [tlast_any] + gdone)
                tst = r.dma("sp", lambda e, hq=hq: e.dma_start(out=YT[512 + hq * 128:512 + (hq + 1) * 128, :],
                                                              in_=ys[:]), [tys], rsem("st"))
                gdone = [tst, tys]
            gdone = gdone + [tlast_any]
        r.emit()


def phase_merge(ctx, GL, YT, MTo, wgu, wbr, bg_cols):
    nc = ctx.nc
    ybase = [0, 4, 12]
    ykc = [4, 8, 4]
    with ExitStack() as st:
        sb = lambda n, s, d: st.enter_context(_sbt(nc, n, s, d))
        gl = sb("m_gl", [128, 8, S], BF16)
        yt = sb("m_yt", [128, 16, S], BF16)
        wg = sb("m_wg", [128, 2, 3, 8 * 128], BF16)
        wb = sb("m_wb", [128, 2, 16 * 128], BF16)
        bg = sb("m_bg", [128, 96], F32)
        sg = sb("m_sg", [128, 2, TB], F32)
        acc = sb("m_acc", [128, TB], F32)
        tmp = sb("m_tmp", [128, TB], F32)
        ob = sb("m_ob", [128, 2, S], BF16)
        r = Rec(ctx)
        ps = ctx.psum
        t_bg = r.dma("sp", lambda e: e.dma_start(out=bg[:], in_=bg_cols), [], r.dsem())
        t_gl = r.dma("sp", lambda e: e.dma_start(out=gl[:], in_=GL.rearrange("(c p) t -> p c t", p=128)), [], r.dsem())
        t_yt = [r.dma("sp", lambda e, i=i: e.dma_start(
            out=yt[:, i * 8:(i + 1) * 8, :], in_=YT.rearrange("(c p) t -> p c t", p=128)[:, i * 8:(i + 1) * 8, :]), [],
            r.dsem()) for i in range(2)]
        wring = Ring(r, 2, sw=True)
        wsem2 = [r.dsem(True), r.dsem(True)]
        gring = Ring(r, 2, with_sems=False)
        bring = Ring(r, 2, with_sems=False)
        sring = Ring(r, 2, with_sems=False)
        oring = Ring(r, 2)
        tacc = None
        for mt in range(KC):
            ws, wdeps = wring.next()
            tw1 = r.dma("pool", lambda e, ws=ws, mt=mt: e.dma_start(
                out=wg[:, ws], in_=wgu.rearrange("(b m) p k -> m p b k", b=3)[mt]), wdeps, wring.sems[ws])
            tw2s = []
            off = 0
            for b in range(3):
                n = ykc[b] * 128
                tw2s.append(r.dma("pool", lambda e, ws=ws, mt=mt, b=b, off=off, n=n: e.dma_start(
                    out=wb[:, ws, off:off + n], in_=wbr[b][mt]), wdeps, wsem2[ws]))
                off += n
            o, odeps = oring.next()
            tlastmm = None
            for tb in range(NTB):
                tsl = slice(tb * TB, (tb + 1) * TB)
                for b in range(3):
                    gb, gdeps = gring.next()
                    tmg = None
                    for c in range(8):
                        tmg = r.op("pe", lambda e, ws=ws, b=b, c=c, gb=gb, tsl=tsl: e.matmul(
                            ps[:, gb, :], wg[:, ws, b, c * 128:(c + 1) * 128], gl[:, c, tsl], start=(c == 0),
                            stop=(c == 7)), [tw1, t_gl] + gdeps, inc=(c == 7))
                    bb, bdeps = bring.next()
                    boff = sum(ykc[:b]) * 128
                    tmb = None
                    for c in range(ykc[b]):
                        tmb = r.op("pe", lambda e, ws=ws, b=b, c=c, bb=bb, tsl=tsl, boff=boff: e.matmul(
                            ps[:, 2 + bb, :], wb[:, ws, boff + c * 128:boff + (c + 1) * 128],
                            yt[:, ybase[b] + c, tsl], start=(c == 0), stop=(c == ykc[b] - 1)),
                            tw2s + t_yt + bdeps, inc=(c == ykc[b] - 1))
                    tlastmm = tmb
                    s_, sdeps = sring.next()
                    tsg = r.op("act", lambda e, s_=s_, gb=gb, b=b, mt=mt: e.activation(
                        out=sg[:, s_, :], in_=ps[:, gb, :], func=AF.Sigmoid,
                        bias=bg[:, b * 32 + mt:b * 32 + mt + 1], scale=1.0), [tmg, t_bg] + sdeps)
                    gring.read(gb, tsg)
                    if b == 0:
                        tacc = r.op("dve", lambda e, s_=s_, bb=bb: e.tensor_tensor(
                            out=acc[:], in0=sg[:, s_, :], in1=ps[:, 2 + bb, :], op=ALU.mult), [tsg, tmb, tacc])
                    elif b == 1:
                        t_ = r.op("dve", lambda e, s_=s_, bb=bb: e.tensor_tensor(
                            out=tmp[:], in0=sg[:, s_, :], in1=ps[:, 2 + bb, :], op=ALU.mult), [tsg, tmb, tacc])
                        tacc = r.op("dve", lambda e: e.tensor_tensor(out=acc[:], in0=acc[:], in1=tmp[:], op=ALU.add),
                                    [t_])
                    else:
                        t_ = r.op("dve", lambda e, s_=s_, bb=bb: e.tensor_tensor(
                            out=tmp[:], in0=sg[:, s_, :], in1=ps[:, 2 + bb, :], op=ALU.mult), [tsg, tmb, tacc])
                        tacc = r.op("dve", lambda e, o=o, tsl=tsl: e.tensor_tensor(
                            out=ob[:, o, tsl], in0=acc[:], in1=tmp[:], op=ALU.add), [t_] + odeps)
                    sring.read(s_, tacc if b == 0 else t_)
                    bring.read(bb, tacc if b == 0 else t_)
            wring.read(ws, tlastmm)
            ts = r.dma("sp", lambda e, o=o, mt=mt: e.dma_start(out=MTo[mt * 128:(mt + 1) * 128, :], in_=ob[:, o, :]),
                       [tacc], oring.sems[o])
            oring.read(o, ts)
        r.emit()


CONST_SPECS = None


class LazyInputs:
    def __init__(self, nc, L, consts):
        import ml_dtypes
        self.nc = nc
        self.decl = {}
        sp = {}
        sp["xT"] = ([D, S], F32)
        sp["rb"] = ([1, 256], F32)
        for k, v in consts.items():
            sp["c_" + k] = (list(v.shape), BF16 if v.dtype == ml_dtypes.bfloat16 else F32)
        for n, shp in [("g1c", [L, 128, KC]), ("g2c", [L, 128, KC]), ("gfc", [128, KC]),
                       ("win_t", [L, 53, 128, D]), ("bin_c", [L, 128, 53]), ("wgd_t", [L, 8, 128, D]),
                       ("conv_c", [L, 128, 32]), ("mgain", [L, 128, 4]), ("rgain", [L, 128, 4]),
                       ("w1", [L, 2, 128, 4096]), ("w2", [L, 2, 128, 128]), ("posT", [L, 128, 2, 32]),
                       ("wgu_t", [L, 96, 128, 1024]), ("bg_c", [L, 128, 96]),
                       ("wbrm_t", [L, 32, 128, 512]), ("wbrn_t", [L, 32, 128, 1024]), ("wbrr_t", [L, 32, 128, 512]),
                       ("wout_t", [L, 32, 128, D]), ("wup_t", [L, 128, 128, D]), ("wdn", [L, 4 * D, D])]:
            sp[n] = (shp, F32)
        self.sp = sp

    def __getitem__(self, name):
        if name not in self.decl:
            shp, dt = self.sp[name]
            self.decl[name] = self.nc.dram_tensor(name, list(shp), dt, kind="ExternalInput").ap()
        return self.decl[name]


def declare_inputs(nc, L, consts):
    return LazyInputs(nc, L, consts)


def build_program(L, consts, debug=False, upto=99):
    nc = bass.Bass("TRN2", target_bir_lowering=False)
    I = declare_inputs(nc, L, consts)
    yT = nc.dram_tensor("yT", [D, S], F32, kind="ExternalOutput").ap()
    kind = "ExternalOutput" if debug else "Internal"
    XA = nc.dram_tensor("XA", [D, S], F32, kind=kind).ap()
    XB = nc.dram_tensor("XB", [D, S], F32, kind=kind).ap()
    HT = nc.dram_tensor("HT", [D, S], BF16).ap()
    ZT = nc.dram_tensor("ZT", [ZROWS, S], F32).ap()
    GL = nc.dram_tensor("GL", [1024, S], BF16).ap()
    YT = nc.dram_tensor("YT", [2048, S], BF16, kind=kind).ap()
    MT = nc.dram_tensor("MT", [D, S], BF16).ap()
    TT = nc.dram_tensor("TT", [8, 128, 1408], BF16).ap()
    TTW = nc.dram_tensor("TTW", [8, 128, 1408], BF16).ap()
    BC = nc.dram_tensor("BC", [8, 128, S], BF16).ap()
    class _C(dict):
        def __missing__(self, k):
            return I["c_" + k]
    cst = _C()
    ctx = Ctx(nc)
    with nc.psum_tensor("ps", [128, 8, 512], F32) as ps:
        ctx.psum = ps
        step = [0]

        def go():
            step[0] += 1
            return step[0] <= upto
        if go():
            phase_setup(ctx, I["rb"], TT, TTW, BC)
        xcur = I["xT"]
        for l in range(L):
            if go() and not os.environ.get("SKIP2"):
                phase_norm(ctx, xcur, HT, I["g1c"][l])
            if go() and not os.environ.get("SKIP3"):
                jobs = [dict(w=I["win_t"][l, m], kind="z", bias=m, out=ZT[m * 128:(m + 1) * 128, :]) for m in range(53)]
                jobs += [dict(w=I["wgd_t"][l, m], kind="bf", out=GL[m * 128:(m + 1) * 128, :]) for m in range(8)]
                phase_linear(ctx, HT, D, jobs, bias_cols=I["bin_c"][l], nbias=53)
            if go() and not os.environ.get("SKIP4"):
                phase_decay(ctx, "mlstm", ZT, YT, 0, 4, 8, 12, 0, cst, dict(gain=I["mgain"][l], conv_cols=I["conv_c"][l]))
            if go() and not os.environ.get("SKIP5"):
                phase_decay(ctx, "ret", ZT, YT, 36, 40, 44, 48, 1536, cst, dict(gain=I["rgain"][l]))
            if go():
                phase_nsa(ctx, ZT, YT, cst, dict(w1=I["w1"][l], w2=I["w2"][l], posT=I["posT"][l]), TT, TTW, BC)
            if go():
                phase_merge(ctx, GL, YT, MT, I["wgu_t"][l], [I["wbrm_t"][l], I["wbrn_t"][l], I["wbrr_t"][l]], I["bg_c"][l])
            if go():
                jobs = [dict(w=I["wout_t"][l, m], kind="res", resid=xcur[m * 128:(m + 1) * 128, :],
                             out=XA[m * 128:(m + 1) * 128, :]) for m in range(KC)]
                phase_linear(ctx, MT, D, jobs)
            if go():
                phase_norm(ctx, XA, HT, I["g2c"][l])
            if go():
                phase_mlp(ctx, HT, XA, XB, I["wup_t"][l], I["wdn"][l])
            xcur = XB
        if go():
            phase_norm(ctx, xcur, yT, I["gfc"], out_f32=True)
    nc._lazy_inputs = I
    return nc


def prep_weights(inp, L, layers=None):
    layers = list(range(L)) if layers is None else layers
    cm, _ = in_colmap()
    W = {}
    W["rb"] = np.ascontiguousarray(inp["rel_bias"].reshape(1, 256))
    W["g1c"] = np.stack([cols_layout(inp["norm_mix_g"][l]) for l in layers])
    W["g2c"] = np.stack([cols_layout(inp["norm_mlp_g"][l]) for l in layers])
    W["gfc"] = cols_layout(inp["final_norm_g"])
    W["win_t"] = np.stack([tile_w(permute_cols(inp["w_in"][l], cm)) for l in layers])
    W["bin_c"] = np.stack([cols_layout(permute_cols(inp["b_in"][l], cm)) for l in layers])
    W["wgd_t"] = np.stack([tile_w(inp["w_gate_down"][l]) for l in layers])
    W["conv_c"] = np.stack([np.ascontiguousarray(inp["conv_qk"][l].reshape(4, 8, 128).transpose(2, 0, 1).reshape(128, 32))
                            for l in layers])
    W["mgain"] = np.stack([cols_layout(inp["mlstm_norm_g"][l]) for l in layers])
    W["rgain"] = np.stack([cols_layout(inp["ret_norm_g"][l]) for l in layers])
    W["w1"] = np.stack([np.stack([np.ascontiguousarray(inp[k][l].reshape(32, 128, 128).transpose(1, 0, 2)).reshape(128, 4096)
                                  for k in ("cmp_w1_k", "cmp_w1_v")]) for l in layers])
    W["w2"] = np.stack([np.stack([inp["cmp_w2_k"][l], inp["cmp_w2_v"][l]]) for l in layers])
    W["posT"] = np.stack([np.ascontiguousarray(np.stack([inp["cmp_pos_k"][l].T, inp["cmp_pos_v"][l].T], 1))
                          for l in layers])
    W["wgu_t"] = np.stack([tile_w(inp["w_gate_up"][l]) for l in layers])
    W["bg_c"] = np.stack([cols_layout(inp["b_gate"][l]) for l in layers])
    W["wbrm_t"] = np.stack([tile_w(inp["w_br_mlstm"][l]) for l in layers])
    W["wbrn_t"] = np.stack([tile_w(inp["w_br_nsa"][l]) for l in layers])
    W["wbrr_t"] = np.stack([tile_w(inp["w_br_ret"][l]) for l in layers])
    W["wout_t"] = np.stack([tile_w(inp["w_out"][l]) for l in layers])
    W["wup_t"] = np.stack([tile_w(inp["w_up"][l]) for l in layers])
    W["wdn"] = np.stack([np.ascontiguousarray(inp["w_down"][l]) for l in layers])
    return W


import ml_dtypes
def make_consts():
    c={}
    sel=np.zeros((64,4*128),np.float32)
    for h in range(4): sel[32+h,h*128:(h+1)*128]=1.0
    c["sel64"]=sel
    c["i64"]=np.eye(64,dtype=np.float32)
    c["ident_bf"]=np.eye(128,dtype=np.float32).astype(ml_dtypes.bfloat16)
    ik=np.arange(128)[:,None]; x=np.arange(896)[None,:]
    c["U_bf"]=((x-384-ik)>=0).astype(np.float32).astype(ml_dtypes.bfloat16)
    G=np.zeros((64,S),np.float32)
    lg=np.log1p(-np.exp2(-5.0-np.arange(4,dtype=np.float32))).astype(np.float32)
    G[32:36,:]=lg[:,None]*np.arange(S,dtype=np.float32)[None,:]
    c["ret_G"]=G
    half=64
    inv_freq=(1.0/(10000.0**np.linspace(0.0,1.0,half,dtype=np.float32))).astype(np.float32)
    ang=np.arange(S,dtype=np.float32)[:,None]*inv_freq[None,:]
    cos=np.cos(ang).astype(np.float32).T; sin=np.sin(ang).astype(np.float32).T
    c["cosT"]=np.ascontiguousarray(np.concatenate([cos,cos],0)); c["sinT"]=np.ascontiguousarray(np.concatenate([sin,sin],0))
    R=np.zeros((128,128),np.float32)
    for m in range(64): R[m+64,m]=-1.0
    for m in range(64,128): R[m-64,m]=1.0
    c["rotT"]=R
    return c
def make_consts_nsa(c):
    k=np.arange(S)[None,:]; s=np.arange(32)[:,None]
    c["expand_bf"]=((k//64)==s).astype(np.float32).astype(ml_dtypes.bfloat16)
    t=(np.arange(16)[None,:,None]*128+np.arange(128)[:,None,None]); cur=t//64; sb=np.arange(32)[None,None,:]
    F=np.zeros((128,16,32),np.float32)
    F[sb>cur]=-1e4
    F[(sb==0)|(sb==cur)|(sb==cur-1)]=1e4
    c["force"]=F
    j=np.arange(128)[:,None]; ss=np.arange(32)[None,:]
    ov=((16*j<64*ss+64)&(16*j+32>64*ss)&(j<=126)).astype(np.float32)
    c["ov_bf"]=ov.astype(ml_dtypes.bfloat16)
    sg=np.zeros((32,24*128),np.float32)
    for i in range(24): sg[i,i*128:(i+1)*128]=1.0
    c["selg"]=sg
    return c

N_CORES = 8
DEPTH = 4


def kernel(**inputs):
    consts = make_consts_nsa(make_consts())
    nc = build_program(DEPTH, consts)
    W = prep_weights(inputs, DEPTH)
    x = np.asarray(inputs["x"], dtype=np.float32)
    decl = nc._lazy_inputs.decl
    base = dict(W)
    for k, v in consts.items():
        base["c_" + k] = v
    in_maps = []
    for b in range(N_CORES):
        m = {k: v for k, v in base.items() if k in decl}
        m["xT"] = np.ascontiguousarray(x[b].T)
        in_maps.append(m)
    res = run_bass_kernel_spmd(nc, in_maps, core_ids=list(range(N_CORES)))
    out = np.stack([np.ascontiguousarray(np.asarray(res.results[b]["yT"]).T) for b in range(N_CORES)])
    return out.astype(np.float32)
```

```python
import numpy as np
import os
from contextlib import ExitStack
import concourse.bass as bass
import concourse.mybir as mybir
from concourse.bass_utils import run_bass_kernel_spmd

F32 = mybir.dt.float32
BF16 = mybir.dt.bfloat16
AF = mybir.ActivationFunctionType
ALU = mybir.AluOpType
AX = mybir.AxisListType

S = 2048
D = 4096
KC = D // 128
TB = 512
NTB = S // TB
EPS = 1e-6
ZROWS = 53 * 128


ENGS = ["pe", "act", "dve", "pool", "sp"]
_UID = [0]


def _sbt(nc, name, shape, dt):
    _UID[0] += 1
    return nc.sbuf_tensor(f"{name}_{_UID[0]}", shape, dt)


class Ctx:
    def __init__(self, nc, n_hw=52, n_sw=40):
        self.nc = nc
        self.esem = {e: nc.alloc_semaphore(name=f"eng_{e}") for e in ENGS[:4]}
        self.hw = [nc.alloc_semaphore(name=f"hw_{i}") for i in range(n_hw)]
        self.sw = [nc.alloc_semaphore(name=f"sw_{i}") for i in range(n_sw)]
        self.base = {}
        self.psum = None


class Rec:
    def __init__(self, ctx):
        self.ctx = ctx
        self.q = {e: [] for e in ENGS}
        self.cnt = {e: 0 for e in ENGS[:4]}
        self.dcnt = {}
        self.nsem = {"hw": 0, "sw": 0}
        self.pending = None

    def dsem(self, sw=False):
        kind = "sw" if sw else "hw"
        idx = self.nsem[kind]
        self.nsem[kind] += 1
        pool = self.ctx.sw if sw else self.ctx.hw
        assert idx < len(pool), f"out of {kind} dma sems"
        k = (kind, idx)
        self.dcnt[k] = self.ctx.base.get(k, 0)
        return k

    def op(self, eng, fn, deps=(), inc=True):
        deps = tuple(d for d in deps if d is not None)
        self.q[eng].append(("op", fn, deps, inc))
        if inc:
            self.cnt[eng] += 1
            return (eng, self.cnt[eng])
        return None

    def dma(self, eng, fn, deps, semkey):
        deps = tuple(d for d in deps if d is not None)
        assert (semkey[0] == "sw") == (eng == "pool"), (eng, semkey)
        self.q[eng].append(("dma", fn, deps, semkey))
        self.dcnt[semkey] += 16
        return (semkey, self.dcnt[semkey])

    def wait(self, eng, deps):
        deps = tuple(d for d in deps if d is not None)
        self.q[eng].append(("wait", None, deps, None))

    def _sem(self, k):
        if isinstance(k, tuple):
            return (self.ctx.sw if k[0] == "sw" else self.ctx.hw)[k[1]]
        return self.ctx.esem[k]

    def check(self):
        pos = {e: 0 for e in ENGS}
        val = {k: self.ctx.base.get(k, 0) for k in self.dcnt}
        progress = True
        while progress:
            progress = False
            for e in ENGS:
                q = self.q[e]
                while pos[e] < len(q):
                    kind, fn, deps, x = q[pos[e]]
                    if any(val.get(k, 0) < v for (k, v) in deps):
                        break
                    if kind == "op" and x:
                        val[e] = val.get(e, 0) + 1
                    elif kind == "dma":
                        val[x] = val.get(x, 0) + 16
                    pos[e] += 1
                    progress = True
        stuck = {e: (pos[e], len(self.q[e])) for e in ENGS if pos[e] < len(self.q[e])}
        if stuck:
            msg = []
            for e, (p, n) in stuck.items():
                kind, fn, deps, x = self.q[e][p]
                msg.append(f"{e}@{p}/{n} waits {[(k, v, val.get(k, 0)) for k, v in deps if val.get(k, 0) < v]}")
            raise RuntimeError("DEADLOCK in recorded schedule: " + "; ".join(msg))

    def emit(self):
        nc = self.ctx.nc
        self.check()
        self.wait("sp", [(k, v) for k, v in self.dcnt.items() if v > self.ctx.base.get(k, 0)])
        with nc.Block() as block:
            for name in ENGS:
                items = self.q[name]
                if not items:
                    continue

                def run(e, items=items, name=name):
                    known = {}
                    for kind, fn, deps, x in items:
                        for (k, v) in deps:
                            if known.get(k, 0) < v:
                                e.wait_ge(self._sem(k), v)
                                known[k] = v
                        if kind == "wait":
                            continue
                        ins = fn(e)
                        if kind == "op":
                            if x:
                                ins.then_inc(self._sem(name), 1)
                        else:
                            ins.then_inc(self._sem(x), 16)

                {"pe": block.tensor, "act": block.scalar, "dve": block.vector,
                 "pool": block.gpsimd, "sp": block.sync}[name](run)
        used = [self.ctx.esem[e] for e in ENGS[:4] if self.cnt[e] > 0]
        for k, v in self.dcnt.items():
            self.ctx.base[k] = v
        if used:
            nc.all_engine_barrier()
            with nc.Block() as block:
                def clr(e):
                    for s in used:
                        e.sem_clear(s)
                block.gpsimd(clr)
            nc.all_engine_barrier()


class Ring:
    def __init__(self, rec, n, with_sems=True, sw=False):
        self.n = n
        self.sems = [rec.dsem(sw) for _ in range(n)] if with_sems else None
        self.readers = [[] for _ in range(n)]
        self.i = 0

    def next(self):
        s = self.i % self.n
        self.i += 1
        deps = self.readers[s]
        self.readers[s] = []
        return s, deps

    def read(self, s, ticket):
        if ticket is not None:
            self.readers[s].append(ticket)


def phase_norm(ctx, xT, hT, g_cols, out_f32=False):
    nc = ctx.nc
    CG = 8
    NG = KC // CG
    odt = F32 if out_f32 else BF16
    with (_sbt(nc, "n_x", [128, 2, KC, TB], F32) as xb,
          _sbt(nc, "n_sq", [128, 2, CG, TB], BF16) as sq,
          _sbt(nc, "n_o", [128, 2, CG, TB], odt) as ob,
          _sbt(nc, "n_rstd", [128, S], F32) as rstd,
          _sbt(nc, "n_g", [128, KC], F32) as gc,
          _sbt(nc, "n_ones", [128, 128], BF16) as ones):
        r = Rec(ctx)
        ps = ctx.psum
        csem = r.dsem()
        tg = r.dma("sp", lambda e: e.dma_start(out=gc[:], in_=g_cols), [], csem)
        tones = r.op("pool", lambda e: e.memset(ones[:], 1.0))
        xsem = [[r.dsem() for _ in range(NG)] for _ in range(2)]
        sqring = Ring(r, 2, with_sems=False)
        oring = Ring(r, 2)
        xv = xT.rearrange("(c p) t -> p c t", p=128)
        hv = hT.rearrange("(c p) t -> p c t", p=128)
        xreaders = [[], []]

        def loads(tb):
            s = tb % 2
            return [r.dma("sp", lambda e, g=g, s=s, tb=tb: e.dma_start(
                out=xb[:, s, g * CG:(g + 1) * CG, :], in_=xv[:, g * CG:(g + 1) * CG, tb * TB:(tb + 1) * TB]),
                xreaders[s], xsem[s][g]) for g in range(NG)]
        tl_next = loads(0)
        for tb in range(NTB):
            s = tb % 2
            tl = tl_next
            if tb + 1 < NTB:
                tl_next = loads(tb + 1)
            tm = None
            for g in range(NG):
                q, qdeps = sqring.next()
                ta = r.op("act", lambda e, g=g, q=q, s=s: e.activation(out=sq[:, q], in_=xb[:, s, g * CG:(g + 1) * CG, :],
                                                                 func=AF.Square), [tl[g]] + qdeps)
                for c in range(CG):
                    last = (c == CG - 1)
                    tm = r.op("pe", lambda e, q=q, c=c, g=g, tb=tb: e.matmul(
                        ps[:, tb % 4, :], ones[:], sq[:, q, c, :], start=(g == 0 and c == 0),
                        stop=(g == NG - 1 and c == CG - 1)), [ta, tones] if c == 0 else [], inc=last)
                sqring.read(q, tm)
            tsl = slice(tb * TB, (tb + 1) * TB)
            t1 = r.op("act", lambda e, tsl=tsl, tb=tb: e.activation(out=rstd[:, tsl], in_=ps[:, tb % 4, :], func=AF.Sqrt, bias=EPS,
                                                   scale=1.0 / D), [tm])
            t2 = r.op("dve", lambda e, tsl=tsl: e.reciprocal(out=rstd[:, tsl], in_=rstd[:, tsl]), [t1])
            tv = None
            for g in range(NG):
                o, odeps = oring.next()
                for c in range(CG):
                    cc = g * CG + c
                    tv = r.op("dve", lambda e, o=o, c=c, cc=cc, s=s, tsl=tsl: e.scalar_tensor_tensor(
                        out=ob[:, o, c, :], in0=xb[:, s, cc, :], scalar=gc[:, cc:cc + 1], in1=rstd[:, tsl],
                        op0=ALU.mult, op1=ALU.mult), [tg, t2] + odeps)
                ts = r.dma("sp", lambda e, o=o, g=g, tb=tb: e.dma_start(
                    out=hv[:, g * CG:(g + 1) * CG, tb * TB:(tb + 1) * TB], in_=ob[:, o]), [tv], oring.sems[o])
                oring.read(o, ts)
            xreaders[s] = [tv]
        r.emit()


_EPS = {}


def EPS_AP(ctx):
    return _EPS["ap"]


def phase_linear(ctx, inT, K, jobs, bias_cols=None, nbias=0):
    nc = ctx.nc
    KCk = K // 128
    with (_sbt(nc, "l_in", [128, KCk, S], BF16) as xin,
          _sbt(nc, "l_w", [128, 3, KCk * 128], BF16) as wb,
          _sbt(nc, "l_sf", [128, 2, S], F32) as sf,
          _sbt(nc, "l_sb", [128, 2, S], BF16) as sb,
          _sbt(nc, "l_bias", [128, max(nbias, 1)], F32) as bc):
        r = Rec(ctx)
        ps = ctx.psum
        csem = r.dsem()
        tb_ = None
        if nbias:
            tb_ = r.dma("sp", lambda e: e.dma_start(out=bc[:], in_=bias_cols), [], csem)
        iv = inT.rearrange("(c p) t -> p c t", p=128)
        tin = []
        CG = 8 if KCk >= 8 else KCk
        for g in range(KCk // CG):
            k = r.dsem()
            tin.append(r.dma("sp", lambda e, g=g: e.dma_start(out=xin[:, g * CG:(g + 1) * CG, :],
                                                             in_=iv[:, g * CG:(g + 1) * CG, :]), [], k))
        wring = Ring(r, 3, sw=True)
        pring = Ring(r, 2, with_sems=False)
        fring = Ring(r, 2)
        bring = Ring(r, 2)
        rsem = [r.dsem(), r.dsem()]
        for m, job in enumerate(jobs):
            ws, wdeps = wring.next()
            tw = r.dma("pool", lambda e, ws=ws, job=job: e.dma_start(out=wb[:, ws, :], in_=job["w"]), wdeps,
                       wring.sems[ws])
            pset, pdeps = pring.next()
            tm = None
            for c in range(KCk):
                for tb in range(NTB):
                    first = (c == 0 and tb == 0)
                    last = (c == KCk - 1 and tb == NTB - 1)
                    deps = []
                    if first:
                        deps = [tw] + pdeps + (tin if m == 0 else [])
                    tm = r.op("pe", lambda e, ws=ws, c=c, tb=tb, pset=pset: e.matmul(
                        ps[:, pset * 4 + tb, :], wb[:, ws, c * 128:(c + 1) * 128], xin[:, c, tb * TB:(tb + 1) * TB],
                        start=(c == 0), stop=(c == KCk - 1)), deps, inc=last)
            wring.read(ws, tm)
            psv = ps[:, pset * 4:(pset + 1) * 4, :]
            kind = job["kind"]
            if kind == "z":
                f, fdeps = fring.next()
                b = job["bias"]
                te = r.op("act", lambda e, f=f, b=b, psv=psv: e.activation(
                    out=sf[:, f, :].rearrange("p (a t) -> p a t", a=4), in_=psv, func=AF.Identity,
                    bias=bc[:, b:b + 1], scale=1.0), [tm, tb_] + fdeps)
                pring.read(pset, te)
                ts = r.dma("sp", lambda e, f=f, job=job: e.dma_start(out=job["out"], in_=sf[:, f, :]), [te],
                           fring.sems[f])
                fring.read(f, ts)
            elif kind == "bf":
                f, fdeps = bring.next()
                te = r.op("act", lambda e, f=f, psv=psv: e.activation(
                    out=sb[:, f, :].rearrange("p (a t) -> p a t", a=4), in_=psv, func=AF.Copy), [tm] + fdeps)
                pring.read(pset, te)
                ts = r.dma("sp", lambda e, f=f, job=job: e.dma_start(out=job["out"], in_=sb[:, f, :]), [te],
                           bring.sems[f])
                bring.read(f, ts)
            elif kind == "res":
                f, fdeps = fring.next()
                tr = r.dma("sp", lambda e, f=f, job=job: e.dma_start(out=sf[:, f, :], in_=job["resid"]), fdeps,
                           rsem[f])
                te = r.op("dve", lambda e, f=f, psv=psv: e.tensor_tensor(
                    out=sf[:, f, :].rearrange("p (a t) -> p a t", a=4),
                    in0=sf[:, f, :].rearrange("p (a t) -> p a t", a=4), in1=psv, op=ALU.add), [tm, tr])
                pring.read(pset, te)
                ts = r.dma("sp", lambda e, f=f, job=job: e.dma_start(out=job["out"], in_=sf[:, f, :]), [te],
                           fring.sems[f])
                fring.read(f, ts)
        r.emit()


def phase_mlp(ctx, hT, xin, xout, wup, wdn, n_ft=128):
    nc = ctx.nc
    G = 2
    NGR = n_ft // G
    with (_sbt(nc, "f_h", [128, KC, TB], BF16) as hb,
          _sbt(nc, "f_x", [128, KC, TB], F32) as xb,
          _sbt(nc, "f_wu", [128, 2, G, KC * 128], BF16) as wu,
          _sbt(nc, "f_wd", [128, 2, G, D], BF16) as wd,
          _sbt(nc, "f_a", [128, 2, G, TB], BF16) as ab):
        r = Rec(ctx)
        ps = ctx.psum
        hv = hT.rearrange("(c p) t -> p c t", p=128)
        xiv = xin.rearrange("(c p) t -> p c t", p=128)
        xov = xout.rearrange("(c p) t -> p c t", p=128)
        wdv = wdn.rearrange("(f p) m -> p f m", p=128)
        wring = Ring(r, 2, sw=True)
        dsems = [r.dsem(True), r.dsem(True)]
        hsem = r.dsem()
        xsem = r.dsem()
        osem = r.dsem()
        aring = Ring(r, 2, with_sems=False)
        upring = Ring(r, 2, with_sems=False)
        dnring = Ring(r, 4, with_sems=False)
        last_store = None
        last_x_readers = []
        last_h_readers = []
        for tb in range(NTB):
            th = r.dma("sp", lambda e, tb=tb: e.dma_start(out=hb[:], in_=hv[:, :, tb * TB:(tb + 1) * TB]),
                       last_h_readers, hsem)
            tx = r.dma("sp", lambda e, tb=tb: e.dma_start(out=xb[:], in_=xiv[:, :, tb * TB:(tb + 1) * TB]),
                       [last_store], xsem)
            tacc = None
            for gr in range(NGR):
                ws, wdeps = wring.next()
                tw = r.dma("pool", lambda e, ws=ws, gr=gr: e.dma_start(
                    out=wu[:, ws], in_=wup[gr * G:(gr + 1) * G].rearrange("g p k -> p g k")), wdeps, wring.sems[ws])
                tw2 = r.dma("pool", lambda e, ws=ws, gr=gr: e.dma_start(
                    out=wd[:, ws], in_=wdv[:, gr * G:(gr + 1) * G, :]), wdeps, dsems[ws])
                a, adeps = aring.next()
                tas = []
                for g in range(G):
                    pb, pdeps = upring.next()
                    tm = None
                    for c in range(KC):
                        deps = ([tw, th] + pdeps) if c == 0 else []
                        tm = r.op("pe", lambda e, ws=ws, g=g, c=c, pb=pb: e.matmul(
                            ps[:, pb, :], wu[:, ws, g, c * 128:(c + 1) * 128], hb[:, c, :],
                            start=(c == 0), stop=(c == KC - 1)), deps, inc=(c == KC - 1))
                    t1 = r.op("act", lambda e, a=a, g=g, pb=pb: e.activation(
                        out=ab[:, a, g, :], in_=ps[:, pb, :], func=AF.Relu), [tm] + adeps)
                    upring.read(pb, t1)
                    t2 = r.op("act", lambda e, a=a, g=g: e.activation(
                        out=ab[:, a, g, :], in_=ab[:, a, g, :], func=AF.Square), [t1])
                    tas.append(t2)
                tmd = None
                for mt in range(KC):
                    db, ddeps = dnring.next()
                    for g in range(G):
                        deps = (tas + [tw2] + ddeps) if g == 0 else []
                        tmd = r.op("pe", lambda e, ws=ws, g=g, mt=mt, a=a, db=db: e.matmul(
                            ps[:, 4 + db, :], wd[:, ws, g, mt * 128:(mt + 1) * 128], ab[:, a, g, :],
                            start=(g == 0), stop=(g == G - 1)), deps, inc=(g == G - 1))
                    tacc = r.op("dve", lambda e, mt=mt, db=db: e.tensor_tensor(
                        out=xb[:, mt, :], in0=xb[:, mt, :], in1=ps[:, 4 + db, :], op=ALU.add), [tmd, tx])
                    dnring.read(db, tacc)
                wring.read(ws, tmd)
                aring.read(a, tmd)
            last_h_readers = [tmd]
            last_store = r.dma("sp", lambda e, tb=tb: e.dma_start(out=xov[:, :, tb * TB:(tb + 1) * TB], in_=xb[:]),
                               [tacc], osem)
        r.emit()


def tile_w(w):
    K, M = w.shape
    return np.ascontiguousarray(w.reshape(K // 128, 128, M // 128, 128).transpose(2, 1, 0, 3)).reshape(
        M // 128, 128, (K // 128) * 128)


def cols_layout(v):
    return np.ascontiguousarray(v.reshape(-1, 128).T)


def in_colmap():
    off = {}
    o = 0
    for name, sz in [("m_q", 512), ("m_k", 512), ("m_v", 512), ("m_o", 512), ("m_i", 4), ("m_f", 4),
                     ("n_q", 1024), ("n_kc", 256), ("n_vc", 256), ("n_ks", 256), ("n_vs", 256),
                     ("n_kw", 256), ("n_vw", 256), ("n_gate", 24), ("r_q", 512), ("r_k", 512),
                     ("r_v", 512), ("r_g", 512)]:
        off[name] = (o, sz)
        o += sz
    order = ["m_q", "m_k", "m_v", "m_o", "n_q", "n_kc", "n_vc", "n_ks", "n_vs", "n_kw", "n_vw",
             "r_q", "r_k", "r_v", "r_g"]
    cm = []
    zoff = {}
    for n in order:
        zoff[n] = len(cm)
        cm += list(range(off[n][0], off[n][0] + off[n][1]))
    small = [-1] * 128
    for i in range(4):
        small[i] = off["m_i"][0] + i
        small[32 + i] = off["m_f"][0] + i
    for i in range(24):
        small[64 + i] = off["n_gate"][0] + i
    zoff["small"] = len(cm)
    cm += small
    return np.array(cm), zoff


def permute_cols(w, cm):
    out = np.zeros(w.shape[:-1] + (len(cm),), w.dtype)
    valid = cm >= 0
    out[..., valid] = w[..., cm[valid]]
    return out


LN_SCALE = -0.5 * float(np.log(128.0))


def phase_decay(ctx, mode, ZT, YT, zq, zk, zv, zg, yrow, cst, par):
    nc = ctx.nc
    ml = (mode == "mlstm")
    NH = 4
    with ExitStack() as _st:
        G = _st.enter_context(_sbt(nc, "d_G", [64, S], F32))
        Gt = _st.enter_context(_sbt(nc, "d_tmp", [64, S], F32))
        G1 = _st.enter_context(_sbt(nc, "d_one64", [64, S], F32))
        Ab = _st.enter_context(_sbt(nc, "d_Ab", [128, NH, S], F32))
        bcol = _st.enter_context(_sbt(nc, "d_bcol", [128, 16, NH], F32))
        sel = _st.enter_context(_sbt(nc, "d_sel", [64, NH * 128], F32))
        i64 = _st.enter_context(_sbt(nc, "d_i64", [64, 64], F32))
        ident = _st.enter_context(_sbt(nc, "d_ident", [128, 128], BF16))
        onesb = _st.enter_context(_sbt(nc, "d_onesb", [128, 128], BF16))
        avg = _st.enter_context(_sbt(nc, "d_avg", [128, 128], BF16))
        U = _st.enter_context(_sbt(nc, "d_U", [128, 896], BF16))
        xf = _st.enter_context(_sbt(nc, "d_x", [128, 2, S], F32))
        acc = _st.enter_context(_sbt(nc, "d_acc", [128, S], F32))
        qT = _st.enter_context(_sbt(nc, "d_qT", [128, S], BF16))
        kT = _st.enter_context(_sbt(nc, "d_kT", [128, S], BF16))
        vT = _st.enter_context(_sbt(nc, "d_vT", [128, S], BF16))
        V = _st.enter_context(_sbt(nc, "d_V", [128, 16, 128], BF16))
        go = _st.enter_context(_sbt(nc, "d_go", [128, S], F32))
        cw = _st.enter_context(_sbt(nc, "d_cw", [128, 32], F32))
        cs = _st.enter_context(_sbt(nc, "d_cs", [128, 2, S], F32))
        rt = _st.enter_context(_sbt(nc, "d_rt", [128, 128], F32))
        gain = _st.enter_context(_sbt(nc, "d_gain", [128, NH], F32))
        Dt = _st.enter_context(_sbt(nc, "d_Dt", [128, 2, TB], F32))
        P = _st.enter_context(_sbt(nc, "d_P", [128, 2, TB], BF16))
        e1 = _st.enter_context(_sbt(nc, "d_e1", [128, TB], F32))
        e2 = _st.enter_context(_sbt(nc, "d_e2", [128, TB], F32))
        e3 = _st.enter_context(_sbt(nc, "d_e3", [128, TB], BF16))
        yb = _st.enter_context(_sbt(nc, "d_y", [128, S], BF16))
        r = Rec(ctx)
        ps = ctx.psum
        ld = lambda dst, src, deps=(): r.dma("sp", lambda e: e.dma_start(out=dst, in_=src), list(deps), r.dsem())
        t_sel = ld(sel[:], cst["sel64"])
        t_i64 = ld(i64[:], cst["i64"])
        t_id = ld(ident[:], cst["ident_bf"])
        t_U = ld(U[:], cst["U_bf"])
        t_on = r.op("pool", lambda e: e.memset(onesb[:], 1.0))
        t_av = r.op("pool", lambda e: e.memset(avg[:], 1.0 / 128.0))
        t_gain = ld(gain[:], par["gain"])
        if ml:
            t_g = ld(G[:], ZT[52 * 128:52 * 128 + 64, :])
            a1 = r.op("act", lambda e: e.activation(out=Gt[32:36, :], in_=G[32:36, :], func=AF.Exp, scale=-1.0), [t_g])
            a2 = r.op("act", lambda e: e.activation(out=Gt[32:36, :], in_=Gt[32:36, :], func=AF.Ln, bias=1.0,
                                                   scale=1.0), [a1])
            a3 = r.op("pool", lambda e: e.memset(G1[32:36, :], 1.0), [])
            t_G = r.op("dve", lambda e: e.tensor_tensor_scan(out=G[32:36, :], data0=G1[32:36, :], data1=Gt[32:36, :],
                                                            initial=0.0, op0=ALU.mult, op1=ALU.subtract), [a2, a3])
        else:
            t_G = ld(G[:], cst["ret_G"])
        tcol = None
        for tt in range(16):
            tcol = r.op("pe", lambda e, tt=tt: e.matmul(
                ps[:, 0:2, :].rearrange("p a (b c) -> p (a b) c", c=64)[:, tt, :], G[:, tt * 128:(tt + 1) * 128],
                i64[:], start=True, stop=True), [t_G, t_i64] if tt == 0 else [], inc=(tt == 15))
        colv = ps[:, 0:2, :].rearrange("p a (b c) -> p (a b) c", c=64)
        t_b0 = r.op("act", lambda e: e.activation(out=bcol[:], in_=colv[:, :, 0:4], func=AF.Copy), [tcol])
        t_b1 = r.op("dve", lambda e: e.tensor_tensor(out=bcol[:], in0=bcol[:], in1=colv[:, :, 32:36],
                                                    op=ALU.subtract), [t_b0])
        t_bc = r.op("dve", lambda e: e.tensor_scalar_add(out=bcol[:], in0=bcol[:], scalar1=LN_SCALE), [t_b1])
        t_ab = []
        bank = 2
        for h in range(NH):
            for tb in range(NTB):
                b = 2 + ((h * NTB + tb) % 4)
                tm = r.op("pe", lambda e, h=h, tb=tb, b=b: e.matmul(
                    ps[:, b, :], sel[:, h * 128:(h + 1) * 128], G[:, tb * TB:(tb + 1) * TB], start=True, stop=True),
                    [t_G, t_sel] + ([t_ab[-4]] if len(t_ab) >= 4 else []))
                tc = r.op("act", lambda e, h=h, tb=tb, b=b: e.activation(
                    out=Ab[:, h, tb * TB:(tb + 1) * TB], in_=ps[:, b, :], func=AF.Copy), [tm])
                t_ab.append(tc)
        t_ab_all = t_ab[-1]
        if ml:
            t_cw = ld(cw[:], par["conv_cols"])
        else:
            t_cos = ld(cs[:, 0, :], cst["cosT"])
            t_sin = ld(cs[:, 1, :], cst["sinT"])
            t_rt = ld(rt[:], cst["rotT"])
        prev_head_done = []
        xr_readers = [[], []]
        ty = None
        dst_ = {"blk": 0, "bank_free": [None, None], "ty": None}
        for h in range(NH):
            tq = []
            for wi, (zt, dst) in enumerate([(zq + h, qT), (zk + h, kT)]):
                tl = ld(xf[:, wi, :], ZT[zt * 128:(zt + 1) * 128, :], xr_readers[wi] + prev_head_done)
                if ml:
                    j = wi * 4 + h
                    t0 = r.op("dve", lambda e, wi=wi, j=j: e.tensor_scalar(
                        out=acc[:], in0=xf[:, wi, :], scalar1=cw[:, 3 * 8 + j:3 * 8 + j + 1], scalar2=None,
                        op0=ALU.mult), [tl, t_cw] + tq + prev_head_done)
                    for sft in (1, 2, 3):
                        t0 = r.op("dve", lambda e, wi=wi, j=j, sft=sft: e.scalar_tensor_tensor(
                            out=acc[:, sft:], in0=xf[:, wi, 0:S - sft],
                            scalar=cw[:, (3 - sft) * 8 + j:(3 - sft) * 8 + j + 1], in1=acc[:, sft:],
                            op0=ALU.mult, op1=ALU.add), [t0])
                    t1 = r.op("act", lambda e, dst=dst: e.activation(out=dst[:], in_=acc[:], func=AF.Silu), [t0])
                    xr_readers[wi] = [t0]
                    tq.append(t1)
                else:
                    tlast = None
                    for tb in range(NTB):
                        sl = slice(tb * TB, (tb + 1) * TB)
                        b = 2 + (tb % 2)
                        tm = r.op("pe", lambda e, wi=wi, sl=sl, b=b: e.matmul(
                            ps[:, b, :], rt[:], xf[:, wi, sl], start=True, stop=True),
                            [tl, t_rt, t_ab_all] + ([tlast] if tlast else []) + prev_head_done)
                        ta = r.op("pool", lambda e, wi=wi, sl=sl: e.tensor_tensor(
                            out=acc[:, sl], in0=xf[:, wi, sl], in1=cs[:, 0, sl], op=ALU.mult), [tl, t_cos] + tq)
                        tb2 = r.op("dve", lambda e, sl=sl, b=b: e.tensor_tensor(
                            out=e1[:], in0=ps[:, b, :], in1=cs[:, 1, sl], op=ALU.mult), [tm, t_sin] + ([tlast] if tlast else []))
                        tlast = r.op("dve", lambda e, sl=sl, dst=dst: e.tensor_tensor(
                            out=dst[:, sl], in0=acc[:, sl], in1=e1[:], op=ALU.add), [ta, tb2])
                    xr_readers[wi] = [tlast]
                    tq.append(tlast)
            tv = r.dma("pool", lambda e, h=h: e.dma_start(out=vT[:], in_=ZT[(zv + h) * 128:(zv + h + 1) * 128, :]),
                       prev_head_done, r.dsem(True))
            tg_ = ld(go[:], ZT[(zg + h) * 128:(zg + h + 1) * 128, :], prev_head_done)
            tV = None
            for g4 in range(4):
                b = 2 + (g4 % 2)
                for i in range(4):
                    tt = g4 * 4 + i
                    tm = r.op("pe", lambda e, tt=tt, i=i, b=b: e.matmul(
                        ps[:, b, i * 128:(i + 1) * 128], vT[:, tt * 128:(tt + 1) * 128], ident[:],
                        start=True, stop=True), [tv, t_id, t_ab_all] + tq + ([tV] if tV else []), inc=(i == 3))
                tV = r.op("act", lambda e, g4=g4, b=b: e.activation(
                    out=V[:, g4 * 4:(g4 + 1) * 4, :], in_=ps[:, b, :].rearrange("p (a c) -> p a c", c=128),
                    func=AF.Copy), [tm])
            tgo = r.op("act", lambda e: e.activation(out=go[:], in_=go[:], func=AF.Sigmoid if ml else AF.Silu), [tg_])
            dring = Ring(r, 2, with_sems=False)
            pring = Ring(r, 2, with_sems=False)
            sring = Ring(r, 2, with_sems=False)
            pending = None
            for qb in range(NTB):
                qsl = slice(qb * TB, (qb + 1) * TB)
                nk = 4 * qb + 4
                tpv = None
                pset = dst_["blk"] % 2
                dst_["blk"] += 1
                ob = 4 + 2 * pset
                def stS(kt, qsl=qsl):
                    sb, sdeps = sring.next()
                    tS = r.op("pe", lambda e: e.matmul(
                        ps[:, 2 + sb, :], kT[:, kt * 128:(kt + 1) * 128], qT[:, qsl], start=True, stop=True),
                        tq + [tV] + sdeps)
                    return sb, tS
                nxt = stS(0)
                for kt in range(nk):
                    c = 512 * qb - 128 * kt
                    sb, tS = nxt
                    if kt + 1 < nk:
                        nxt = stS(kt + 1)
                    d, ddeps = dring.next()
                    tD = r.op("act", lambda e, d=d, h=h, kt=kt, qsl=qsl: e.activation(
                        out=Dt[:, d, :], in_=Ab[:, h, qsl], func=AF.Exp, bias=bcol[:, kt, h:h + 1], scale=1.0),
                        [t_ab_all, t_bc] + ddeps)
                    if c <= 0:
                        tD = r.op("pool", lambda e, d=d, c=c: e.tensor_tensor(
                            out=Dt[:, d, :], in0=Dt[:, d, :], in1=U[:, c + 384:c + 384 + TB], op=ALU.mult),
                            [tD, t_U])
                    p, pdeps = pring.next()
                    tP = r.op("dve", lambda e, p=p, d=d, sb=sb: e.tensor_tensor(
                        out=P[:, p, :], in0=ps[:, 2 + sb, :], in1=Dt[:, d, :], op=ALU.mult), [tS, tD] + pdeps)
                    sring.read(sb, tP)
                    dring.read(d, tP)
                    first = (kt == 0)
                    lastk = (kt == nk - 1)
                    bfree = dst_["bank_free"][pset]
                    r.op("pe", lambda e, p=p, kt=kt, first=first, lastk=lastk, ob=ob: e.matmul(
                        ps[:, ob, :], V[:, kt, :], P[:, p, :], start=first, stop=lastk),
                        [tP] + ([bfree] if first and bfree else []), inc=False)
                    tpv = r.op("pe", lambda e, p=p, first=first, lastk=lastk, ob=ob: e.matmul(
                        ps[:, ob + 1, :], onesb[:], P[:, p, :], start=first, stop=lastk), [t_on])
                    pring.read(p, tpv)
                    if kt >= 1 and pending is not None:
                        if next(pending, "done") == "done":
                            pending = None

                def ep(h=h, qsl=qsl, ob=ob, pset=pset, tpv=tpv):
                    ty = dst_["ty"]
                    if ml:
                        t1 = r.op("act", lambda e: e.activation(out=e1[:], in_=ps[:, ob + 1, :], func=AF.Abs),
                                  [tpv, ty])
                        yield
                        t1 = r.op("dve", lambda e: e.tensor_scalar_max(out=e1[:], in0=e1[:], scalar1=1.0), [t1])
                        t1 = r.op("act", lambda e: e.activation(out=e1[:], in_=e1[:], func=AF.Ln), [t1])
                        t1 = r.op("act", lambda e: e.activation(out=e1[:], in_=e1[:], func=AF.Exp, scale=-1.0), [t1])
                        t2 = r.op("dve", lambda e: e.tensor_tensor(out=e2[:], in0=ps[:, ob, :], in1=e1[:],
                                                                  op=ALU.mult), [t1])
                    else:
                        t2 = r.op("act", lambda e: e.activation(out=e2[:], in_=ps[:, ob, :], func=AF.Copy),
                                  [tpv, ty])
                    dst_["bank_free"][pset] = t2
                    yield
                    t3 = r.op("act", lambda e: e.activation(out=e3[:], in_=e2[:], func=AF.Copy), [t2])
                    yield
                    tm1 = r.op("pe", lambda e: e.matmul(ps[:, 0, :], avg[:], e3[:], start=True, stop=True),
                               [t3, t_av, t_bc])
                    yield
                    t4 = r.op("dve", lambda e: e.tensor_tensor(out=e2[:], in0=e2[:], in1=ps[:, 0, :],
                                                              op=ALU.subtract), [tm1])
                    yield
                    t5 = r.op("act", lambda e: e.activation(out=e3[:], in_=e2[:], func=AF.Square), [t4])
                    yield
                    tm2 = r.op("pe", lambda e: e.matmul(ps[:, 1, :], avg[:], e3[:], start=True, stop=True), [t5])
                    yield
                    t6 = r.op("act", lambda e: e.activation(out=e1[:], in_=ps[:, 1, :], func=AF.Ln, bias=EPS,
                                                           scale=1.0), [tm2])
                    t7 = r.op("act", lambda e: e.activation(out=e1[:], in_=e1[:], func=AF.Exp, scale=-0.5), [t6])
                    yield
                    t8 = r.op("dve", lambda e: e.tensor_tensor(out=e2[:], in0=e2[:], in1=e1[:], op=ALU.mult), [t7])
                    dst_["ty"] = r.op("dve", lambda e: e.scalar_tensor_tensor(
                        out=yb[:, qsl], in0=e2[:], scalar=gain[:, h:h + 1], in1=go[:, qsl], op0=ALU.mult,
                        op1=ALU.mult), [t8, tgo, t_gain])
                if pending is not None:
                    for _ in pending:
                        pass
                pending = ep()
            for _ in pending:
                pass
            pending = None
            ty = dst_["ty"]
            tst = r.dma("sp", lambda e, h=h: e.dma_start(out=YT[yrow + h * 128:yrow + (h + 1) * 128, :], in_=yb[:]),
                        [ty], r.dsem())
            prev_head_done = [tst, ty]
        r.emit()


def bucket_starts():
    n = np.arange(0, 4096)
    exact = 16
    lr = np.log(np.maximum(n, 1).astype(np.float32) / np.float32(exact)) / np.float32(np.log(128 / exact))
    large = np.minimum(exact + (lr.astype(np.float32) * np.float32(32 - exact)).astype(np.int32), 31)
    bk = np.where(n < exact, n, large)
    return [int(np.argmax(bk == b)) for b in range(32)], bk


def phase_setup(ctx, rb_row, TT, TTW, BC):
    nc = ctx.nc
    starts, _ = bucket_starts()
    W = 1408
    with ExitStack() as st:
        sb = lambda n, s, d: st.enter_context(_sbt(nc, n, s, d))
        rbr = sb("s_rbr", [1, 256], F32)
        one1 = sb("s_one1", [1, 128], F32)
        rbB = sb("s_rbB", [128, 32, 8], F32)
        eB = sb("s_eB", [128, 32, 8], F32)
        CB = sb("s_CB", [128, 32, 8], F32)
        dT = sb("s_dT", [128, W], F32)
        dC = sb("s_dC", [128, S], F32)
        mT = sb("s_mT", [128, W], F32)
        mC = sb("s_mC", [128, S], F32)
        aT = sb("s_aT", [128, 8, W], F32)
        aC = sb("s_aC", [128, 8, S], F32)
        aW = sb("s_aW", [128, 8, W], F32)
        r = Rec(ctx)
        ps = ctx.psum
        t0 = r.dma("sp", lambda e: e.dma_start(out=rbr[:], in_=rb_row), [], r.dsem())
        t1 = r.op("pool", lambda e: e.memset(one1[:], 1.0))
        tm = r.op("pe", lambda e: e.matmul(ps[:, 0, 0:256], one1[:], rbr[:], start=True, stop=True), [t0, t1])
        tc = r.op("act", lambda e: e.activation(out=rbB[:].rearrange("p b h -> p (b h)"), in_=ps[:, 0, 0:256],
                                               func=AF.Copy), [tm])
        td = tc
        for b in range(32):
            td = r.op("dve", lambda e, b=b: e.tensor_tensor(out=eB[:, b, :], in0=rbB[:, b, :], in1=rbB[:, 31, :],
                                                           op=ALU.subtract), [tc])
        te = r.op("act", lambda e: e.activation(out=eB[:].rearrange("p b h -> p (b h)"),
                                               in_=eB[:].rearrange("p b h -> p (b h)"), func=AF.Exp), [td])
        tcb = r.op("dve", lambda e: e.tensor_tensor(out=CB[:, 1:32, :], in0=eB[:, 1:32, :], in1=eB[:, 0:31, :],
                                                   op=ALU.subtract), [te])
        tcb = r.op("dve", lambda e: e.tensor_copy(out=CB[:, 0, :], in_=eB[:, 0, :]), [tcb])
        ti1 = r.op("pool", lambda e: e.iota(dT[:], [[1, W]], base=-384, channel_multiplier=-1,
                                           allow_small_or_imprecise_dtypes=True))
        ti2 = r.op("pool", lambda e: e.iota(dC[:], [[1, S]], base=-31, channel_multiplier=-16,
                                           allow_small_or_imprecise_dtypes=True))
        ta = None
        for b in range(32):
            sv = float(starts[b])
            tmk = r.op("dve", lambda e, sv=sv: e.tensor_single_scalar(out=mT[:], in_=dT[:], scalar=sv, op=ALU.is_ge),
                       [ti1, tcb] + ([ta] if ta else []))
            tmk2 = r.op("dve", lambda e, sv=sv: e.tensor_single_scalar(out=mC[:], in_=dC[:], scalar=sv, op=ALU.is_ge),
                        [ti2])
            for h in range(8):
                if b == 0:
                    r.op("dve", lambda e, h=h, b=b: e.tensor_scalar(out=aT[:, h, :], in0=mT[:], scalar1=CB[:, b, h:h + 1],
                                                                   scalar2=None, op0=ALU.mult), [tmk])
                    ta = r.op("dve", lambda e, h=h, b=b: e.tensor_scalar(out=aC[:, h, :], in0=mC[:],
                                                                        scalar1=CB[:, b, h:h + 1], scalar2=None,
                                                                        op0=ALU.mult), [tmk2])
                else:
                    r.op("dve", lambda e, h=h, b=b: e.scalar_tensor_tensor(
                        out=aT[:, h, :], in0=mT[:], scalar=CB[:, b, h:h + 1], in1=aT[:, h, :], op0=ALU.mult,
                        op1=ALU.add), [tmk])
                    ta = r.op("dve", lambda e, h=h, b=b: e.scalar_tensor_tensor(
                        out=aC[:, h, :], in0=mC[:], scalar=CB[:, b, h:h + 1], in1=aC[:, h, :], op0=ALU.mult,
                        op1=ALU.add), [tmk2])
        tw = r.op("dve", lambda e: e.tensor_single_scalar(out=mT[:], in_=dT[:], scalar=512.0, op=ALU.is_lt), [ta])
        for h in range(8):
            tw2 = r.op("dve", lambda e, h=h: e.tensor_tensor(out=aW[:, h, :], in0=aT[:, h, :], in1=mT[:], op=ALU.mult),
                       [tw])
        r.dma("pool", lambda e: e.dma_start(out=TT.rearrange("h p x -> p h x"), in_=aT[:]), [tw2], r.dsem(True))
        r.dma("pool", lambda e: e.dma_start(out=TTW.rearrange("h p x -> p h x"), in_=aW[:]), [tw2], r.dsem(True))
        r.dma("pool", lambda e: e.dma_start(out=BC.rearrange("h p x -> p h x"), in_=aC[:]), [tw2], r.dsem(True))
        r.emit()


QSCALE = float(128.0 ** -0.5)
GC1 = 0.044715
GC2 = 2.0 * float(np.sqrt(2.0 / np.pi))


def phase_nsa(ctx, ZT, YT, cst, par, TT, TTW, BC):
    nc = ctx.nc
    ZQ, ZKC, ZVC, ZKS, ZVS, ZKW, ZVW = 16, 24, 26, 28, 30, 32, 34
    with ExitStack() as st:
        sb = lambda n, s, d: st.enter_context(_sbt(nc, n, s, d))
        ident = sb("a_ident", [128, 128], BF16)
        onesb = sb("a_onesb", [128, 128], BF16)
        expand = sb("a_expand", [32, S], BF16)
        selg = sb("a_selg", [128, 24 * 128], F32)
        force = sb("a_force", [128, 16, 32], F32)
        SG = sb("a_SG", [128, S], F32)
        w1 = sb("a_w1", [128, 2, 32, 128], BF16)
        w2 = sb("a_w2", [128, 2, 128], BF16)
        posT = sb("a_pos", [128, 2, 32], F32)
        xf = sb("a_xf", [128, S], F32)
        xl = sb("a_xl", [128, 32, 127], BF16)
        g1 = sb("a_g1", [128, 128], F32)
        g2 = sb("a_g2", [128, 128], F32)
        gT = sb("a_gT", [128, 128], BF16)
        kcT = sb("a_kcT", [128, 128], BF16)
        VC = sb("a_VC", [128, 161], BF16)
        qT = sb("a_qT", [128, 4, S], BF16)
        ksT = sb("a_ksT", [128, S], BF16)
        kwT = sb("a_kwT", [128, S], BF16)
        vT = sb("a_vT", [128, S], BF16)
        VS = sb("a_VS", [128, 16, 128], BF16)
        VW = sb("a_VW", [128, 16, 128], BF16)
        imp = sb("a_imp", [128, 16, 32], F32)
        m8 = sb("a_m8", [128, 16, 8], F32)
        Mm = sb("a_M", [128, 16, 32], BF16)
        MT = sb("a_MT", [32, S], BF16)
        bc = sb("a_bc", [128, S], BF16)
        tt = sb("a_tt", [128, 1408], BF16)
        ttw = sb("a_ttw", [128, 1408], BF16)
        yacc = sb("a_yacc", [128, 4, S], F32)
        E = sb("a_E", [128, 2, TB], F32)
        P = sb("a_P", [128, 2, TB], BF16)
        e1 = sb("a_e1", [128, TB], F32)
        e2 = sb("a_e2", [128, TB], F32)
        rd = sb("a_rd", [128, 4], F32)
        ys = sb("a_ys", [128, S], BF16)
        gbS = sb("a_gbS", [128, 2, S], F32)
        r = Rec(ctx)
        ps = ctx.psum
        roles = {}

        def rsem(role, sw=False):
            if role is None:
                return r.dsem(sw)
            if role not in roles:
                roles[role] = r.dsem(sw)
            return roles[role]
        ld = lambda dst, src, deps=(), role=None: r.dma("sp", lambda e: e.dma_start(out=dst, in_=src), list(deps),
                                                       rsem(role))
        ldc = lambda dst, src, deps=(), role=None: r.dma("pool", lambda e: e.dma_start(out=dst, in_=src), list(deps),
                                                        rsem(role, True))
        t_id = ld(ident[:], cst["ident_bf"])
        t_ex = ld(expand[:], cst["expand_bf"])
        t_sg = ld(selg[64:96, :], cst["selg"])
        t_fo = ld(force[:], cst["force"])
        t_on = r.op("pool", lambda e: e.memset(onesb[:], 1.0))
        t_SG = ld(SG[64:96, :], ZT[52 * 128 + 64:52 * 128 + 96, :])
        t_SG = r.op("act", lambda e: e.activation(out=SG[64:96, :], in_=SG[64:96, :], func=AF.Sigmoid), [t_SG])
        t_w1 = [ldc(w1[:, i].rearrange("p l o -> p (l o)"), par["w1"][i]) for i in range(2)]
        t_w2 = [ldc(w2[:, i], par["w2"][i]) for i in range(2)]
        t_pos = ld(posT[:], par["posT"])
        gdone = []
        sring = Ring(r, 2, with_sems=False)
        mring = Ring(r, 2, with_sems=False)
        ering = Ring(r, 2, with_sems=False)
        pring = Ring(r, 2, with_sems=False)
        ep_prev = None
        ep2_prev = None
        tlast_any = None
        for g in range(2):
            t_init = r.op("pool", lambda e: e.memset(kcT[:], 0.0), gdone)
            t_init2 = r.op("pool", lambda e: e.memset(VC[:], 0.0), gdone)
            t_ov = ld(VC[:, 129:161], cst["ov_bf"], [t_init2], "ov")
            t_one = r.op("pool", lambda e: e.memset(VC[0:127, 128:129], 1.0), [t_init2])
            tcmp = []
            for i, zt in enumerate((ZKC + g, ZVC + g)):
                tl = ld(xf[:], ZT[zt * 128:(zt + 1) * 128, :], gdone + tcmp, "xf")
                xv = xf[:].rearrange("p (j i) -> p j i", i=16)
                tx = None
                for l in range(32):
                    j0 = 0 if l < 16 else 1
                    tx = r.op("dve", lambda e, l=l, j0=j0, i=i, xv=xv: e.tensor_scalar(
                        out=xl[:, l, :], in0=xv[:, j0:j0 + 127, l % 16], scalar1=posT[:, i, l:l + 1], scalar2=None,
                        op0=ALU.add), [tl, t_pos] + tcmp)
                b, bdeps = mring.next()
                tm = None
                for l in range(32):
                    tm = r.op("pe", lambda e, l=l, i=i, b=b: e.matmul(
                        ps[:, 2 + b, 0:127], w1[:, i, l, :], xl[:, l, :], start=(l == 0), stop=(l == 31)),
                        [tx, t_w1[i]] + bdeps, inc=(l == 31))
                pre = ps[:, 2 + b, 0:127]
                ta = r.op("act", lambda e, pre=pre: e.activation(out=g1[:, 0:127], in_=pre, func=AF.Square), [tm])
                ta = r.op("dve", lambda e: e.tensor_scalar(out=g1[:, 0:127], in0=g1[:, 0:127], scalar1=GC1,
                                                          scalar2=1.0, op0=ALU.mult, op1=ALU.add), [ta])
                ta = r.op("dve", lambda e, pre=pre: e.tensor_tensor(out=g1[:, 0:127], in0=g1[:, 0:127], in1=pre,
                                                                  op=ALU.mult), [ta])
                ta = r.op("act", lambda e: e.activation(out=g2[:, 0:127], in_=g1[:, 0:127], func=AF.Sigmoid,
                                                       scale=GC2), [ta])
                tg = r.op("dve", lambda e, pre=pre: e.tensor_tensor(out=gT[:, 0:127], in0=g2[:, 0:127], in1=pre,
                                                                  op=ALU.mult), [ta])
                mring.read(b, tg)
                b2, b2deps = mring.next()
                if i == 0:
                    tm2 = r.op("pe", lambda e, b2=b2: e.matmul(ps[:, 2 + b2, 0:127], w2[:, 0, :], gT[:, 0:127],
                                                              start=True, stop=True), [tg, t_w2[0]] + b2deps)
                    tk = r.op("act", lambda e, b2=b2: e.activation(out=kcT[:, 0:127], in_=ps[:, 2 + b2, 0:127],
                                                                  func=AF.Copy), [tm2, t_init])
                else:
                    tm2 = r.op("pe", lambda e, b2=b2: e.matmul(ps[0:127, 2 + b2, 0:128], gT[:, 0:127], w2[:, 1, :],
                                                              start=True, stop=True), [tg, t_w2[1]] + b2deps)
                    tk = r.op("act", lambda e, b2=b2: e.activation(out=VC[0:127, 0:128], in_=ps[0:127, 2 + b2, 0:128],
                                                                  func=AF.Copy), [tm2, t_init2])
                mring.read(b2, tk)
                tcmp = [tk]
            t_cmp = [tk, t_ov, t_one]
            if os.environ.get("NSA_STOP") == "1":
                r.emit()
                return
            t_q = [ldc(qT[:, rr, :], ZT[(ZQ + g * 4 + rr) * 128:(ZQ + g * 4 + rr + 1) * 128, :], gdone, "q%d" % rr)
                   for rr in range(4)]
            t_ks = ldc(ksT[:], ZT[(ZKS + g) * 128:(ZKS + g + 1) * 128, :], gdone, "ks")
            t_kw = ldc(kwT[:], ZT[(ZKW + g) * 128:(ZKW + g + 1) * 128, :], gdone, "kw")
            tV = {}
            tprev = t_cmp
            for nm, zt, dstV in (("s", ZVS + g, VS), ("w", ZVW + g, VW)):
                tv = ldc(vT[:], ZT[zt * 128:(zt + 1) * 128, :], gdone + ([tV["s"]] if nm == "w" else []), "vT")
                tvv = None
                for g4 in range(4):
                    b, bdeps = mring.next()
                    for i in range(4):
                        ttt = g4 * 4 + i
                        tm = r.op("pe", lambda e, ttt=ttt, i=i, b=b: e.matmul(
                            ps[:, 2 + b, i * 128:(i + 1) * 128], vT[:, ttt * 128:(ttt + 1) * 128], ident[:],
                            start=True, stop=True), [tv, t_id] + bdeps, inc=(i == 3))
                    tvv = r.op("act", lambda e, g4=g4, b=b, dstV=dstV: e.activation(
                        out=dstV[:, g4 * 4:(g4 + 1) * 4, :], in_=ps[:, 2 + b, :].rearrange("p (a c) -> p a c", c=128),
                        func=AF.Copy), [tm] + gdone)
                    mring.read(b, tvv)
                tV[nm] = tvv
            if os.environ.get("NSA_STOP") == "2":
                r.emit()
                return
            timp = None
            for rr in range(4):
                hq = g * 4 + rr
                t_bc = ldc(bc[:], BC[hq], [tlast_any] if tlast_any else [], "bc")
                for qb in range(NTB):
                    qsl = slice(qb * TB, (qb + 1) * TB)
                    s_, sdeps = sring.next()
                    tS = r.op("pe", lambda e, rr=rr, qsl=qsl, s_=s_: e.matmul(
                        ps[:, s_, :], kcT[:], qT[:, rr, qsl], start=True, stop=True), t_cmp + [t_q[rr]] + sdeps)
                    ee, edeps = ering.next()
                    tE = r.op("act", lambda e, ee=ee, s_=s_: e.activation(out=E[:, ee, :], in_=ps[:, s_, :],
                                                                        func=AF.Exp, scale=QSCALE), [tS] + edeps)
                    sring.read(s_, tE)
                    pp, pdeps = pring.next()
                    tP = r.op("dve", lambda e, pp=pp, ee=ee, qsl=qsl: e.tensor_tensor(
                        out=P[:, pp, :], in0=E[:, ee, :], in1=bc[:, qsl], op=ALU.mult), [tE, t_bc] + pdeps)
                    ering.read(ee, tP)
                    r.op("pe", lambda e, pp=pp: e.matmul(ps[:, 4, :], VC[:, 0:128], P[:, pp, :], start=True,
                                                        stop=True), [tP] + ([ep_prev] if ep_prev else []), inc=False)
                    r.op("pe", lambda e, pp=pp: e.matmul(ps[:, 5, :], onesb[:], P[:, pp, :], start=True, stop=True),
                         [t_on], inc=False)
                    tI = None
                    for i in range(4):
                        tI = r.op("pe", lambda e, pp=pp, i=i: e.matmul(
                            ps[:, 6, i * 64:i * 64 + 33], P[:, pp, i * 128:(i + 1) * 128], VC[:, 128:161],
                            start=True, stop=True), [ep2_prev] if (i == 0 and ep2_prev) else [], inc=(i == 3))
                    pring.read(pp, tI)
                    mb, mdeps = mring.next()
                    tgm = r.op("pe", lambda e, mb=mb, hq=hq, qsl=qsl: e.matmul(
                        ps[:, 2 + mb, :], selg[64:96, (0 * 8 + hq) * 128:(0 * 8 + hq + 1) * 128], SG[64:96, qsl],
                        start=True, stop=True), [t_SG, t_sg] + mdeps)
                    t1 = r.op("dve", lambda e: e.tensor_scalar_max(out=e1[:], in0=ps[:, 5, :], scalar1=1e-18),
                              [tI, tlast_any])
                    t1 = r.op("act", lambda e: e.activation(out=e1[:], in_=e1[:], func=AF.Ln), [t1])
                    t1 = r.op("act", lambda e: e.activation(out=e1[:], in_=e1[:], func=AF.Exp, scale=-1.0), [t1])
                    t2 = r.op("dve", lambda e: e.tensor_tensor(out=e2[:], in0=ps[:, 4, :], in1=e1[:], op=ALU.mult),
                              [t1])
                    ep_prev = t2
                    t3 = r.op("dve", lambda e, rr=rr, qsl=qsl, mb=mb: e.tensor_tensor(
                        out=yacc[:, rr, qsl], in0=e2[:], in1=ps[:, 2 + mb, :], op=ALU.mult), [t2, tgm] + gdone)
                    mring.read(mb, t3)
                    t4 = r.op("dve", lambda e: e.tensor_scalar_max(
                        out=rd[:], in0=ps[:, 6, 0:256].rearrange("p (i c) -> p i c", c=64)[:, :, 0], scalar1=1e-30),
                        [tI, t3])
                    t4 = r.op("dve", lambda e: e.reciprocal(out=rd[:], in_=rd[:]), [t4])
                    for i in range(4):
                        qt = qb * 4 + i
                        if rr == 0:
                            timp = r.op("dve", lambda e, i=i, qt=qt: e.tensor_scalar(
                                out=imp[:, qt, :], in0=ps[:, 6, i * 64 + 1:i * 64 + 33], scalar1=rd[:, i:i + 1],
                                scalar2=None, op0=ALU.mult), [t4] + gdone)
                        else:
                            timp = r.op("dve", lambda e, i=i, qt=qt: e.scalar_tensor_tensor(
                                out=imp[:, qt, :], in0=ps[:, 6, i * 64 + 1:i * 64 + 33], scalar=rd[:, i:i + 1],
                                in1=imp[:, qt, :], op0=ALU.mult, op1=ALU.add), [t4])
                    ep2_prev = timp
                    tlast_any = timp
            if os.environ.get("NSA_STOP") == "3":
                r.emit()
                return
            tk_ = r.op("dve", lambda e: e.tensor_tensor(out=imp[:], in0=imp[:], in1=force[:], op=ALU.add),
                       [timp, t_fo])
            for qt in range(16):
                tk1 = r.op("dve", lambda e, qt=qt: e.max(out=m8[:, qt, :], in_=imp[:, qt, :]), [tk_])
                tk2 = r.op("dve", lambda e, qt=qt: e.tensor_scalar(
                    out=Mm[:, qt, :], in0=imp[:, qt, :], scalar1=m8[:, qt, 7:8], scalar2=None, op0=ALU.is_ge),
                    [tk1] + gdone)
            tMT = None
            for g4 in range(4):
                b, bdeps = mring.next()
                for i in range(4):
                    qt = g4 * 4 + i
                    tm = r.op("pe", lambda e, qt=qt, i=i, b=b: e.matmul(
                        ps[0:32, 2 + b, i * 128:(i + 1) * 128], Mm[:, qt, :], ident[:], start=True, stop=True),
                        [tk2, t_id] + bdeps, inc=(i == 3))
                tMT = r.op("act", lambda e, g4=g4, b=b: e.activation(
                    out=MT[:, g4 * TB:(g4 + 1) * TB], in_=ps[0:32, 2 + b, :], func=AF.Copy), [tm] + gdone)
                mring.read(b, tMT)
            if os.environ.get("NSA_STOP") == "4":
                r.emit()
                return
            pend = [None]
            for rr in range(4):
                hq = g * 4 + rr
                t_tt = ldc(tt[:], TT[hq], [tlast_any], "tt")
                t_tw = ldc(ttw[:], TTW[hq], [tlast_any], "ttw")
                t_gb = None
                for bi, br_ in enumerate((1, 2)):
                    for qb_ in range(NTB):
                        mb, mdeps = mring.next()
                        tgm = r.op("pe", lambda e, mb=mb, br_=br_, qb_=qb_, hq=hq: e.matmul(
                            ps[:, 2 + mb, :], selg[64:96, (br_ * 8 + hq) * 128:(br_ * 8 + hq + 1) * 128],
                            SG[64:96, qb_ * TB:(qb_ + 1) * TB], start=True, stop=True), [t_SG, t_sg] + mdeps)
                        t_gb = r.op("act", lambda e, mb=mb, bi=bi, qb_=qb_: e.activation(
                            out=gbS[:, bi, qb_ * TB:(qb_ + 1) * TB], in_=ps[:, 2 + mb, :], func=AF.Copy),
                            [tgm, tlast_any])
                        mring.read(mb, t_gb)
                for qb in range(NTB):
                    qsl = slice(qb * TB, (qb + 1) * TB)
                    for br in (2, 1):
                        win = (br == 2)
                        kts = list(range(max(0, 4 * qb - 4), 4 * qb + 4)) if win else list(range(0, 4 * qb + 4))
                        Kt = kwT if win else ksT
                        Vt = VW if win else VS
                        tkk = t_kw if win else t_ks
                        ob = 4 if win else 6
                        tpv = None
                        def stS(kt, rr=rr, qsl=qsl, Kt=Kt, win=win, tkk=tkk):
                            s_, sdeps = sring.next()
                            tS = r.op("pe", lambda e: e.matmul(
                                ps[:, s_, :], Kt[:, kt * 128:(kt + 1) * 128], qT[:, rr, qsl], start=True, stop=True),
                                [tkk, t_q[rr], tV["w"]] + sdeps)
                            mb = tM = None
                            if not win:
                                mb, mdeps = mring.next()
                                tM = r.op("pe", lambda e: e.matmul(
                                    ps[:, 2 + mb, :], expand[:, kt * 128:(kt + 1) * 128], MT[:, qsl], start=True,
                                    stop=True), [tMT, t_ex] + mdeps)
                            return s_, tS, mb, tM
                        NOLA = bool(os.environ.get("NSA_NOLA"))
                        nxt = None if NOLA else stS(kts[0])
                        for n_, kt in enumerate(kts):
                            c = 512 * qb - 128 * kt
                            if NOLA:
                                s_, tS, mb, tM = stS(kt)
                            else:
                                s_, tS, mb, tM = nxt
                                if n_ + 1 < len(kts):
                                    nxt = stS(kts[n_ + 1])
                            ee, edeps = ering.next()
                            tE = r.op("act", lambda e, ee=ee, s_=s_: e.activation(
                                out=E[:, ee, :], in_=ps[:, s_, :], func=AF.Exp, scale=QSCALE), [tS] + edeps)
                            sring.read(s_, tE)
                            pp, pdeps = pring.next()
                            if win:
                                tP = r.op("dve", lambda e, pp=pp, ee=ee, c=c: e.tensor_tensor(
                                    out=P[:, pp, :], in0=E[:, ee, :], in1=ttw[:, c + 384:c + 384 + TB], op=ALU.mult),
                                    [tE, t_tw] + pdeps)
                            else:
                                if c < 256:
                                    tE = r.op("pool", lambda e, ee=ee, c=c: e.tensor_tensor(
                                        out=E[:, ee, :], in0=E[:, ee, :], in1=tt[:, c + 384:c + 384 + TB],
                                        op=ALU.mult), [tE, t_tt])
                                tP = r.op("dve", lambda e, pp=pp, ee=ee, mb=mb: e.tensor_tensor(
                                    out=P[:, pp, :], in0=E[:, ee, :], in1=ps[:, 2 + mb, :], op=ALU.mult),
                                    [tE, tM] + pdeps)
                                mring.read(mb, tP)
                            ering.read(ee, tP)
                            first = (n_ == 0)
                            lastk = (n_ == len(kts) - 1)
                            prevdep = (ep_prev if win else ep2_prev)
                            r.op("pe", lambda e, pp=pp, kt=kt, first=first, lastk=lastk, Vt=Vt, ob=ob: e.matmul(
                                ps[:, ob, :], Vt[:, kt, :], P[:, pp, :], start=first, stop=lastk),
                                [tP] + ([prevdep] if first and prevdep else []), inc=False)
                            tpv = r.op("pe", lambda e, pp=pp, first=first, lastk=lastk, ob=ob: e.matmul(
                                ps[:, ob + 1, :], onesb[:], P[:, pp, :], start=first, stop=lastk), [t_on])
                            pring.read(pp, tpv)
                            if n_ >= 1 and pend[0] is not None:
                                if next(pend[0], "done") == "done":
                                    pend[0] = None

                        def ep(hq=hq, qsl=qsl, br=br, ob=ob, win=win, tpv=tpv, rr=rr, t_gb=t_gb):
                            nonlocal ep_prev, ep2_prev, tlast_any
                            if os.environ.get("NSA_RECIP"):
                                t1 = r.op("dve", lambda e: e.reciprocal(out=e1[:], in_=ps[:, ob + 1, :]),
                                          [tpv, tlast_any])
                            else:
                                t1 = r.op("act", lambda e: e.activation(out=e1[:], in_=ps[:, ob + 1, :], func=AF.Ln),
                                          [tpv, tlast_any])
                                t1 = r.op("act", lambda e: e.activation(out=e1[:], in_=e1[:], func=AF.Exp,
                                                                       scale=-1.0), [t1])
                            t2 = r.op("dve", lambda e: e.tensor_tensor(out=e2[:], in0=ps[:, ob, :], in1=e1[:],
                                                                      op=ALU.mult), [t1])
                            if win:
                                ep_prev = t2
                            else:
                                ep2_prev = t2
                            tlast_any = t2
                            yield
                            t3 = r.op("dve", lambda e: e.tensor_tensor(out=e2[:], in0=e2[:], in1=gbS[:, br - 1, qsl],
                                                                      op=ALU.mult), [t2, t_gb])
                            tlast_any = r.op("dve", lambda e: e.tensor_tensor(
                                out=yacc[:, rr, qsl], in0=yacc[:, rr, qsl], in1=e2[:], op=ALU.add), [t3])
                        if pend[0] is not None:
                            for _ in pend[0]:
                                pass
                        pend[0] = ep()
                if pend[0] is not None:
                    for _ in pend[0]:
                        pass
                    pend[0] = None
                tys = r.op("act", lambda e, rr=rr: e.activation(out=ys[:], in_=yacc[:, rr, :], func=AF.Copy),
                           [tlast_any] + gdone)
                tst = r.dma("sp", lambda e, hq=hq: e.dma_start(out=YT[512 + hq * 128:512 + (hq + 1) * 128, :],
                                                              in_=ys[:]), [tys], rsem("st"))
                gdone = [tst, tys]
            gdone = gdone + [tlast_any]
        r.emit()


def phase_merge(ctx, GL, YT, MTo, wgu, wbr, bg_cols):
    nc = ctx.nc
    ybase = [0, 4, 12]
    ykc = [4, 8, 4]
    with ExitStack() as st:
        sb = lambda n, s, d: st.enter_context(_sbt(nc, n, s, d))
        gl = sb("m_gl", [128, 8, S], BF16)
        yt = sb("m_yt", [128, 16, S], BF16)
        wg = sb("m_wg", [128, 2, 3, 8 * 128], BF16)
        wb = sb("m_wb", [128, 2, 16 * 128], BF16)
        bg = sb("m_bg", [128, 96], F32)
        sg = sb("m_sg", [128, 2, TB], F32)
        acc = sb("m_acc", [128, TB], F32)
        tmp = sb("m_tmp", [128, TB], F32)
        ob = sb("m_ob", [128, 2, S], BF16)
        r = Rec(ctx)
        ps = ctx.psum
        t_bg = r.dma("sp", lambda e: e.dma_start(out=bg[:], in_=bg_cols), [], r.dsem())
        t_gl = r.dma("sp", lambda e: e.dma_start(out=gl[:], in_=GL.rearrange("(c p) t -> p c t", p=128)), [], r.dsem())
        t_yt = [r.dma("sp", lambda e, i=i: e.dma_start(
            out=yt[:, i * 8:(i + 1) * 8, :], in_=YT.rearrange("(c p) t -> p c t", p=128)[:, i * 8:(i + 1) * 8, :]), [],
            r.dsem()) for i in range(2)]
        wring = Ring(r, 2, sw=True)
        wsem2 = [r.dsem(True), r.dsem(True)]
        gring = Ring(r, 2, with_sems=False)
        bring = Ring(r, 2, with_sems=False)
        sring = Ring(r, 2, with_sems=False)
        oring = Ring(r, 2)
        tacc = None
        for mt in range(KC):
            ws, wdeps = wring.next()
            tw1 = r.dma("pool", lambda e, ws=ws, mt=mt: e.dma_start(
                out=wg[:, ws], in_=wgu.rearrange("(b m) p k -> m p b k", b=3)[mt]), wdeps, wring.sems[ws])
            tw2s = []
            off = 0
            for b in range(3):
                n = ykc[b] * 128
                tw2s.append(r.dma("pool", lambda e, ws=ws, mt=mt, b=b, off=off, n=n: e.dma_start(
                    out=wb[:, ws, off:off + n], in_=wbr[b][mt]), wdeps, wsem2[ws]))
                off += n
            o, odeps = oring.next()
            tlastmm = None
            for tb in range(NTB):
                tsl = slice(tb * TB, (tb + 1) * TB)
                for b in range(3):
                    gb, gdeps = gring.next()
                    tmg = None
                    for c in range(8):
                        tmg = r.op("pe", lambda e, ws=ws, b=b, c=c, gb=gb, tsl=tsl: e.matmul(
                            ps[:, gb, :], wg[:, ws, b, c * 128:(c + 1) * 128], gl[:, c, tsl], start=(c == 0),
                            stop=(c == 7)), [tw1, t_gl] + gdeps, inc=(c == 7))
                    bb, bdeps = bring.next()
                    boff = sum(ykc[:b]) * 128
                    tmb = None
                    for c in range(ykc[b]):
                        tmb = r.op("pe", lambda e, ws=ws, b=b, c=c, bb=bb, tsl=tsl, boff=boff: e.matmul(
                            ps[:, 2 + bb, :], wb[:, ws, boff + c * 128:boff + (c + 1) * 128],
                            yt[:, ybase[b] + c, tsl], start=(c == 0), stop=(c == ykc[b] - 1)),
                            tw2s + t_yt + bdeps, inc=(c == ykc[b] - 1))
                    tlastmm = tmb
                    s_, sdeps = sring.next()
                    tsg = r.op("act", lambda e, s_=s_, gb=gb, b=b, mt=mt: e.activation(
                        out=sg[:, s_, :], in_=ps[:, gb, :], func=AF.Sigmoid,
                        bias=bg[:, b * 32 + mt:b * 32 + mt + 1], scale=1.0), [tmg, t_bg] + sdeps)
                    gring.read(gb, tsg)
                    if b == 0:
                        tacc = r.op("dve", lambda e, s_=s_, bb=bb: e.tensor_tensor(
                            out=acc[:], in0=sg[:, s_, :], in1=ps[:, 2 + bb, :], op=ALU.mult), [tsg, tmb, tacc])
                    elif b == 1:
                        t_ = r.op("dve", lambda e, s_=s_, bb=bb: e.tensor_tensor(
                            out=tmp[:], in0=sg[:, s_, :], in1=ps[:, 2 + bb, :], op=ALU.mult), [tsg, tmb, tacc])
                        tacc = r.op("dve", lambda e: e.tensor_tensor(out=acc[:], in0=acc[:], in1=tmp[:], op=ALU.add),
                                    [t_])
                    else:
                        t_ = r.op("dve", lambda e, s_=s_, bb=bb: e.tensor_tensor(
                            out=tmp[:], in0=sg[:, s_, :], in1=ps[:, 2 + bb, :], op=ALU.mult), [tsg, tmb, tacc])
                        tacc = r.op("dve", lambda e, o=o, tsl=tsl: e.tensor_tensor(
                            out=ob[:, o, tsl], in0=acc[:], in1=tmp[:], op=ALU.add), [t_] + odeps)
                    sring.read(s_, tacc if b == 0 else t_)
                    bring.read(bb, tacc if b == 0 else t_)
            wring.read(ws, tlastmm)
            ts = r.dma("sp", lambda e, o=o, mt=mt: e.dma_start(out=MTo[mt * 128:(mt + 1) * 128, :], in_=ob[:, o, :]),
                       [tacc], oring.sems[o])
            oring.read(o, ts)
        r.emit()


CONST_SPECS = None


class LazyInputs:
    def __init__(self, nc, L, consts):
        import ml_dtypes
        self.nc = nc
        self.decl = {}
        sp = {}
        sp["xT"] = ([D, S], F32)
        sp["rb"] = ([1, 256], F32)
        for k, v in consts.items():
            sp["c_" + k] = (list(v.shape), BF16 if v.dtype == ml_dtypes.bfloat16 else F32)
        for n, shp in [("g1c", [L, 128, KC]), ("g2c", [L, 128, KC]), ("gfc", [128, KC]),
                       ("win_t", [L, 53, 128, D]), ("bin_c", [L, 128, 53]), ("wgd_t", [L, 8, 128, D]),
                       ("conv_c", [L, 128, 32]), ("mgain", [L, 128, 4]), ("rgain", [L, 128, 4]),
                       ("w1", [L, 2, 128, 4096]), ("w2", [L, 2, 128, 128]), ("posT", [L, 128, 2, 32]),
                       ("wgu_t", [L, 96, 128, 1024]), ("bg_c", [L, 128, 96]),
                       ("wbrm_t", [L, 32, 128, 512]), ("wbrn_t", [L, 32, 128, 1024]), ("wbrr_t", [L, 32, 128, 512]),
                       ("wout_t", [L, 32, 128, D]), ("wup_t", [L, 128, 128, D]), ("wdn", [L, 4 * D, D])]:
            sp[n] = (shp, F32)
        self.sp = sp

    def __getitem__(self, name):
        if name not in self.decl:
            shp, dt = self.sp[name]
            self.decl[name] = self.nc.dram_tensor(name, list(shp), dt, kind="ExternalInput").ap()
        return self.decl[name]


def declare_inputs(nc, L, consts):
    return LazyInputs(nc, L, consts)


def build_program(L, consts, debug=False, upto=99):
    nc = bass.Bass("TRN2", target_bir_lowering=False)
    I = declare_inputs(nc, L, consts)
    yT = nc.dram_tensor("yT", [D, S], F32, kind="ExternalOutput").ap()
    kind = "ExternalOutput" if debug else "Internal"
    XA = nc.dram_tensor("XA", [D, S], F32, kind=kind).ap()
    XB = nc.dram_tensor("XB", [D, S], F32, kind=kind).ap()
    HT = nc.dram_tensor("HT", [D, S], BF16).ap()
    ZT = nc.dram_tensor("ZT", [ZROWS, S], F32).ap()
    GL = nc.dram_tensor("GL", [1024, S], BF16).ap()
    YT = nc.dram_tensor("YT", [2048, S], BF16, kind=kind).ap()
    MT = nc.dram_tensor("MT", [D, S], BF16).ap()
    TT = nc.dram_tensor("TT", [8, 128, 1408], BF16).ap()
    TTW = nc.dram_tensor("TTW", [8, 128, 1408], BF16).ap()
    BC = nc.dram_tensor("BC", [8, 128, S], BF16).ap()
    class _C(dict):
        def __missing__(self, k):
            return I["c_" + k]
    cst = _C()
    ctx = Ctx(nc)
    with nc.psum_tensor("ps", [128, 8, 512], F32) as ps:
        ctx.psum = ps
        step = [0]

        def go():
            step[0] += 1
            return step[0] <= upto
        if go():
            phase_setup(ctx, I["rb"], TT, TTW, BC)
        xcur = I["xT"]
        for l in range(L):
            if go() and not os.environ.get("SKIP2"):
                phase_norm(ctx, xcur, HT, I["g1c"][l])
            if go() and not os.environ.get("SKIP3"):
                jobs = [dict(w=I["win_t"][l, m], kind="z", bias=m, out=ZT[m * 128:(m + 1) * 128, :]) for m in range(53)]
                jobs += [dict(w=I["wgd_t"][l, m], kind="bf", out=GL[m * 128:(m + 1) * 128, :]) for m in range(8)]
                phase_linear(ctx, HT, D, jobs, bias_cols=I["bin_c"][l], nbias=53)
            if go() and not os.environ.get("SKIP4"):
                phase_decay(ctx, "mlstm", ZT, YT, 0, 4, 8, 12, 0, cst, dict(gain=I["mgain"][l], conv_cols=I["conv_c"][l]))
            if go() and not os.environ.get("SKIP5"):
                phase_decay(ctx, "ret", ZT, YT, 36, 40, 44, 48, 1536, cst, dict(gain=I["rgain"][l]))
            if go():
                phase_nsa(ctx, ZT, YT, cst, dict(w1=I["w1"][l], w2=I["w2"][l], posT=I["posT"][l]), TT, TTW, BC)
            if go():
                phase_merge(ctx, GL, YT, MT, I["wgu_t"][l], [I["wbrm_t"][l], I["wbrn_t"][l], I["wbrr_t"][l]], I["bg_c"][l])
            if go():
                jobs = [dict(w=I["wout_t"][l, m], kind="res", resid=xcur[m * 128:(m + 1) * 128, :],
                             out=XA[m * 128:(m + 1) * 128, :]) for m in range(KC)]
                phase_linear(ctx, MT, D, jobs)
            if go():
                phase_norm(ctx, XA, HT, I["g2c"][l])
            if go():
                phase_mlp(ctx, HT, XA, XB, I["wup_t"][l], I["wdn"][l])
            xcur = XB
        if go():
            phase_norm(ctx, xcur, yT, I["gfc"], out_f32=True)
    nc._lazy_inputs = I
    return nc


def prep_weights(inp, L, layers=None):
    layers = list(range(L)) if layers is None else layers
    cm, _ = in_colmap()
    W = {}
    W["rb"] = np.ascontiguousarray(inp["rel_bias"].reshape(1, 256))
    W["g1c"] = np.stack([cols_layout(inp["norm_mix_g"][l]) for l in layers])
    W["g2c"] = np.stack([cols_layout(inp["norm_mlp_g"][l]) for l in layers])
    W["gfc"] = cols_layout(inp["final_norm_g"])
    W["win_t"] = np.stack([tile_w(permute_cols(inp["w_in"][l], cm)) for l in layers])
    W["bin_c"] = np.stack([cols_layout(permute_cols(inp["b_in"][l], cm)) for l in layers])
    W["wgd_t"] = np.stack([tile_w(inp["w_gate_down"][l]) for l in layers])
    W["conv_c"] = np.stack([np.ascontiguousarray(inp["conv_qk"][l].reshape(4, 8, 128).transpose(2, 0, 1).reshape(128, 32))
                            for l in layers])
    W["mgain"] = np.stack([cols_layout(inp["mlstm_norm_g"][l]) for l in layers])
    W["rgain"] = np.stack([cols_layout(inp["ret_norm_g"][l]) for l in layers])
    W["w1"] = np.stack([np.stack([np.ascontiguousarray(inp[k][l].reshape(32, 128, 128).transpose(1, 0, 2)).reshape(128, 4096)
                                  for k in ("cmp_w1_k", "cmp_w1_v")]) for l in layers])
    W["w2"] = np.stack([np.stack([inp["cmp_w2_k"][l], inp["cmp_w2_v"][l]]) for l in layers])
    W["posT"] = np.stack([np.ascontiguousarray(np.stack([inp["cmp_pos_k"][l].T, inp["cmp_pos_v"][l].T], 1))
                          for l in layers])
    W["wgu_t"] = np.stack([tile_w(inp["w_gate_up"][l]) for l in layers])
    W["bg_c"] = np.stack([cols_layout(inp["b_gate"][l]) for l in layers])
    W["wbrm_t"] = np.stack([tile_w(inp["w_br_mlstm"][l]) for l in layers])
    W["wbrn_t"] = np.stack([tile_w(inp["w_br_nsa"][l]) for l in layers])
    W["wbrr_t"] = np.stack([tile_w(inp["w_br_ret"][l]) for l in layers])
    W["wout_t"] = np.stack([tile_w(inp["w_out"][l]) for l in layers])
    W["wup_t"] = np.stack([tile_w(inp["w_up"][l]) for l in layers])
    W["wdn"] = np.stack([np.ascontiguousarray(inp["w_down"][l]) for l in layers])
    return W


import ml_dtypes
def make_consts():
    c={}
    sel=np.zeros((64,4*128),np.float32)
    for h in range(4): sel[32+h,h*128:(h+1)*128]=1.0
    c["sel64"]=sel
    c["i64"]=np.eye(64,dtype=np.float32)
    c["ident_bf"]=np.eye(128,dtype=np.float32).astype(ml_dtypes.bfloat16)
    ik=np.arange(128)[:,None]; x=np.arange(896)[None,:]
    c["U_bf"]=((x-384-ik)>=0).astype(np.float32).astype(ml_dtypes.bfloat16)
    G=np.zeros((64,S),np.float32)
    lg=np.log1p(-np.exp2(-5.0-np.arange(4,dtype=np.float32))).astype(np.float32)
    G[32:36,:]=lg[:,None]*np.arange(S,dtype=np.float32)[None,:]
    c["ret_G"]=G
    half=64
    inv_freq=(1.0/(10000.0**np.linspace(0.0,1.0,half,dtype=np.float32))).astype(np.float32)
    ang=np.arange(S,dtype=np.float32)[:,None]*inv_freq[None,:]
    cos=np.cos(ang).astype(np.float32).T; sin=np.sin(ang).astype(np.float32).T
    c["cosT"]=np.ascontiguousarray(np.concatenate([cos,cos],0)); c["sinT"]=np.ascontiguousarray(np.concatenate([sin,sin],0))
    R=np.zeros((128,128),np.float32)
    for m in range(64): R[m+64,m]=-1.0
    for m in range(64,128): R[m-64,m]=1.0
    c["rotT"]=R
    return c
def make_consts_nsa(c):
    k=np.arange(S)[None,:]; s=np.arange(32)[:,None]
    c["expand_bf"]=((k//64)==s).astype(np.float32).astype(ml_dtypes.bfloat16)
    t=(np.arange(16)[None,:,None]*128+np.arange(128)[:,None,None]); cur=t//64; sb=np.arange(32)[None,None,:]
    F=np.zeros((128,16,32),np.float32)
    F[sb>cur]=-1e4
    F[(sb==0)|(sb==cur)|(sb==cur-1)]=1e4
    c["force"]=F
    j=np.arange(128)[:,None]; ss=np.arange(32)[None,:]
    ov=((16*j<64*ss+64)&(16*j+32>64*ss)&(j<=126)).astype(np.float32)
    c["ov_bf"]=ov.astype(ml_dtypes.bfloat16)
    sg=np.zeros((32,24*128),np.float32)
    for i in range(24): sg[i,i*128:(i+1)*128]=1.0
    c["selg"]=sg
    return c

N_CORES = 8
DEPTH = 4


def kernel(**inputs):
    consts = make_consts_nsa(make_consts())
    nc = build_program(DEPTH, consts)
    W = prep_weights(inputs, DEPTH)
    x = np.asarray(inputs["x"], dtype=np.float32)
    decl = nc._lazy_inputs.decl
    base = dict(W)
    for k, v in consts.items():
        base["c_" + k] = v
    in_maps = []
    for b in range(N_CORES):
        m = {k: v for k, v in base.items() if k in decl}
        m["xT"] = np.ascontiguousarray(x[b].T)
        in_maps.append(m)
    res = run_bass_kernel_spmd(nc, in_maps, core_ids=list(range(N_CORES)))
    out = np.stack([np.ascontiguousarray(np.asarray(res.results[b]["yT"]).T) for b in range(N_CORES)])
    return out.astype(np.float32)
```
